# Optimizing a Trainium2 kernel written in Bass

```python
import math
import jax, jax.numpy as jnp
from jax import lax
import numpy as np

D_MODEL = 1024
BATCH = 2
SEQ = 8192
DEPTH = 2

CHUNK = 64
N_MIXERS = 2
N_LAYERS_A = (DEPTH + 1) // 2
N_LAYERS_B = DEPTH // 2

A_HEADS = 8
A_HEAD_DIM = 64
A_V_DIM = 2 * A_HEAD_DIM
Q_BLOCK = 128
T5_BUCKETS = 32
T5_MAX_DIST = 1024

B_HEADS = 16
B_HEAD_DIM = D_MODEL // B_HEADS
LEFT_CHUNKS = 8
BAND = (LEFT_CHUNKS + 1) * CHUNK
MAX_REL = 256

N_EXPERTS = 16
N_GROUPS = 4
E_PER_GROUP = N_EXPERTS // N_GROUPS
TOPK_GROUPS = 1
TOP_K = 2
D_FF_EXPERT = 512

NORM_EPS = 1e-6
NEG_INF = -1e30

kernel_name = "hybrid_diffattn_chunkattn_grouped_moe"


def rms_norm(x, g):
    xf = x.astype(jnp.float32)
    y = xf * lax.rsqrt(jnp.mean(xf * xf, axis=-1, keepdims=True) + NORM_EPS)
    return (y * g.astype(jnp.float32)).astype(x.dtype)


def t5_bucket(rel):
    nb = T5_BUCKETS // 2
    ret = jnp.where(rel > 0, nb, 0)
    n = jnp.abs(rel)
    max_exact = nb // 2
    nf = jnp.maximum(n, 1).astype(jnp.float32)
    large = max_exact + (jnp.log(nf / max_exact) / math.log(T5_MAX_DIST / max_exact)
                         * (nb - max_exact)).astype(jnp.int32)
    large = jnp.minimum(large, nb - 1)
    return ret + jnp.where(n < max_exact, n, large)


def diff_attention(h, w_qkv, q_gain, k_gain, lam, subln_g, w_o, t5_bias, lambda_init):
    B, S, D = h.shape
    qkv = h @ w_qkv
    q, k, v = jnp.split(qkv, 3, axis=-1)
    q = q.reshape(B, S, A_HEADS, 2, A_HEAD_DIM)
    k = k.reshape(B, S, A_HEADS, 2, A_HEAD_DIM)
    v = v.reshape(B, S, A_HEADS, A_V_DIM)
    q = rms_norm(q, q_gain) * (A_HEAD_DIM ** -0.5)
    k = rms_norm(k, k_gain)
    lamf = lam.astype(jnp.float32)
    lam_full = (jnp.exp(jnp.sum(lamf[0] * lamf[1])) - jnp.exp(jnp.sum(lamf[2] * lamf[3]))
                + lambda_init)
    n_blocks = S // Q_BLOCK
    q_blocks = q.reshape(B, n_blocks, Q_BLOCK, A_HEADS, 2, A_HEAD_DIM).transpose(1, 0, 2, 3, 4, 5)
    k_pos = jnp.arange(S, dtype=jnp.int32)

    def one_block(args):
        qb, blk = args
        q_pos = blk * Q_BLOCK + jnp.arange(Q_BLOCK, dtype=jnp.int32)
        rel = k_pos[None, :] - q_pos[:, None]
        bias = t5_bias.astype(jnp.float32)[t5_bucket(rel)].transpose(2, 0, 1)
        allowed = (k_pos[None, :] // CHUNK) <= (q_pos[:, None] // CHUNK)
        s = jnp.einsum('bqhmd,bkhmd->bmhqk', qb, k).astype(jnp.float32) + bias[None, None]
        s = jnp.where(allowed[None, None, None], s, NEG_INF)
        p = jax.nn.softmax(s, axis=-1)
        a = p[:, 0] - lam_full * p[:, 1]
        return jnp.einsum('bhqk,bkhe->bqhe', a.astype(v.dtype), v)

    o = lax.map(one_block, (q_blocks, jnp.arange(n_blocks, dtype=jnp.int32)))
    o = o.transpose(1, 0, 2, 3, 4).reshape(B, S, A_HEADS, A_V_DIM)
    o = rms_norm(o, subln_g) * (1.0 - lambda_init)
    return o.reshape(B, S, D) @ w_o


def chunked_attention(h, w_qkv, q_gain, k_gain, rel_bias, w_o):
    B, S, D = h.shape
    NC = S // CHUNK
    qkv = h @ w_qkv
    q, k, v = jnp.split(qkv, 3, axis=-1)
    q = rms_norm(q.reshape(B, S, B_HEADS, B_HEAD_DIM), q_gain) * (B_HEAD_DIM ** -0.5)
    k = rms_norm(k.reshape(B, S, B_HEADS, B_HEAD_DIM), k_gain)
    v = v.reshape(B, S, B_HEADS, B_HEAD_DIM)
    pad = ((0, 0), (LEFT_CHUNKS * CHUNK, 0), (0, 0), (0, 0))
    kp = jnp.pad(k, pad)
    vp = jnp.pad(v, pad)
    qi = jnp.arange(CHUNK, dtype=jnp.int32)
    kb = jnp.arange(BAND, dtype=jnp.int32)
    rel = kb[None, :] - LEFT_CHUNKS * CHUNK - qi[:, None]
    idx = jnp.clip(rel, -MAX_REL, MAX_REL) + MAX_REL
    bias = rel_bias.astype(jnp.float32)[:, idx]
    band_chunk = kb // CHUNK
    q_chunks = q.reshape(B, NC, CHUNK, B_HEADS, B_HEAD_DIM).transpose(1, 0, 2, 3, 4)

    def one_chunk(args):
        qc, ci = args
        kband = lax.dynamic_slice_in_dim(kp, ci * CHUNK, BAND, axis=1)
        vband = lax.dynamic_slice_in_dim(vp, ci * CHUNK, BAND, axis=1)
        valid = (ci - LEFT_CHUNKS + band_chunk) >= 0
        s = jnp.einsum('bqhd,bkhd->bhqk', qc, kband).astype(jnp.float32) + bias[None]
        s = jnp.where(valid[None, None, None, :], s, NEG_INF)
        p = jax.nn.softmax(s, axis=-1)
        return jnp.einsum('bhqk,bkhd->bqhd', p.astype(vband.dtype), vband)

    o = lax.map(one_chunk, (q_chunks, jnp.arange(NC, dtype=jnp.int32)))
    o = o.transpose(1, 0, 2, 3, 4).reshape(B, S, D)
    return o @ w_o


def grouped_moe(h, router_w, router_bias, w_gate, w_up, w_down):
    B, S, D = h.shape
    t = h.reshape(B * S, D)
    scores = jax.nn.sigmoid((t @ router_w).astype(jnp.float32))
    sel = scores + router_bias.astype(jnp.float32)
    grp = sel.reshape(-1, N_GROUPS, E_PER_GROUP)
    group_score = lax.top_k(grp, TOP_K)[0].sum(-1)
    _, g_idx = lax.top_k(group_score, TOPK_GROUPS)
    group_mask = jax.nn.one_hot(g_idx, N_GROUPS, dtype=jnp.float32).sum(1) > 0
    expert_mask = jnp.repeat(group_mask, E_PER_GROUP, axis=1)
    masked = jnp.where(expert_mask, sel, NEG_INF)
    _, e_idx = lax.top_k(masked, TOP_K)
    w = jnp.take_along_axis(scores, e_idx, axis=1)
    w = w / jnp.sum(w, axis=-1, keepdims=True)
    gates = jnp.sum(jax.nn.one_hot(e_idx, N_EXPERTS, dtype=jnp.float32) * w[..., None], axis=1)
    gates = gates.astype(t.dtype)
    out = jnp.zeros_like(t)
    for e in range(N_EXPERTS):
        he = jax.nn.silu(t @ w_gate[e]) * (t @ w_up[e])
        out = out + gates[:, e:e + 1] * (he @ w_down[e])
    return out.reshape(B, S, D)


def setup_inputs(seed: int = 0) -> dict:
    key = jax.random.key(seed)
    ks = jax.random.split(key, 24)
    D, F, E = D_MODEL, D_FF_EXPERT, N_EXPERTS
    nrm = lambda k, shape, s: jax.random.normal(k, shape, jnp.float32) * s
    gain = lambda k, shape: 1.0 + 0.02 * jax.random.normal(k, shape, jnp.float32)
    return {
        "x": nrm(ks[0], (BATCH, SEQ, D), 1.0),
        "c": nrm(ks[1], (BATCH, D), 1.0),
        "ada_w": nrm(ks[2], (DEPTH, D, 6 * D), 0.5 * D ** -0.5),
        "ada_b": nrm(ks[3], (DEPTH, 6 * D), 0.01),
        "norm_mix_g": gain(ks[4], (DEPTH, D)),
        "norm_ffn_g": gain(ks[5], (DEPTH, D)),
        "t5_bias": nrm(ks[6], (T5_BUCKETS, A_HEADS), 0.2),
        "a_w_qkv": nrm(ks[7], (N_LAYERS_A, D, 3 * D), D ** -0.5),
        "a_q_gain": gain(ks[8], (N_LAYERS_A, A_HEAD_DIM)),
        "a_k_gain": gain(ks[9], (N_LAYERS_A, A_HEAD_DIM)),
        "a_lambda": nrm(ks[10], (N_LAYERS_A, 4, A_HEAD_DIM), 0.1),
        "a_subln_g": gain(ks[11], (N_LAYERS_A, A_V_DIM)),
        "a_w_o": nrm(ks[12], (N_LAYERS_A, D, D), D ** -0.5),
        "b_w_qkv": nrm(ks[13], (N_LAYERS_B, D, 3 * D), D ** -0.5),
        "b_q_gain": gain(ks[14], (N_LAYERS_B, B_HEAD_DIM)),
        "b_k_gain": gain(ks[15], (N_LAYERS_B, B_HEAD_DIM)),
        "b_rel_bias": nrm(ks[16], (N_LAYERS_B, B_HEADS, 2 * MAX_REL + 1), 0.2),
        "b_w_o": nrm(ks[17], (N_LAYERS_B, D, D), D ** -0.5),
        "router_w": nrm(ks[18], (D, E), D ** -0.5),
        "router_bias": nrm(ks[19], (E,), 0.01),
        "moe_w_gate": nrm(ks[20], (DEPTH, E, D, F), D ** -0.5),
        "moe_w_up": nrm(ks[21], (DEPTH, E, D, F), D ** -0.5),
        "moe_w_down": nrm(ks[22], (DEPTH, E, F, D), F ** -0.5),
    }


def reference(x, c, ada_w, ada_b, norm_mix_g, norm_ffn_g, t5_bias,
              a_w_qkv, a_q_gain, a_k_gain, a_lambda, a_subln_g, a_w_o,
              b_w_qkv, b_q_gain, b_k_gain, b_rel_bias, b_w_o,
              router_w, router_bias, moe_w_gate, moe_w_up, moe_w_down):
    silu_c = jax.nn.silu(c)
    for i in range(DEPTH):
        mod = silu_c @ ada_w[i] + ada_b[i]
        sh1, sc1, g1, sh2, sc2, g2 = [m[:, None, :] for m in jnp.split(mod, 6, axis=-1)]
        h = rms_norm(x, norm_mix_g[i]) * (1.0 + sc1) + sh1
        if i % N_MIXERS == 0:
            j = i // N_MIXERS
            lambda_init = 0.8 - 0.6 * math.exp(-0.3 * i)
            y = diff_attention(h, a_w_qkv[j], a_q_gain[j], a_k_gain[j], a_lambda[j],
                               a_subln_g[j], a_w_o[j], t5_bias, lambda_init)
        else:
            j = i // N_MIXERS
            y = chunked_attention(h, b_w_qkv[j], b_q_gain[j], b_k_gain[j], b_rel_bias[j], b_w_o[j])
        x = x + g1 * y
        h = rms_norm(x, norm_ffn_g[i]) * (1.0 + sc2) + sh2
        x = x + g2 * grouped_moe(h, router_w, router_bias, moe_w_gate[i], moe_w_up[i], moe_w_down[i])
    return x
```

```python
import numpy as np, math
from contextlib import ExitStack
import ml_dtypes
import concourse.bass as bass
import concourse.mybir as mybir
from concourse.bass_utils import run_bass_kernel_spmd

F32 = mybir.dt.float32
BF16 = mybir.dt.bfloat16
I32 = mybir.dt.int32
ALU = mybir.AluOpType
AF = mybir.ActivationFunctionType
AX = mybir.AxisListType
NPBF = ml_dtypes.bfloat16
EPS = 1e-6


class Buf:
    def __init__(self, name):
        self.name = name
        self.w = {}
        self.r = {}
        self.ds = {}


class T:
    def __init__(self, t, buf):
        self.t = t
        self.b = buf

    def __getitem__(self, k):
        return self.t[k]


class Eng:
    def __init__(self, name, h):
        self.name = name
        self.h = h
        self.sem = None
        self.cnt = 0
        self.waited = {}


class KB:
    def __init__(self, nc, es):
        self.nc = nc
        self.es = es
        self.root = es
        self.scopes = []
        self.E = {n: Eng(n, h) for n, h in [('pe', nc.tensor), ('act', nc.scalar), ('dve', nc.vector),
                                            ('pool', nc.gpsimd), ('sp', nc.sync)]}
        self.nsem = 0
        self.ntens = 0
        self.all_bufs = []
        self.free_sems = {'hw': [], 'sw': []}

    def newsem(self, nm):
        self.nsem += 1
        return self.root.enter_context(self.nc.semaphore(f"s_{nm}{self.nsem}"))

    def push_scope(self):
        st = ExitStack()
        self.scopes.append((self.es, len(self.all_bufs)))
        self.es = st
        return st

    def pop_scope(self):
        self.barrier()
        self.es.close()
        self.es, nb = self.scopes.pop()
        for b in self.all_bufs[nb:]:
            for kind, (dsem, dcnt) in b.ds.items():
                self.free_sems[kind].append((dsem, dcnt))
            b.ds = {}
        del self.all_bufs[nb:]

    def collective_allgather(self, in_T, out_T, groups, dummy, partial=False):
        e = self.E['pool']
        rb, wb = self._bufs([in_T]), self._bufs([out_T])
        self._wait(e, self._deps(rb, [], wb) if partial else self._deps(rb, wb, []))
        sem = self.newsem('cc')
        e.h.collective_compute("AllGather", ALU.bypass, replica_groups=groups, ins=[in_T[:]], outs=[out_T[:]]).then_inc(sem)
        e.h.wait_ge(sem, 1)
        if partial:
            self.op('pool', lambda g: g.memset(dummy[:], 0.0), reads=[in_T], writes=[dummy], pwrites=[out_T])
        else:
            self.op('pool', lambda g: g.memset(dummy[:], 0.0), reads=[in_T], writes=[out_T, dummy])

    def igather(self, out, in_, idx_ap, sem_owner, reads=(), writes=(), pwrites=()):
        e = self.E['pool']
        reads, writes, pwrites = self._bufs(reads), self._bufs(writes), self._bufs(pwrites)
        self._wait(e, self._deps(reads, writes, pwrites))
        b = sem_owner.b if isinstance(sem_owner, T) else sem_owner
        ent = self._dsem(b, 'sw')
        inst = e.h.indirect_dma_start(out=out, out_offset=None, in_=in_, in_offset=bass.IndirectOffsetOnAxis(ap=idx_ap, axis=0))
        ent[1] += 16
        inst.then_inc(ent[0], 16)
        self._commit((ent[0], ent[1], 'dma'), reads, writes, pwrites)
        return inst

    def _dsem(self, b, kind):
        if kind not in b.ds:
            if self.free_sems[kind]:
                sem, cnt = self.free_sems[kind].pop()
            else:
                sem, cnt = self.newsem('d' + kind), 0
            b.ds[kind] = [sem, cnt]
        return b.ds[kind]

    def barrier(self):
        toks = {}
        for en, ee in self.E.items():
            if ee.sem is not None and ee.cnt > 0:
                toks[id(ee.sem)] = (ee.sem, ee.cnt, 'bar')
        for b in self.all_bufs:
            for kind, (dsem, dcnt) in b.ds.items():
                if dcnt > 0:
                    toks[id(dsem)] = (dsem, dcnt, 'bar')
        for en, e in self.E.items():
            for k, (sem, val, src) in toks.items():
                if e.sem is not None and k == id(e.sem):
                    continue
                if e.waited.get(k, 0) >= val:
                    continue
                e.h.wait_ge(sem, val)
                e.waited[k] = val

    def sb(self, name, shape, dt):
        self.ntens += 1
        t = self.es.enter_context(self.nc.sbuf_tensor(f"{name}_{self.ntens}", list(shape), dt))
        b = Buf(name)
        self.all_bufs.append(b)
        return T(t, b)

    def ps(self, name, shape, dt):
        self.ntens += 1
        t = self.es.enter_context(self.nc.psum_tensor(f"{name}_{self.ntens}", list(shape), dt))
        b = Buf(name)
        self.all_bufs.append(b)
        return T(t, b)

    def dram(self, name, shape, dt, kind):
        t = self.nc.dram_tensor(name, list(shape), dt, kind=kind)
        b = Buf(name)
        self.all_bufs.append(b)
        return T(t.ap(), b)

    @staticmethod
    def _bufs(lst):
        return [x.b if isinstance(x, T) else x for x in lst]

    def _deps(self, reads, writes, pwrites):
        deps = {}

        def add(d):
            for k, v in d.items():
                if k not in deps or deps[k][1] < v[1]:
                    deps[k] = v
        for b in reads:
            add(b.w)
        for b in writes:
            add(b.w)
            add(b.r)
        for b in pwrites:
            add(b.r)
        return deps

    def _wait(self, e, deps):
        for k, (sem, val, src) in deps.items():
            if src == 'pe' and e.name == 'pe':
                continue
            if e.waited.get(k, 0) >= val:
                continue
            e.h.wait_ge(sem, val)
            e.waited[k] = val

    def _commit(self, tok, reads, writes, pwrites):
        k = id(tok[0])
        for b in writes:
            b.w = {k: tok}
            b.r = {}
        for b in pwrites:
            if k not in b.w or b.w[k][1] < tok[1]:
                b.w[k] = tok
        for b in reads:
            if k not in b.r or b.r[k][1] < tok[1]:
                b.r[k] = tok

    def op(self, en, fn, reads=(), writes=(), pwrites=()):
        e = self.E[en]
        reads, writes, pwrites = self._bufs(reads), self._bufs(writes), self._bufs(pwrites)
        self._wait(e, self._deps(reads, writes, pwrites))
        if e.sem is None or e.cnt >= 32000:
            e.sem = self.newsem(en)
            e.cnt = 0
        inst = fn(e.h)
        e.cnt += 1
        inst.then_inc(e.sem, 1)
        self._commit((e.sem, e.cnt, en), reads, writes, pwrites)
        return inst

    def dma(self, qn, out, in_, sem_owner, reads=(), writes=(), pwrites=(), **kw):
        e = self.E[qn]
        reads, writes, pwrites = self._bufs(reads), self._bufs(writes), self._bufs(pwrites)
        self._wait(e, self._deps(reads, writes, pwrites))
        b = sem_owner.b if isinstance(sem_owner, T) else sem_owner
        ent = self._dsem(b, 'sw' if qn == 'pool' else 'hw')
        inst = e.h.dma_start(out=out, in_=in_, **kw)
        ent[1] += 16
        inst.then_inc(ent[0], 16)
        self._commit((ent[0], ent[1], 'dma'), reads, writes, pwrites)
        return inst

    def finish(self, out_bufs):
        e = self.E['sp']
        deps = {}
        for b in self._bufs(out_bufs):
            for k, v in b.w.items():
                deps[k] = v
        self._wait(e, deps)
        for en, ee in self.E.items():
            if ee.sem is not None and ee.cnt > 0:
                k = id(ee.sem)
                if e.waited.get(k, 0) < ee.cnt:
                    e.h.wait_ge(ee.sem, ee.cnt)
                    e.waited[k] = ee.cnt
        for b in self.all_bufs:
            for kind, (dsem, dcnt) in b.ds.items():
                k = id(dsem)
                if dcnt > 0 and e.waited.get(k, 0) < dcnt:
                    e.h.wait_ge(dsem, dcnt)
                    e.waited[k] = dcnt


S = 8192; D = 1024; NB = 2
LAMBDA_INIT0 = 0.8 - 0.6 * math.exp(-0.3 * 0)
NEAR_DELTAS = [-640 + 128 * i for i in range(9)]


def t5_bucket_np(rel):
    nb = 16
    ret = np.where(rel > 0, nb, 0)
    n = np.abs(rel)
    max_exact = 8
    nf = np.maximum(n, 1).astype(np.float32)
    large = max_exact + (np.log(nf / np.float32(max_exact)) / np.float32(math.log(1024 / max_exact))
                         * np.float32(nb - max_exact)).astype(np.int32)
    large = np.minimum(large, nb - 1)
    return ret + np.where(n < max_exact, n, large)


def host_inputs_A(inp, h):
    f32 = np.float32
    d = {}
    d["x"] = np.ascontiguousarray(inp["x"].reshape(NB * S, D))
    c = inp["c"]
    d["cT"] = np.ascontiguousarray(c.reshape(NB, 8, 128).transpose(2, 1, 0))
    d["adaw"] = np.ascontiguousarray(inp["ada_w"][0][:, 0:2048])
    d["adabT"] = np.ascontiguousarray(inp["ada_b"][0][0:2048].reshape(16, 128).T)
    d["gT"] = np.ascontiguousarray(inp["norm_mix_g"][0].reshape(8, 128).T)
    w = inp["a_w_qkv"][0]
    d["w"] = np.ascontiguousarray(np.concatenate([w[:, h * 128:(h + 1) * 128], w[:, 1024 + h * 128:1024 + (h + 1) * 128],
                                                  w[:, 2048 + h * 128:2048 + (h + 1) * 128]], axis=1))
    qg = inp["a_q_gain"][0]; kg = inp["a_k_gain"][0]
    d["gain"] = np.ascontiguousarray(np.broadcast_to(np.concatenate([qg, qg, kg, kg])[None, :], (128, 256))).astype(f32)
    d["lam"] = np.ascontiguousarray(np.broadcast_to(inp["a_lambda"][0].reshape(1, 256), (128, 256))).astype(f32)
    d["subg"] = np.ascontiguousarray(np.broadcast_to(inp["a_subln_g"][0][None, :], (128, 128))).astype(f32)
    kk = np.arange(128)[:, None]; qq = np.arange(512)[None, :]
    t5 = inp["t5_bias"]
    bt = np.stack([t5[t5_bucket_np(dl + kk - qq), h] for dl in NEAR_DELTAS], axis=1)
    d["biasT"] = np.ascontiguousarray(bt).astype(f32)
    d["c15"] = np.full((128, 1), t5[15, h], f32)
    mk = np.stack([np.where(((dl + kk) // 64) <= (qq // 64), 0.0, -30000.0) for dl in (0, 128, 256, 384)], axis=1)
    d["maskT"] = np.ascontiguousarray(mk).astype(f32)
    d["idb"] = np.eye(128).astype(NPBF)
    return d


def build_A(nc, nb_run=NB, nqg_run=16):
    es = ExitStack()
    kb = KB(nc, es)
    x = kb.dram("x", [NB * S, D], F32, "ExternalInput")
    cT = kb.dram("cT", [128, 8, 2], F32, "ExternalInput")
    adaw = kb.dram("adaw", [1024, 2048], F32, "ExternalInput")
    adabT = kb.dram("adabT", [128, 16], F32, "ExternalInput")
    gT = kb.dram("gT", [128, 8], F32, "ExternalInput")
    w = kb.dram("w", [1024, 384], F32, "ExternalInput")
    gain = kb.dram("gain", [128, 256], F32, "ExternalInput")
    lam = kb.dram("lam", [128, 256], F32, "ExternalInput")
    subg = kb.dram("subg", [128, 128], F32, "ExternalInput")
    biasT = kb.dram("biasT", [128, 9, 512], F32, "ExternalInput")
    c15 = kb.dram("c15", [128, 1], F32, "ExternalInput")
    maskT = kb.dram("maskT", [128, 4, 512], F32, "ExternalInput")
    idb = kb.dram("idb", [128, 128], BF16, "ExternalInput")
    oT = kb.dram("oT", [128, NB * S], BF16, "ExternalOutput")
    with es:
        emit_A(kb, x, cT, adaw, adabT, gT, w, gain, lam, subg, biasT, c15, maskT, idb, oT, nb_run, nqg_run)
        kb.finish([oT])
    return nc


def emit_A(kb, x, cT, adaw, adabT, gT, w, gain, lam, subg, biasT, c15, maskT, idb, oT, nb_run=NB, nqg_run=16):
    ident = kb.sb("ident", [128, 128], BF16)
    kb.dma('sp', ident[:], idb[:], ident, reads=[idb], writes=[ident])
    epsb = kb.sb("epsb", [128, 1], F32)
    kb.op('dve', lambda e: e.memset(epsb[:], EPS), writes=[epsb])
    cT_sb = kb.sb("cT_sb", [128, 8, 2], F32)
    kb.dma('sp', cT_sb[:], cT[:], cT_sb, reads=[cT], writes=[cT_sb])
    siluT = kb.sb("siluT", [128, 8, 2], F32)
    kb.op('act', lambda e: e.activation(out=siluT[:], in_=cT_sb[:], func=AF.Silu), reads=[cT_sb], writes=[siluT])
    adabT_sb = kb.sb("adabT_sb", [128, 16], F32)
    kb.dma('sp', adabT_sb[:], adabT[:], adabT_sb, reads=[adabT], writes=[adabT_sb])
    gT_sb = kb.sb("gT_sb", [128, 8], F32)
    kb.dma('sp', gT_sb[:], gT[:], gT_sb, reads=[gT], writes=[gT_sb])
    w_sb = kb.sb("w_sb", [128, 8, 384], F32)
    kb.dma('sp', w_sb[:], w.t.rearrange("(kc p) n -> p kc n", p=128), w_sb, reads=[w], writes=[w_sb])
    gain_sb = kb.sb("gain_sb", [128, 256], F32)
    kb.dma('sp', gain_sb[:], gain[:], gain_sb, reads=[gain], writes=[gain_sb])
    kb.op('dve', lambda e: e.tensor_scalar(out=gain_sb[:, 0:128], in0=gain_sb[:, 0:128], scalar1=0.125, scalar2=None, op0=ALU.mult),
          reads=[gain_sb], writes=[gain_sb])
    lam_sb = kb.sb("lam_sb", [128, 256], F32)
    kb.dma('sp', lam_sb[:], lam[:], lam_sb, reads=[lam], writes=[lam_sb])
    subg_sb = kb.sb("subg_sb", [128, 128], F32)
    kb.dma('sp', subg_sb[:], subg[:], subg_sb, reads=[subg], writes=[subg_sb])
    kb.op('dve', lambda e: e.tensor_scalar(out=subg_sb[:], in0=subg_sb[:], scalar1=1.0 - LAMBDA_INIT0, scalar2=None, op0=ALU.mult),
          reads=[subg_sb], writes=[subg_sb])
    bias_sb = kb.sb("bias_sb", [128, 9, 512], F32)
    kb.dma('sp', bias_sb[:], biasT[:], bias_sb, reads=[biasT], writes=[bias_sb])
    mask_sb = kb.sb("mask_sb", [128, 4, 512], F32)
    kb.dma('sp', mask_sb[:], maskT[:], mask_sb, reads=[maskT], writes=[mask_sb])
    kb.op('dve', lambda e: e.tensor_tensor(out=bias_sb[:, 5:9, :], in0=bias_sb[:, 5:9, :], in1=mask_sb[:], op=ALU.add),
          reads=[bias_sb, mask_sb], writes=[bias_sb])
    c15_sb = kb.sb("c15_sb", [128, 1], F32)
    kb.dma('sp', c15_sb[:], c15[:], c15_sb, reads=[c15], writes=[c15_sb])

    PB = [kb.ps(f"pb{i}", [128, 512], F32) for i in range(8)]

    lprod = kb.sb("lprod", [128, 2, 64], F32)
    kb.op('dve', lambda e: e.tensor_tensor(out=lprod[:, 0, :], in0=lam_sb[:, 0:64], in1=lam_sb[:, 64:128], op=ALU.mult), reads=[lam_sb], writes=[lprod])
    kb.op('dve', lambda e: e.tensor_tensor(out=lprod[:, 1, :], in0=lam_sb[:, 128:192], in1=lam_sb[:, 192:256], op=ALU.mult), reads=[lam_sb, lprod], writes=[lprod])
    lsum = kb.sb("lsum", [128, 2], F32)
    kb.op('dve', lambda e: e.tensor_reduce(out=lsum[:], in_=lprod[:], axis=AX.X, op=ALU.add), reads=[lprod], writes=[lsum])
    lexp = kb.sb("lexp", [128, 2], F32)
    kb.op('act', lambda e: e.activation(out=lexp[:], in_=lsum[:], func=AF.Exp), reads=[lsum], writes=[lexp])
    neglam = kb.sb("neglam", [128, 1], F32)
    kb.op('dve', lambda e: e.tensor_tensor(out=neglam[:], in0=lexp[:, 1:2], in1=lexp[:, 0:1], op=ALU.subtract), reads=[lexp], writes=[neglam])
    kb.op('dve', lambda e: e.tensor_scalar(out=neglam[:], in0=neglam[:], scalar1=-LAMBDA_INIT0, scalar2=None, op0=ALU.add), reads=[neglam], writes=[neglam])

    adaw_sb = kb.sb("adaw_sb", [128, 8, 512], F32)
    modT = kb.sb("modT", [128, 16, 2], F32)
    pm = PB[0]
    for g in range(4):
        kb.dma('sp', adaw_sb[:], adaw.t[:, g * 512:(g + 1) * 512].rearrange("(kc p) n -> p kc n", p=128), adaw_sb,
               reads=[adaw], writes=[adaw_sb])
        for jj in range(4):
            j = g * 4 + jj
            for kc in range(8):
                kb.op('pe', lambda e, j=j, jj=jj, kc=kc: e.matmul(pm[:, j * 2:(j + 1) * 2], lhsT=adaw_sb[:, kc, jj * 128:(jj + 1) * 128],
                                                                rhs=siluT[:, kc, :], start=(kc == 0), stop=(kc == 7)),
                      reads=[adaw_sb, siluT], pwrites=[pm])
    kb.op('dve', lambda e: e.tensor_tensor(out=modT[:], in0=pm[:, 0:32].rearrange("p (j b) -> p j b", b=2),
                                           in1=adabT_sb[:].unsqueeze(2).to_broadcast([128, 16, 2]), op=ALU.add),
          reads=[pm, adabT_sb], writes=[modT])
    Ssc = kb.sb("Ssc", [128, 8, 2], F32)
    kb.op('dve', lambda e: e.scalar_tensor_tensor(out=Ssc[:], in0=modT[:, 8:16, :], scalar=1.0, in1=gT_sb[:].unsqueeze(2).to_broadcast([128, 8, 2]),
                                                  op0=ALU.add, op1=ALU.mult), reads=[modT, gT_sb], writes=[Ssc])
    Wb = [kb.sb(f"Wb{b}", [128, 8, 384], BF16) for b in range(NB)]
    for b in range(NB):
        for kc in range(8):
            kb.op('dve', lambda e, b=b, kc=kc: e.tensor_scalar(out=Wb[b][:, kc, :], in0=w_sb[:, kc, :], scalar1=Ssc[:, kc, b:b + 1], scalar2=None, op0=ALU.mult),
                  reads=[w_sb, Ssc], pwrites=[Wb[b]])
    SHrep = kb.sb("SHrep", [128, 8, 128], F32)
    biasbc = [kb.sb(f"biasbc{b}", [128, 384], F32) for b in range(NB)]
    for b in range(NB):
        kb.op('dve', lambda e, b=b: e.tensor_copy(out=SHrep[:], in_=modT[:, 0:8, b:b + 1].to_broadcast([128, 8, 128])), reads=[modT], writes=[SHrep])
        pbias = PB[1]
        for kc in range(8):
            kb.op('pe', lambda e, kc=kc: e.matmul(pbias[:, 0:384], lhsT=SHrep[:, kc, :], rhs=w_sb[:, kc, :], start=(kc == 0), stop=(kc == 7)),
                  reads=[SHrep, w_sb], pwrites=[pbias])
        kb.op('dve', lambda e, b=b: e.tensor_copy(out=biasbc[b][:], in_=pbias[:, 0:384]), reads=[pbias], writes=[biasbc[b]])

    qT = kb.sb("qT", [128, S], BF16)
    kT = kb.sb("kT", [128, S], BF16)
    Vb = kb.sb("Vb", [128, 64, 129], BF16)
    kb.op('pool', lambda e: e.memset(Vb[:, :, 128:129], 1.0), pwrites=[Vb])
    NX = 3
    xbuf = [kb.sb(f"xbuf{i}", [128, 1024], F32) for i in range(NX)]
    junk = kb.sb("junk", [128, 1024], BF16)
    ssb = [kb.sb(f"ss{i}", [128, 1], F32) for i in range(2)]
    rstd = [kb.sb(f"rstd{i}", [128, 1], F32) for i in range(2)]
    xs = [kb.sb(f"xs{i}", [128, 1024], BF16) for i in range(2)]
    xT = [kb.sb(f"xT{i}", [128, 1024], BF16) for i in range(2)]
    qkv = [kb.sb(f"qkv{i}", [128, 384], F32) for i in range(2)]
    sq = [kb.sb(f"sq{i}", [128, 256], F32) for i in range(2)]
    ss4 = [kb.sb(f"ss4{i}", [128, 4], F32) for i in range(2)]
    qkt = [kb.sb(f"qkt{i}", [128, 256], F32) for i in range(2)]
    qkn = [kb.sb(f"qkn{i}", [128, 256], BF16) for i in range(2)]
    Pb = [kb.sb(f"P{i}", [128, 2, 512], BF16) for i in range(3)]
    ostage = [kb.sb(f"ost{i}", [128, 512], BF16) for i in range(2)]
    rr = [kb.sb(f"rr{i}", [128, 3], F32) for i in range(2)]
    t1 = [kb.sb(f"t1{i}", [128, 128], F32) for i in range(2)]
    ob = [kb.sb(f"ob{i}", [128, 128], F32) for i in range(2)]
    oss = [kb.sb(f"oss{i}", [128, 1], F32) for i in range(2)]
    on = [kb.sb(f"on{i}", [128, 128], BF16) for i in range(2)]

    def acc(m, j):
        a = m * 4 + j
        return PB[4 + a // 3], (a % 3) * 129, a

    for b in range(nb_run):
        for t in range(64):
            i2 = t % 2
            xt = xbuf[t % NX]
            r0 = b * S + t * 128
            kb.dma('sp', xt[:], x.t[r0:r0 + 128, :], xt, reads=[x], writes=[xt])
            kb.op('act', lambda e, xt=xt, i2=i2: e.activation(out=junk[:], in_=xt[:], func=AF.Square, accum_out=ssb[i2][:]), reads=[xt], writes=[junk, ssb[i2]])
            kb.op('act', lambda e, i2=i2: e.activation(out=rstd[i2][:], in_=ssb[i2][:], func=AF.Sqrt, scale=1.0 / 1024, bias=epsb[:, 0:1]),
                  reads=[ssb[i2], epsb], writes=[rstd[i2]])
            kb.op('dve', lambda e, i2=i2: e.reciprocal(out=rstd[i2][:], in_=rstd[i2][:]), reads=[rstd[i2]], writes=[rstd[i2]])
            kb.op('dve', lambda e, xt=xt, i2=i2: e.tensor_scalar(out=xs[i2][:], in0=xt[:], scalar1=rstd[i2][:, 0:1], scalar2=None, op0=ALU.mult),
                  reads=[xt, rstd[i2]], writes=[xs[i2]])
            pT = PB[i2]
            pTv = pT[:].bitcast(BF16)
            for kc in range(8):
                kb.op('pe', lambda e, kc=kc, i2=i2, pTv=pTv: e.transpose(out=pTv[:, kc * 128:(kc + 1) * 128], in_=xs[i2][:, kc * 128:(kc + 1) * 128], identity=ident[:]),
                      reads=[xs[i2], ident], pwrites=[pT])
            kb.op('act', lambda e, i2=i2, pTv=pTv: e.copy(out=xT[i2][:], in_=pTv[:, 0:1024]), reads=[pT], writes=[xT[i2]])
            pq = PB[2 + i2]
            for kc in range(8):
                kb.op('pe', lambda e, kc=kc, i2=i2, pq=pq: e.matmul(pq[:, 0:384], lhsT=xT[i2][:, kc * 128:(kc + 1) * 128], rhs=Wb[b][:, kc, :], start=(kc == 0), stop=(kc == 7)),
                      reads=[xT[i2], Wb[b]], pwrites=[pq])
            kb.op('dve', lambda e, i2=i2, pq=pq: e.tensor_tensor(out=qkv[i2][:], in0=pq[:, 0:384], in1=biasbc[b][:], op=ALU.add), reads=[pq, biasbc[b]], writes=[qkv[i2]])
            kb.op('act', lambda e, i2=i2: e.activation(out=sq[i2][:], in_=qkv[i2][:, 0:256], func=AF.Square), reads=[qkv[i2]], writes=[sq[i2]])
            kb.op('dve', lambda e, i2=i2: e.tensor_reduce(out=ss4[i2][:], in_=sq[i2][:].rearrange("p (g d) -> p g d", d=64), axis=AX.X, op=ALU.add),
                  reads=[sq[i2]], writes=[ss4[i2]])
            kb.op('act', lambda e, i2=i2: e.activation(out=ss4[i2][:], in_=ss4[i2][:], func=AF.Sqrt, scale=1.0 / 64, bias=epsb[:, 0:1]),
                  reads=[ss4[i2], epsb], writes=[ss4[i2]])
            kb.op('dve', lambda e, i2=i2: e.reciprocal(out=ss4[i2][:], in_=ss4[i2][:]), reads=[ss4[i2]], writes=[ss4[i2]])
            kb.op('dve', lambda e, i2=i2: e.tensor_tensor(out=qkt[i2][:].rearrange("p (g d) -> p g d", d=64), in0=qkv[i2][:, 0:256].rearrange("p (g d) -> p g d", d=64),
                                                          in1=ss4[i2][:].unsqueeze(2).to_broadcast([128, 4, 64]), op=ALU.mult),
                  reads=[qkv[i2], ss4[i2]], writes=[qkt[i2]])
            kb.op('dve', lambda e, i2=i2: e.tensor_tensor(out=qkn[i2][:], in0=qkt[i2][:], in1=gain_sb[:], op=ALU.mult), reads=[qkt[i2], gain_sb], writes=[qkn[i2]])
            kb.op('act', lambda e, i2=i2, t=t: e.copy(out=Vb[:, t, 0:128], in_=qkv[i2][:, 256:384]), reads=[qkv[i2]], pwrites=[Vb])
            pqk = PB[4 + i2]
            pqkv = pqk[:].bitcast(BF16)
            kb.op('pe', lambda e, i2=i2, pqkv=pqkv: e.transpose(out=pqkv[:, 0:128], in_=qkn[i2][:, 0:128], identity=ident[:]), reads=[qkn[i2], ident], pwrites=[pqk])
            kb.op('pe', lambda e, i2=i2, pqkv=pqkv: e.transpose(out=pqkv[:, 128:256], in_=qkn[i2][:, 128:256], identity=ident[:]), reads=[qkn[i2], ident], pwrites=[pqk])
            kb.op('dve', lambda e, t=t, pqkv=pqkv: e.tensor_copy(out=qT[:, t * 128:(t + 1) * 128], in_=pqkv[:, 0:128]), reads=[pqk], pwrites=[qT])
            kb.op('dve', lambda e, t=t, pqkv=pqkv: e.tensor_copy(out=kT[:, t * 128:(t + 1) * 128], in_=pqkv[:, 128:256]), reads=[pqk], pwrites=[kT])

        for qg in range(nqg_run):
            nkt = 4 * qg + 4
            qbase = qg * 512

            def emit_qk(kt):
                sl = kt % 2
                d = kt * 128 - qbase
                q0 = max(d, 0)
                for m in range(2):
                    Sp = PB[sl * 2 + m]
                    kb.op('pe', lambda e, m=m, Sp=Sp, q0=q0, kt=kt: e.matmul(Sp[:, q0:512], lhsT=kT[m * 64:(m + 1) * 64, kt * 128:(kt + 1) * 128],
                                                                      rhs=qT[m * 64:(m + 1) * 64, qbase + q0:qbase + 512], start=True, stop=True),
                          reads=[kT, qT], writes=[Sp])

            emit_qk(0)
            for kt in range(nkt):
                if kt + 1 < nkt:
                    emit_qk(kt + 1)
                sl = kt % 2
                d = kt * 128 - qbase
                q0 = max(d, 0)
                P = Pb[kt % 3]
                for m in range(2):
                    Sp = PB[sl * 2 + m]
                    if d >= -640:
                        bi = (d + 640) // 128
                        kb.op('dve', lambda e, Sp=Sp, bi=bi, q0=q0: e.tensor_tensor(out=Sp[:, q0:512], in0=Sp[:, q0:512], in1=bias_sb[:, bi, q0:512], op=ALU.add),
                              reads=[Sp, bias_sb], writes=[Sp])
                        kb.op('act', lambda e, Sp=Sp, P=P, m=m, q0=q0: e.activation(out=P[:, m, q0:512], in_=Sp[:, q0:512], func=AF.Exp),
                              reads=[Sp], pwrites=[P])
                    else:
                        kb.op('act', lambda e, Sp=Sp, P=P, m=m, q0=q0: e.activation(out=P[:, m, q0:512], in_=Sp[:, q0:512], func=AF.Exp, bias=c15_sb[:, 0:1]),
                              reads=[Sp, c15_sb], pwrites=[P])
                for m in range(2):
                    for j in range(q0 // 128, 4):
                        bank, off, a = acc(m, j)
                        kb.op('pe', lambda e, bank=bank, off=off, a=a, P=P, m=m, j=j, kt=kt: e.matmul(
                            bank[:, off:off + 129], lhsT=P[:, m, j * 128:(j + 1) * 128], rhs=Vb[:, kt, :],
                            start=(kt == 0 and a % 3 == 0), stop=(kt == 4 * qg + j), skip_group_check=True),
                            reads=[P, Vb], pwrites=[bank])
            ost = ostage[qg % 2]
            for j in range(4):
                i2 = j % 2
                b1, o1, _ = acc(0, j)
                b2, o2, _ = acc(1, j)
                kb.op('dve', lambda e, i2=i2, b1=b1, o1=o1: e.reciprocal(out=rr[i2][:, 0:1], in_=b1[:, o1 + 128:o1 + 129]), reads=[b1], writes=[rr[i2]])
                kb.op('dve', lambda e, i2=i2, b2=b2, o2=o2: e.reciprocal(out=rr[i2][:, 1:2], in_=b2[:, o2 + 128:o2 + 129]), reads=[b2, rr[i2]], writes=[rr[i2]])
                kb.op('dve', lambda e, i2=i2: e.tensor_tensor(out=rr[i2][:, 2:3], in0=rr[i2][:, 1:2], in1=neglam[:], op=ALU.mult), reads=[rr[i2], neglam], writes=[rr[i2]])
                kb.op('dve', lambda e, i2=i2, b1=b1, o1=o1: e.tensor_scalar(out=t1[i2][:], in0=b1[:, o1:o1 + 128], scalar1=rr[i2][:, 0:1], scalar2=None, op0=ALU.mult),
                      reads=[b1, rr[i2]], writes=[t1[i2]])
                kb.op('dve', lambda e, i2=i2, b2=b2, o2=o2: e.scalar_tensor_tensor(out=ob[i2][:], in0=b2[:, o2:o2 + 128], scalar=rr[i2][:, 2:3], in1=t1[i2][:],
                                                                           op0=ALU.mult, op1=ALU.add), reads=[b2, rr[i2], t1[i2]], writes=[ob[i2]])
                kb.op('act', lambda e, i2=i2: e.activation(out=junk[:, 0:128], in_=ob[i2][:], func=AF.Square, accum_out=oss[i2][:]), reads=[ob[i2]], writes=[junk, oss[i2]])
                kb.op('act', lambda e, i2=i2: e.activation(out=oss[i2][:], in_=oss[i2][:], func=AF.Sqrt, scale=1.0 / 128, bias=epsb[:, 0:1]), reads=[oss[i2], epsb], writes=[oss[i2]])
                kb.op('dve', lambda e, i2=i2: e.reciprocal(out=oss[i2][:], in_=oss[i2][:]), reads=[oss[i2]], writes=[oss[i2]])
                kb.op('dve', lambda e, i2=i2: e.scalar_tensor_tensor(out=on[i2][:], in0=ob[i2][:], scalar=oss[i2][:, 0:1], in1=subg_sb[:], op0=ALU.mult, op1=ALU.mult),
                      reads=[ob[i2], oss[i2], subg_sb], writes=[on[i2]])
                pt = PB[7]
                ptv = pt[:].bitcast(BF16)
                kb.op('pe', lambda e, i2=i2, ptv=ptv, j=j: e.transpose(out=ptv[:, j * 128:(j + 1) * 128], in_=on[i2][:], identity=ident[:]), reads=[on[i2], ident], pwrites=[pt])
            kb.op('dve', lambda e, ost=ost: e.tensor_copy(out=ost[:], in_=PB[7][:].bitcast(BF16)[:, 0:512]), reads=[PB[7]], writes=[ost])
            c0 = b * S + qbase
            kb.dma('sp', oT.t[:, c0:c0 + 512], ost[:], ost, reads=[ost], pwrites=[oT])


def emit_A2(kb, cx, x, cT, adaw, adabT, gT, w2, gain, lam, subg, biasT2, c15_2, maskT, ag_ins, after_chunk=None, xTs=None, nb_run=2, nqg_run=16):
    ident = cx.identb
    epsb = cx.epsb
    cT_sb = kb.sb("cT_sb", [128, 8, 2], F32)
    kb.dma('sp', cT_sb[:], cT[:], cT_sb, reads=[cT], writes=[cT_sb])
    siluT = kb.sb("siluT", [128, 8, 2], F32)
    kb.op('act', lambda e: e.activation(out=siluT[:], in_=cT_sb[:], func=AF.Silu), reads=[cT_sb], writes=[siluT])
    adabT_sb = kb.sb("adabT_sb", [128, 16], F32)
    kb.dma('sp', adabT_sb[:], adabT[:], adabT_sb, reads=[adabT], writes=[adabT_sb])
    gT_sb = kb.sb("gT_sb", [128, 8], F32)
    kb.dma('sp', gT_sb[:], gT[:], gT_sb, reads=[gT], writes=[gT_sb])
    w_sb = kb.sb("w_sb", [128, 8, 384], F32)
    gain_sb = kb.sb("gain_sb", [128, 256], F32)
    kb.dma('sp', gain_sb[:], gain[:], gain_sb, reads=[gain], writes=[gain_sb])
    kb.op('dve', lambda e: e.tensor_scalar(out=gain_sb[:, 0:128], in0=gain_sb[:, 0:128], scalar1=0.125, scalar2=None, op0=ALU.mult),
          reads=[gain_sb], writes=[gain_sb])
    lam_sb = kb.sb("lam_sb", [128, 256], F32)
    kb.dma('sp', lam_sb[:], lam[:], lam_sb, reads=[lam], writes=[lam_sb])
    subg_sb = kb.sb("subg_sb", [128, 128], F32)
    kb.dma('sp', subg_sb[:], subg[:], subg_sb, reads=[subg], writes=[subg_sb])
    kb.op('dve', lambda e: e.tensor_scalar(out=subg_sb[:], in0=subg_sb[:], scalar1=1.0 - LAMBDA_INIT0, scalar2=None, op0=ALU.mult),
          reads=[subg_sb], writes=[subg_sb])
    bias_sb = kb.sb("bias_sb", [128, 9, 512], F32)
    mask_sb = kb.sb("mask_sb", [128, 4, 512], F32)
    kb.dma('sp', mask_sb[:], maskT[:], mask_sb, reads=[maskT], writes=[mask_sb])
    c15_sb = kb.sb("c15_sb", [128, 2], F32)
    kb.dma('sp', c15_sb[:], c15_2[:], c15_sb, reads=[c15_2], writes=[c15_sb])
    PB = cx.PB

    lprod = kb.sb("lprod", [128, 2, 64], F32)
    kb.op('dve', lambda e: e.tensor_tensor(out=lprod[:, 0, :], in0=lam_sb[:, 0:64], in1=lam_sb[:, 64:128], op=ALU.mult), reads=[lam_sb], writes=[lprod])
    kb.op('dve', lambda e: e.tensor_tensor(out=lprod[:, 1, :], in0=lam_sb[:, 128:192], in1=lam_sb[:, 192:256], op=ALU.mult), reads=[lam_sb, lprod], writes=[lprod])
    lsum = kb.sb("lsum", [128, 2], F32)
    kb.op('dve', lambda e: e.tensor_reduce(out=lsum[:], in_=lprod[:], axis=AX.X, op=ALU.add), reads=[lprod], writes=[lsum])
    lexp = kb.sb("lexp", [128, 2], F32)
    kb.op('act', lambda e: e.activation(out=lexp[:], in_=lsum[:], func=AF.Exp), reads=[lsum], writes=[lexp])
    neglam = kb.sb("neglam", [128, 1], F32)
    kb.op('dve', lambda e: e.tensor_tensor(out=neglam[:], in0=lexp[:, 1:2], in1=lexp[:, 0:1], op=ALU.subtract), reads=[lexp], writes=[neglam])
    kb.op('dve', lambda e: e.tensor_scalar(out=neglam[:], in0=neglam[:], scalar1=-LAMBDA_INIT0, scalar2=None, op0=ALU.add), reads=[neglam], writes=[neglam])

    adaw_sb = cx.adaw_sb
    modT = kb.sb("modT", [128, 16, 2], F32)
    pm = PB[0]
    for g in range(4):
        kb.dma('sp', adaw_sb[:], adaw.t[:, g * 512:(g + 1) * 512].rearrange("(kc p) n -> p kc n", p=128), adaw_sb,
               reads=[adaw], writes=[adaw_sb])
        for jj in range(4):
            j = g * 4 + jj
            for kc in range(8):
                kb.op('pe', lambda e, j=j, jj=jj, kc=kc: e.matmul(pm[:, j * 2:(j + 1) * 2], lhsT=adaw_sb[:, kc, jj * 128:(jj + 1) * 128],
                                                                rhs=siluT[:, kc, :], start=(kc == 0), stop=(kc == 7)),
                      reads=[adaw_sb, siluT], pwrites=[pm])
    kb.op('dve', lambda e: e.tensor_tensor(out=modT[:], in0=pm[:, 0:32].rearrange("p (j b) -> p j b", b=2),
                                           in1=adabT_sb[:].unsqueeze(2).to_broadcast([128, 16, 2]), op=ALU.add),
          reads=[pm, adabT_sb], writes=[modT])
    Ssc = kb.sb("Ssc", [128, 8, 2], F32)
    kb.op('dve', lambda e: e.scalar_tensor_tensor(out=Ssc[:], in0=modT[:, 8:16, :], scalar=1.0, in1=gT_sb[:].unsqueeze(2).to_broadcast([128, 8, 2]),
                                                  op0=ALU.add, op1=ALU.mult), reads=[modT, gT_sb], writes=[Ssc])
    Wb0 = kb.sb("Wb0", [128, 8, 384], BF16)
    SHrep = kb.sb("SHrep", [128, 8, 128], F32)
    biasbc0 = kb.sb("biasbc0", [128, 384], F32)

    qT = kb.sb("qT", [128, S], BF16)
    kT = kb.sb("kT", [128, S], BF16)
    Vb = kb.sb("Vb", [128, 64, 129], BF16)
    kb.op('pool', lambda e: e.memset(Vb[:, :, 128:129], 1.0), pwrites=[Vb])
    NX = 4
    xbuf = [kb.sb(f"xbuf{i}", [128, 1024], F32) for i in range(NX)]
    junk = cx.junk
    ssb = [kb.sb(f"ss{i}", [128, 1], F32) for i in range(2)]
    rstd = [kb.sb(f"rstd{i}", [128, 1], F32) for i in range(2)]
    xs = [kb.sb(f"xs{i}", [128, 1024], BF16) for i in range(2)]
    xT = [kb.sb(f"xT{i}", [128, 1024], BF16) for i in range(2)]
    qkv = [kb.sb(f"qkv{i}", [128, 384], F32) for i in range(4)]
    sq = [kb.sb(f"sq{i}", [128, 256], F32) for i in range(4)]
    ss4 = [kb.sb(f"ss4{i}", [128, 4], F32) for i in range(4)]
    qkt = [kb.sb(f"qkt{i}", [128, 256], F32) for i in range(4)]
    qkn = [kb.sb(f"qkn{i}", [128, 256], BF16) for i in range(4)]
    Pb = [kb.sb(f"P{i}", [128, 2, 512], BF16) for i in range(4)]
    ostage = [kb.sb(f"ost{i}", [128, 512], BF16) for i in range(2)]
    rr = [kb.sb(f"rr{i}", [128, 3], F32) for i in range(2)]
    t1 = [kb.sb(f"t1{i}", [128, 128], F32) for i in range(2)]
    ob = [kb.sb(f"ob{i}", [128, 128], F32) for i in range(2)]
    oss = [kb.sb(f"oss{i}", [128, 1], F32) for i in range(2)]
    on = [kb.sb(f"on{i}", [128, 128], BF16) for i in range(2)]

    PQB = [2, 3, 6, 7]

    def acc(m, j):
        a = m * 4 + j
        return PB[4 + a // 3], (a % 3) * 129, a

    for b in range(nb_run):
        kb.dma('sp', w_sb[:], w2.t[b].rearrange("(kc p) n -> p kc n", p=128), w_sb, reads=[w2], writes=[w_sb])
        for kc in range(8):
            kb.op('dve', lambda e, b=b, kc=kc: e.tensor_scalar(out=Wb0[:, kc, :], in0=w_sb[:, kc, :], scalar1=Ssc[:, kc, b:b + 1], scalar2=None, op0=ALU.mult),
                  reads=[w_sb, Ssc], writes=([Wb0] if kc == 0 else []), pwrites=([] if kc == 0 else [Wb0]))
        kb.op('dve', lambda e, b=b: e.tensor_copy(out=SHrep[:], in_=modT[:, 0:8, b:b + 1].to_broadcast([128, 8, 128])), reads=[modT], writes=[SHrep])
        pbias = PB[1]
        for kc in range(8):
            kb.op('pe', lambda e, kc=kc: e.matmul(pbias[:, 0:384], lhsT=SHrep[:, kc, :], rhs=w_sb[:, kc, :], start=(kc == 0), stop=(kc == 7)),
                  reads=[SHrep, w_sb], pwrites=[pbias])
        kb.op('dve', lambda e: e.tensor_copy(out=biasbc0[:], in_=pbias[:, 0:384]), reads=[pbias], writes=[biasbc0])
        kb.dma('sp', bias_sb[:], biasT2.t[:, b], bias_sb, reads=[biasT2], writes=[bias_sb])
        kb.op('dve', lambda e: e.tensor_tensor(out=bias_sb[:, 5:9, :], in0=bias_sb[:, 5:9, :], in1=mask_sb[:], op=ALU.add),
              reads=[bias_sb, mask_sb], writes=[bias_sb])
        kb.op('dve', lambda e, b=b: e.tensor_scalar(out=bias_sb[:], in0=bias_sb[:], scalar1=c15_sb[:, b:b + 1], scalar2=None, op0=ALU.subtract),
              reads=[bias_sb, c15_sb], writes=[bias_sb])
        def p1(t):
            i2 = t % 2
            if xTs is not None and b > 0:
                kb.dma('sp', xT[i2][:], xTs.t[t], xT[i2], reads=[xTs], writes=[xT[i2]])
            else:
                xt = xbuf[t % NX]
                r0 = t * 128
                kb.dma('sp', xt[:], x.t[r0:r0 + 128, :], xt, reads=[x], writes=[xt])
                kb.op('act', lambda e, xt=xt, i2=i2: e.activation(out=junk[:], in_=xt[:], func=AF.Square, accum_out=ssb[i2][:]), reads=[xt], writes=[junk, ssb[i2]])
                kb.op('act', lambda e, i2=i2: e.activation(out=rstd[i2][:], in_=ssb[i2][:], func=AF.Sqrt, scale=1.0 / 1024, bias=epsb[:, 0:1]),
                      reads=[ssb[i2], epsb], writes=[rstd[i2]])
                kb.op('dve', lambda e, i2=i2: e.reciprocal(out=rstd[i2][:], in_=rstd[i2][:]), reads=[rstd[i2]], writes=[rstd[i2]])
                kb.op('dve', lambda e, xt=xt, i2=i2: e.tensor_scalar(out=xs[i2][:], in0=xt[:], scalar1=rstd[i2][:, 0:1], scalar2=None, op0=ALU.mult),
                      reads=[xt, rstd[i2]], writes=[xs[i2]])
                pT = PB[i2]
                pTv = pT[:].bitcast(BF16)
                for kc in range(8):
                    kb.op('pe', lambda e, kc=kc, i2=i2, pTv=pTv: e.transpose(out=pTv[:, kc * 128:(kc + 1) * 128], in_=xs[i2][:, kc * 128:(kc + 1) * 128], identity=ident[:]),
                          reads=[xs[i2], ident], pwrites=[pT])
                kb.op('act', lambda e, i2=i2, pTv=pTv: e.copy(out=xT[i2][:], in_=pTv[:, 0:1024]), reads=[pT], writes=[xT[i2]])
                if xTs is not None:
                    kb.dma('pool', xTs.t[t], xT[i2][:], xT[i2], reads=[xT[i2]], pwrites=[xTs])
            pq = PB[PQB[t % 4]]
            for kc in range(8):
                kb.op('pe', lambda e, kc=kc, i2=i2, pq=pq: e.matmul(pq[:, 0:384], lhsT=xT[i2][:, kc * 128:(kc + 1) * 128], rhs=Wb0[:, kc, :], start=(kc == 0), stop=(kc == 7)),
                      reads=[xT[i2], Wb0], pwrites=[pq])

        def p2_stages():
            st = []

            def S(fn):
                st.append(fn)
            S(lambda t: kb.op('dve', lambda e, i=t % 4, pq=PB[PQB[t % 4]]: e.tensor_tensor(out=qkv[i][:], in0=pq[:, 0:384], in1=biasbc0[:], op=ALU.add),
                              reads=[PB[PQB[t % 4]], biasbc0], writes=[qkv[t % 4]]))
            S(lambda t: kb.op('act', lambda e, i=t % 4: e.activation(out=sq[i][:], in_=qkv[i][:, 0:256], func=AF.Square), reads=[qkv[t % 4]], writes=[sq[t % 4]]))
            S(lambda t: kb.op('dve', lambda e, i=t % 4: e.tensor_reduce(out=ss4[i][:], in_=sq[i][:].rearrange("p (g d) -> p g d", d=64), axis=AX.X, op=ALU.add),
                              reads=[sq[t % 4]], writes=[ss4[t % 4]]))
            S(lambda t: kb.op('act', lambda e, i=t % 4: e.activation(out=ss4[i][:], in_=ss4[i][:], func=AF.Sqrt, scale=1.0 / 64, bias=epsb[:, 0:1]),
                              reads=[ss4[t % 4], epsb], writes=[ss4[t % 4]]))
            S(lambda t: kb.op('dve', lambda e, i=t % 4: e.reciprocal(out=ss4[i][:], in_=ss4[i][:]), reads=[ss4[t % 4]], writes=[ss4[t % 4]]))
            S(lambda t: kb.op('dve', lambda e, i=t % 4: e.tensor_tensor(out=qkt[i][:].rearrange("p (g d) -> p g d", d=64), in0=qkv[i][:, 0:256].rearrange("p (g d) -> p g d", d=64),
                                                                  in1=ss4[i][:].unsqueeze(2).to_broadcast([128, 4, 64]), op=ALU.mult),
                              reads=[qkv[t % 4], ss4[t % 4]], writes=[qkt[t % 4]]))
            S(lambda t: kb.op('dve', lambda e, i=t % 4: e.tensor_tensor(out=qkn[i][:], in0=qkt[i][:], in1=gain_sb[:], op=ALU.mult), reads=[qkt[t % 4], gain_sb], writes=[qkn[t % 4]]))
            S(lambda t: kb.op('act', lambda e, i=t % 4, t=t: e.copy(out=Vb[:, t, 0:128], in_=qkv[i][:, 256:384]), reads=[qkv[t % 4]], pwrites=[Vb]))

            def tr(t):
                i = t % 4
                pqk = PB[4 + i // 2]
                pqkv = pqk[:].bitcast(BF16)
                c0 = (i % 2) * 256
                kb.op('pe', lambda e: e.transpose(out=pqkv[:, c0:c0 + 128], in_=qkn[i][:, 0:128], identity=ident[:]), reads=[qkn[i], ident], pwrites=[pqk])
                kb.op('pe', lambda e: e.transpose(out=pqkv[:, c0 + 128:c0 + 256], in_=qkn[i][:, 128:256], identity=ident[:]), reads=[qkn[i], ident], pwrites=[pqk])
            S(tr)

            def cp(t):
                i = t % 4
                pqk = PB[4 + i // 2]
                pqkv = pqk[:].bitcast(BF16)
                c0 = (i % 2) * 256
                kb.op('dve', lambda e: e.tensor_copy(out=qT[:, t * 128:(t + 1) * 128], in_=pqkv[:, c0:c0 + 128]), reads=[pqk], pwrites=[qT])
                kb.op('dve', lambda e: e.tensor_copy(out=kT[:, t * 128:(t + 1) * 128], in_=pqkv[:, c0 + 128:c0 + 256]), reads=[pqk], pwrites=[kT])
            S(cp)
            return st

        stages = p2_stages()
        G = 4
        for t in range(G):
            p1(t)
        for g in range(64 // G):
            tiles = list(range(g * G, (g + 1) * G))
            for t in tiles:
                stages[0](t)
            if g + 1 < 64 // G:
                for t in range((g + 1) * G, (g + 2) * G):
                    p1(t)
            for stg_ in stages[1:]:
                for t in tiles:
                    stg_(t)

        for qg in range(nqg_run):
            nkt = 4 * qg + 4
            qbase = qg * 512

            def emit_qk(kt):
                sl = kt % 2
                d = kt * 128 - qbase
                q0 = max(d, 0)
                for m in range(2):
                    Sp = PB[sl * 2 + m]
                    kb.op('pe', lambda e, m=m, Sp=Sp, q0=q0, kt=kt: e.matmul(Sp[:, q0:512], lhsT=kT[m * 64:(m + 1) * 64, kt * 128:(kt + 1) * 128],
                                                                      rhs=qT[m * 64:(m + 1) * 64, qbase + q0:qbase + 512], start=True, stop=True),
                          reads=[kT, qT], writes=[Sp])

            emit_qk(0)
            for kt in range(nkt):
                if kt + 1 < nkt:
                    emit_qk(kt + 1)
                sl = kt % 2
                d = kt * 128 - qbase
                q0 = max(d, 0)
                P = Pb[kt % 4]
                for m in range(2):
                    Sp = PB[sl * 2 + m]
                    if d >= -640:
                        bi = (d + 640) // 128
                        kb.op('dve', lambda e, Sp=Sp, bi=bi, q0=q0: e.tensor_tensor(out=Sp[:, q0:512], in0=Sp[:, q0:512], in1=bias_sb[:, bi, q0:512], op=ALU.add),
                              reads=[Sp, bias_sb], writes=[Sp])
                        kb.op('act', lambda e, Sp=Sp, P=P, m=m, q0=q0: e.activation(out=P[:, m, q0:512], in_=Sp[:, q0:512], func=AF.Exp),
                              reads=[Sp], pwrites=[P])
                    else:
                        kb.op('act', lambda e, Sp=Sp, P=P, m=m, q0=q0: e.activation(out=P[:, m, q0:512], in_=Sp[:, q0:512], func=AF.Exp),
                              reads=[Sp], pwrites=[P])
                for m in range(2):
                    for j in range(q0 // 128, 4):
                        bank, off, a = acc(m, j)
                        kb.op('pe', lambda e, bank=bank, off=off, a=a, P=P, m=m, j=j, kt=kt: e.matmul(
                            bank[:, off:off + 129], lhsT=P[:, m, j * 128:(j + 1) * 128], rhs=Vb[:, kt, :],
                            start=(kt == 0 and a % 3 == 0), stop=(kt == 4 * qg + j), skip_group_check=True),
                            reads=[P, Vb], pwrites=[bank])
            ost = ostage[qg % 2]
            for j in range(4):
                i2 = j % 2
                b1, o1, _ = acc(0, j)
                b2, o2, _ = acc(1, j)
                kb.op('dve', lambda e, i2=i2, b1=b1, o1=o1: e.reciprocal(out=rr[i2][:, 0:1], in_=b1[:, o1 + 128:o1 + 129]), reads=[b1], writes=[rr[i2]])
                kb.op('dve', lambda e, i2=i2, b2=b2, o2=o2: e.reciprocal(out=rr[i2][:, 1:2], in_=b2[:, o2 + 128:o2 + 129]), reads=[b2, rr[i2]], writes=[rr[i2]])
                kb.op('dve', lambda e, i2=i2: e.tensor_tensor(out=rr[i2][:, 2:3], in0=rr[i2][:, 1:2], in1=neglam[:], op=ALU.mult), reads=[rr[i2], neglam], writes=[rr[i2]])
                kb.op('dve', lambda e, i2=i2, b1=b1, o1=o1: e.tensor_scalar(out=t1[i2][:], in0=b1[:, o1:o1 + 128], scalar1=rr[i2][:, 0:1], scalar2=None, op0=ALU.mult),
                      reads=[b1, rr[i2]], writes=[t1[i2]])
                kb.op('dve', lambda e, i2=i2, b2=b2, o2=o2: e.scalar_tensor_tensor(out=ob[i2][:], in0=b2[:, o2:o2 + 128], scalar=rr[i2][:, 2:3], in1=t1[i2][:],
                                                                           op0=ALU.mult, op1=ALU.add), reads=[b2, rr[i2], t1[i2]], writes=[ob[i2]])
                kb.op('act', lambda e, i2=i2: e.activation(out=junk[:, 0:128], in_=ob[i2][:], func=AF.Square, accum_out=oss[i2][:]), reads=[ob[i2]], writes=[junk, oss[i2]])
                kb.op('act', lambda e, i2=i2: e.activation(out=oss[i2][:], in_=oss[i2][:], func=AF.Sqrt, scale=1.0 / 128, bias=epsb[:, 0:1]), reads=[oss[i2], epsb], writes=[oss[i2]])
                kb.op('dve', lambda e, i2=i2: e.reciprocal(out=oss[i2][:], in_=oss[i2][:]), reads=[oss[i2]], writes=[oss[i2]])
                kb.op('dve', lambda e, i2=i2: e.scalar_tensor_tensor(out=on[i2][:], in0=ob[i2][:], scalar=oss[i2][:, 0:1], in1=subg_sb[:], op0=ALU.mult, op1=ALU.mult),
                      reads=[ob[i2], oss[i2], subg_sb], writes=[on[i2]])
                pt = PB[7]
                ptv = pt[:].bitcast(BF16)
                kb.op('pe', lambda e, i2=i2, ptv=ptv, j=j: e.transpose(out=ptv[:, j * 128:(j + 1) * 128], in_=on[i2][:], identity=ident[:]), reads=[on[i2], ident], pwrites=[pt])
            kb.op('dve', lambda e, ost=ost: e.tensor_copy(out=ost[:], in_=PB[7][:].bitcast(BF16)[:, 0:512]), reads=[PB[7]], writes=[ost])
            kb.dma('sp', ag_ins[qg // 4].t[b, :, (qg % 4) * 512:(qg % 4) * 512 + 512], ost[:], ost, reads=[ost], pwrites=[ag_ins[qg // 4]])
            if after_chunk is not None and b == nb_run - 1 and qg % 4 == 3:
                after_chunk(qg // 4)


NT = 16
TOK = 2048
D = 1024
NE = 16
FF = 512


class Ctx:
    pass


def setup_ctx(kb, idf, idb, cT, consts):
    cx = Ctx()
    cx.identf = kb.sb("identf", [128, 128], F32)
    kb.dma('sp', cx.identf[:], idf[:], cx.identf, reads=[idf], writes=[cx.identf])
    cx.identb = kb.sb("identb", [128, 128], BF16)
    kb.dma('sp', cx.identb[:], idb[:], cx.identb, reads=[idb], writes=[cx.identb])
    cx.epsb = kb.sb("epsb", [128, 1], F32)
    kb.op('dve', lambda e: e.memset(cx.epsb[:], EPS), writes=[cx.epsb])
    cx.consts = kb.sb("consts", [128, 20], F32)
    kb.dma('sp', cx.consts[:], consts[:], cx.consts, reads=[consts], writes=[cx.consts])
    cT_sb = kb.sb("cT_sb", [128, 8, 1], F32)
    kb.dma('sp', cT_sb[:], cT[:], cT_sb, reads=[cT], writes=[cT_sb])
    cx.siluT = kb.sb("siluT", [128, 8, 1], F32)
    kb.op('act', lambda e: e.activation(out=cx.siluT[:], in_=cT_sb[:], func=AF.Silu), reads=[cT_sb], writes=[cx.siluT])
    cx.silubc = kb.sb("silubc", [128, 8, 128], F32)
    kb.op('dve', lambda e: e.tensor_copy(out=cx.silubc[:], in_=cx.siluT[:].to_broadcast([128, 8, 128])), reads=[cx.siluT], writes=[cx.silubc])
    cx.PB = [kb.ps(f"pb{i}", [128, 512], F32) for i in range(8)]
    cx.adaw_sb = kb.sb("adaw_sb", [128, 8, 512], F32)
    cx.junk = kb.sb("junk", [128, 1024], BF16)
    return cx


def mod_T(kb, cx, adaw, col0, nch, adabT_sb, j0, name):
    out = kb.sb(name, [128, nch], F32)
    pm = cx.PB[0]
    done = 0
    while done < nch:
        n = min(4, nch - done)
        c0 = col0 + done * 128
        kb.dma('sp', cx.adaw_sb[:, :, 0:n * 128], adaw.t[:, c0:c0 + n * 128].rearrange("(kc p) n -> p kc n", p=128), cx.adaw_sb,
               reads=[adaw], writes=[cx.adaw_sb])
        for jj in range(n):
            j = done + jj
            for kc in range(8):
                kb.op('pe', lambda e, j=j, jj=jj, kc=kc: e.matmul(pm[:, j:j + 1], lhsT=cx.adaw_sb[:, kc, jj * 128:(jj + 1) * 128],
                                                                rhs=cx.siluT[:, kc, :], start=(kc == 0), stop=(kc == 7)),
                      reads=[cx.adaw_sb, cx.siluT], pwrites=[pm])
        done += n
    kb.op('dve', lambda e: e.tensor_tensor(out=out[:], in0=pm[:, 0:nch], in1=adabT_sb[:, j0:j0 + nch], op=ALU.add), reads=[pm, adabT_sb], writes=[out])
    return out


def mod_row(kb, cx, adaw, col0, brow_sb, name):
    out = kb.sb(name, [128, 1024], F32)
    for hf in range(2):
        c0 = col0 + hf * 512
        kb.dma('sp', cx.adaw_sb[:], adaw.t[:, c0:c0 + 512].rearrange("(kc p) n -> p kc n", p=128), cx.adaw_sb, reads=[adaw], writes=[cx.adaw_sb])
        pm = cx.PB[1]
        for kc in range(8):
            kb.op('pe', lambda e, kc=kc: e.matmul(pm[:, 0:512], lhsT=cx.silubc[:, kc, :], rhs=cx.adaw_sb[:, kc, :], start=(kc == 0), stop=(kc == 7)),
                  reads=[cx.silubc, cx.adaw_sb], pwrites=[pm])
        kb.op('dve', lambda e, hf=hf: e.tensor_tensor(out=out[:, hf * 512:(hf + 1) * 512], in0=pm[:, 0:512], in1=brow_sb[:, hf * 512:(hf + 1) * 512], op=ALU.add),
              reads=[pm, brow_sb], pwrites=[out])
    return out


def emit_hT(kb, cx, xres, S_T, SH_T, hT_all, router=None):
    ss = [kb.sb(f"hss{i}", [128, 1], F32) for i in range(2)]
    xh = [kb.sb(f"xh{i}", [128, 1024], F32) for i in range(2)]
    hTf = [kb.sb(f"hTf{i}", [128, 8, 128], F32) for i in range(2)]
    def h1(t):
        i2 = t % 2
        kb.op('act', lambda e, t=t, i2=i2: e.activation(out=cx.junk[:], in_=xres[:, t, :], func=AF.Square, accum_out=ss[i2][:]), reads=[xres], writes=[cx.junk, ss[i2]])
        kb.op('act', lambda e, i2=i2: e.activation(out=ss[i2][:], in_=ss[i2][:], func=AF.Sqrt, scale=1.0 / 1024, bias=cx.epsb[:, 0:1]), reads=[ss[i2], cx.epsb], writes=[ss[i2]])
        kb.op('dve', lambda e, i2=i2: e.reciprocal(out=ss[i2][:], in_=ss[i2][:]), reads=[ss[i2]], writes=[ss[i2]])
        kb.op('dve', lambda e, t=t, i2=i2: e.tensor_scalar(out=xh[i2][:], in0=xres[:, t, :], scalar1=ss[i2][:, 0:1], scalar2=None, op0=ALU.mult),
              reads=[xres, ss[i2]], writes=[xh[i2]])
        pa, pb = cx.PB[2 + 2 * i2], cx.PB[3 + 2 * i2]
        for kc in range(8):
            pp = pa if kc < 4 else pb
            kb.op('pe', lambda e, kc=kc, pp=pp, i2=i2: e.transpose(out=pp[:, (kc % 4) * 128:(kc % 4 + 1) * 128], in_=xh[i2][:, kc * 128:(kc + 1) * 128], identity=cx.identf[:]),
                  reads=[xh[i2], cx.identf], pwrites=[pp])

    def h2(t):
        i2 = t % 2
        pa, pb = cx.PB[2 + 2 * i2], cx.PB[3 + 2 * i2]
        for kc in range(8):
            pp = pa if kc < 4 else pb
            kb.op('act', lambda e, kc=kc, pp=pp, i2=i2: e.activation(out=hTf[i2][:, kc, :], in_=pp[:, (kc % 4) * 128:(kc % 4 + 1) * 128], func=AF.Identity,
                                                                  scale=S_T[:, kc:kc + 1], bias=SH_T[:, kc:kc + 1]),
                  reads=[pp, S_T, SH_T], pwrites=[hTf[i2]])
        kb.op('dve', lambda e, t=t, i2=i2: e.tensor_copy(out=hT_all[:, :, t * 128:(t + 1) * 128], in_=hTf[i2][:]), reads=[hTf[i2]], pwrites=[hT_all])
        if router is not None:
            wr_sb, logits = router
            pl = cx.PB[6 + i2]
            for kc in range(8):
                kb.op('pe', lambda e, kc=kc, pl=pl, i2=i2: e.matmul(pl[:, 0:16], lhsT=hTf[i2][:, kc, :], rhs=wr_sb[:, kc, :], start=(kc == 0), stop=(kc == 7)),
                      reads=[hTf[i2], wr_sb], pwrites=[pl])
            kb.op('dve', lambda e, t=t, pl=pl: e.tensor_copy(out=logits[:, t, :], in_=pl[:, 0:16]), reads=[pl], pwrites=[logits])
    h1(0)
    for t in range(NT):
        if t + 1 < NT:
            h1(t + 1)
        h2(t)


def emit_routing(kb, cx, logits, rb_sb):
    def tl(name, shape):
        return kb.sb(name, shape, F32)
    n = [0]

    def dv(fn, reads, writes):
        kb.op('dve', fn, reads=reads, writes=writes)
    scores = tl("scores", [128, NT, 16])
    kb.op('act', lambda e: e.activation(out=scores[:], in_=logits[:], func=AF.Sigmoid), reads=[logits], writes=[scores])
    sel = tl("sel", [128, NT, 16])
    dv(lambda e: e.tensor_tensor(out=sel[:], in0=scores[:], in1=rb_sb[:].unsqueeze(1).to_broadcast([128, NT, 16]), op=ALU.add), [scores, rb_sb], [sel])
    s4 = sel[:].rearrange("p t (g k) -> p (t g) k", k=4)
    gs = tl("gs", [128, NT * 4])
    tmp = tl("tmpg", [128, NT * 4])
    pairs = [(0, 1), (0, 2), (0, 3), (1, 2), (1, 3), (2, 3)]
    for idx, (a, b) in enumerate(pairs):
        dst = gs if idx == 0 else tmp
        dv(lambda e, a=a, b=b, dst=dst: e.tensor_tensor(out=dst[:], in0=s4[:, :, a], in1=s4[:, :, b], op=ALU.add), [sel], [dst])
        if idx > 0:
            dv(lambda e: e.tensor_tensor(out=gs[:], in0=gs[:], in1=tmp[:], op=ALU.max), [gs, tmp], [gs])
    gs3 = gs[:].rearrange("p (t g) -> p t g", g=4)
    gmax = tl("gmax", [128, NT])
    dv(lambda e: e.tensor_reduce(out=gmax[:], in_=gs3, axis=AX.X, op=ALU.max), [gs], [gmax])
    eqg = tl("eqg", [128, NT, 4])
    dv(lambda e: e.tensor_tensor(out=eqg[:], in0=gs3, in1=gmax[:].unsqueeze(2).to_broadcast([128, NT, 4]), op=ALU.is_equal), [gs, gmax], [eqg])
    WG = cx.consts[:, 0:4].unsqueeze(1).to_broadcast([128, NT, 4])
    WE = cx.consts[:, 4:20].unsqueeze(1).to_broadcast([128, NT, 16])
    dv(lambda e: e.tensor_tensor(out=eqg[:], in0=eqg[:], in1=WG, op=ALU.mult), [eqg, cx.consts], [eqg])
    wmax = tl("wmax", [128, NT])
    dv(lambda e: e.tensor_reduce(out=wmax[:], in_=eqg[:], axis=AX.X, op=ALU.max), [eqg], [wmax])
    ohg = tl("ohg", [128, NT * 4])
    dv(lambda e: e.tensor_tensor(out=ohg[:].rearrange("p (t g) -> p t g", g=4), in0=eqg[:], in1=wmax[:].unsqueeze(2).to_broadcast([128, NT, 4]), op=ALU.is_equal),
       [eqg, wmax], [ohg])
    gm = tl("gm", [128, NT, 16])
    dv(lambda e: e.tensor_copy(out=gm[:].rearrange("p t (g k) -> p (t g) k", k=4), in_=ohg[:].unsqueeze(2).to_broadcast([128, NT * 4, 4])), [ohg], [gm])
    dv(lambda e: e.tensor_scalar(out=gm[:], in0=gm[:], scalar1=-1.0, scalar2=1e30, op0=ALU.add, op1=ALU.mult), [gm], [gm])
    selm = tl("selm", [128, NT, 16])
    dv(lambda e: e.tensor_tensor(out=selm[:], in0=sel[:], in1=gm[:], op=ALU.add), [sel, gm], [selm])
    ohs = []
    for r in range(2):
        m = tl(f"m{r}", [128, NT])
        dv(lambda e, m=m: e.tensor_reduce(out=m[:], in_=selm[:], axis=AX.X, op=ALU.max), [selm], [m])
        eq = tl(f"eq{r}", [128, NT, 16])
        dv(lambda e, m=m, eq=eq: e.tensor_tensor(out=eq[:], in0=selm[:], in1=m[:].unsqueeze(2).to_broadcast([128, NT, 16]), op=ALU.is_equal), [selm, m], [eq])
        dv(lambda e, eq=eq: e.tensor_tensor(out=eq[:], in0=eq[:], in1=WE, op=ALU.mult), [eq, cx.consts], [eq])
        wm = tl(f"wm{r}", [128, NT])
        dv(lambda e, wm=wm, eq=eq: e.tensor_reduce(out=wm[:], in_=eq[:], axis=AX.X, op=ALU.max), [eq], [wm])
        oh = tl(f"oh{r}", [128, NT, 16])
        dv(lambda e, oh=oh, eq=eq, wm=wm: e.tensor_tensor(out=oh[:], in0=eq[:], in1=wm[:].unsqueeze(2).to_broadcast([128, NT, 16]), op=ALU.is_equal), [eq, wm], [oh])
        ohs.append(oh)
        if r == 0:
            dv(lambda e, oh=oh: e.scalar_tensor_tensor(out=selm[:], in0=oh[:], scalar=-1e30, in1=selm[:], op0=ALU.mult, op1=ALU.add), [oh, selm], [selm])
    sv = []
    for r in range(2):
        pr = tl(f"pr{r}", [128, NT, 16])
        dv(lambda e, pr=pr, r=r: e.tensor_tensor(out=pr[:], in0=scores[:], in1=ohs[r][:], op=ALU.mult), [scores, ohs[r]], [pr])
        s = tl(f"sv{r}", [128, NT])
        dv(lambda e, pr=pr, s=s: e.tensor_reduce(out=s[:], in_=pr[:], axis=AX.X, op=ALU.add), [pr], [s])
        sv.append(s)
    den = tl("den", [128, NT])
    dv(lambda e: e.tensor_tensor(out=den[:], in0=sv[0][:], in1=sv[1][:], op=ALU.add), [sv[0], sv[1]], [den])
    dv(lambda e: e.reciprocal(out=den[:], in_=den[:]), [den], [den])
    for r in range(2):
        dv(lambda e, r=r: e.tensor_tensor(out=sv[r][:], in0=sv[r][:], in1=den[:], op=ALU.mult), [sv[r], den], [sv[r]])
    gates = tl("gates", [128, NT, 16])
    dv(lambda e: e.tensor_tensor(out=gates[:], in0=ohs[0][:], in1=sv[0][:].unsqueeze(2).to_broadcast([128, NT, 16]), op=ALU.mult), [ohs[0], sv[0]], [gates])
    dv(lambda e: e.tensor_tensor(out=ohs[1][:], in0=ohs[1][:], in1=sv[1][:].unsqueeze(2).to_broadcast([128, NT, 16]), op=ALU.mult), [ohs[1], sv[1]], [ohs[1]])
    dv(lambda e: e.tensor_tensor(out=gates[:], in0=gates[:], in1=ohs[1][:], op=ALU.add), [gates, ohs[1]], [gates])
    return gates


def emit_moe_dense(kb, cx, xres, hT_all, gates, wg, wu, wd, g2row):
    Wg = [kb.sb(f"Wg{i}", [128, 8, FF], BF16) for i in range(2)]
    Wu = [kb.sb(f"Wu{i}", [128, 8, FF], BF16) for i in range(2)]
    Wd = [kb.sb(f"Wd{i}", [128, 4, D], BF16) for i in range(2)]
    g2b = kb.sb("g2b", [128, D], BF16)
    kb.op('dve', lambda e: e.tensor_copy(out=g2b[:], in_=g2row[:]), reads=[g2row], writes=[g2b])
    sg = [kb.sb(f"sg{i}", [128, 512], F32) for i in range(2)]
    heT = [kb.sb(f"heT{i}", [128, 4, 512], BF16) for i in range(2)]
    PB = cx.PB
    for e_ in range(NE):
        s = e_ % 2
        kb.dma('pool', Wg[s][:], wg.t[e_].rearrange("(kc p) f -> p kc f", p=128), Wg[s], reads=[wg], writes=[Wg[s]])
        kb.dma('pool', Wu[s][:], wu.t[e_].rearrange("(kc p) f -> p kc f", p=128), Wu[s], reads=[wu], writes=[Wu[s]])
        kb.dma('pool', Wd[s][:], wd.t[e_].rearrange("(fc p) o -> p fc o", p=128), Wd[s], reads=[wd], writes=[Wd[s]])
        kb.op('pool', lambda e, s=s: e.tensor_tensor(out=Wd[s][:], in0=Wd[s][:], in1=g2b[:].unsqueeze(1).to_broadcast([128, 4, D]), op=ALU.mult),
              reads=[Wd[s], g2b], writes=[Wd[s]])
        for tg in range(4):
            hs = heT[tg % 2]
            for fc in range(4):
                pg, pu = PB[(fc % 2) * 2], PB[(fc % 2) * 2 + 1]
                for kc in range(8):
                    kb.op('pe', lambda e, kc=kc, fc=fc, pg=pg, s=s, tg=tg: e.matmul(pg[:, 0:512], lhsT=Wg[s][:, kc, fc * 128:(fc + 1) * 128], rhs=hT_all[:, kc, tg * 512:(tg + 1) * 512],
                                                                               start=(kc == 0), stop=(kc == 7)), reads=[Wg[s], hT_all], pwrites=[pg])
                for kc in range(8):
                    kb.op('pe', lambda e, kc=kc, fc=fc, pu=pu, s=s, tg=tg: e.matmul(pu[:, 0:512], lhsT=Wu[s][:, kc, fc * 128:(fc + 1) * 128], rhs=hT_all[:, kc, tg * 512:(tg + 1) * 512],
                                                                               start=(kc == 0), stop=(kc == 7)), reads=[Wu[s], hT_all], pwrites=[pu])
                sgs = sg[fc % 2]
                kb.op('act', lambda e, pg=pg, sgs=sgs: e.activation(out=sgs[:], in_=pg[:, 0:512], func=AF.Silu), reads=[pg], writes=[sgs])
                kb.op('dve', lambda e, pu=pu, sgs=sgs, hs=hs, fc=fc: e.tensor_tensor(out=hs[:, fc, :], in0=pu[:, 0:512], in1=sgs[:], op=ALU.mult), reads=[pu, sgs], pwrites=[hs])
            for tt in range(4):
                t = tg * 4 + tt
                py = [PB[4 + (tt % 2) * 2], PB[5 + (tt % 2) * 2]]
                for hf in range(2):
                    for fc in range(4):
                        kb.op('pe', lambda e, fc=fc, hf=hf, py=py, hs=hs, tt=tt, s=s: e.matmul(py[hf][:, 0:512], lhsT=hs[:, fc, tt * 128:(tt + 1) * 128], rhs=Wd[s][:, fc, hf * 512:(hf + 1) * 512],
                                                                                         start=(fc == 0), stop=(fc == 3)), reads=[hs, Wd[s]], pwrites=[py[hf]])
                for hf in range(2):
                    kb.op('dve', lambda e, hf=hf, py=py, t=t, e_=e_: e.scalar_tensor_tensor(out=xres[:, t, hf * 512:(hf + 1) * 512], in0=py[hf][:, 0:512], scalar=gates[:, t, e_:e_ + 1],
                                                                                      in1=xres[:, t, hf * 512:(hf + 1) * 512], op0=ALU.mult, op1=ALU.add),
                          reads=[py[hf], gates, xres], pwrites=[xres])


def emit_wo(kb, cx, xres, oT_sb, wo, g1row, nchunk=8):
    Wo = kb.sb("Wo", [128, 8, D], BF16)
    kb.dma('pool', Wo[:], wo.t.rearrange("(c p) o -> p c o", p=128), Wo, reads=[wo], writes=[Wo])
    g1b = kb.sb("g1b", [128, D], BF16)
    kb.op('dve', lambda e: e.tensor_copy(out=g1b[:], in_=g1row[:]), reads=[g1row], writes=[g1b])
    kb.op('pool', lambda e: e.tensor_tensor(out=Wo[:], in0=Wo[:], in1=g1b[:].unsqueeze(1).to_broadcast([128, 8, D]), op=ALU.mult), reads=[Wo, g1b], writes=[Wo])
    for t in range(NT):
        py = [cx.PB[(t % 2) * 2], cx.PB[(t % 2) * 2 + 1]]
        for hf in range(2):
            for c in range(8):
                kb.op('pe', lambda e, c=c, hf=hf, py=py, t=t: e.matmul(py[hf][:, 0:512], lhsT=oT_sb[:, c, t * 128:(t + 1) * 128], rhs=Wo[:, c, hf * 512:(hf + 1) * 512],
                                                                 start=(c == 0), stop=(c == 7)), reads=[oT_sb, Wo], pwrites=[py[hf]])
        for hf in range(2):
            kb.op('dve', lambda e, hf=hf, py=py, t=t: e.tensor_tensor(out=xres[:, t, hf * 512:(hf + 1) * 512], in0=py[hf][:, 0:512], in1=xres[:, t, hf * 512:(hf + 1) * 512], op=ALU.add),
                  reads=[py[hf], xres], pwrites=[xres])


def host_common(inp, j, layer):
    f32 = np.float32
    b = j // 4
    d = {}
    d["cT"] = np.ascontiguousarray(inp["c"][b].reshape(8, 128).T.reshape(128, 8, 1))
    d["idf"] = np.eye(128, dtype=f32)
    d["idb"] = np.eye(128).astype(NPBF)
    d["consts"] = np.ascontiguousarray(np.broadcast_to(np.concatenate([np.arange(4, 0, -1), np.arange(16, 0, -1)]).astype(f32)[None, :], (128, 20)))
    d["wr"] = np.ascontiguousarray(inp["router_w"])
    d["rb"] = np.ascontiguousarray(np.broadcast_to(inp["router_bias"][None, :], (128, 16))).astype(f32)
    return d


def rep(v, n=128):
    return np.ascontiguousarray(np.broadcast_to(np.asarray(v, np.float32).reshape(1, -1), (n, np.asarray(v).size)))


def host_inputs_B(inp, j, oT_all):
    f32 = np.float32
    d = host_common(inp, j, 0)
    r0 = j * TOK
    d["xin"] = np.ascontiguousarray(inp["x"].reshape(-1, D)[r0:r0 + TOK])
    d["oTin"] = np.ascontiguousarray(oT_all[:, :, r0:r0 + TOK])
    d["adaw0"] = np.ascontiguousarray(inp["ada_w"][0])
    d["adaw1"] = np.ascontiguousarray(inp["ada_w"][1][:, 0:2048])
    d["adabT0"] = np.ascontiguousarray(inp["ada_b"][0].reshape(48, 128).T)
    d["adabT1"] = np.ascontiguousarray(inp["ada_b"][1].reshape(48, 128).T)
    d["brow_g1"] = rep(inp["ada_b"][0][2048:3072])
    d["brow_g2"] = rep(inp["ada_b"][0][5120:6144])
    d["gffnT"] = np.ascontiguousarray(inp["norm_ffn_g"][0].reshape(8, 128).T)
    d["gmixT1"] = np.ascontiguousarray(inp["norm_mix_g"][1].reshape(8, 128).T)
    d["wo"] = np.ascontiguousarray(inp["a_w_o"][0])
    d["wg"] = np.ascontiguousarray(inp["moe_w_gate"][0]); d["wu"] = np.ascontiguousarray(inp["moe_w_up"][0]); d["wd"] = np.ascontiguousarray(inp["moe_w_down"][0])
    d["wqkv1"] = np.ascontiguousarray(inp["b_w_qkv"][0])
    d["gainq"] = rep(np.tile(inp["b_q_gain"][0], 8)); d["gaink"] = rep(np.tile(inp["b_k_gain"][0], 8))
    return d


def build_B(nc, do_moe=True):
    es = ExitStack()
    kb = KB(nc, es)
    I = {}
    def din(name, shape, dt=F32):
        I[name] = kb.dram(name, shape, dt, "ExternalInput")
    din("cT", [128, 8, 1]); din("idf", [128, 128]); din("idb", [128, 128], BF16); din("consts", [128, 20]); din("wr", [1024, 16]); din("rb", [128, 16])
    din("xin", [TOK, D]); din("oTin", [8, 128, TOK], BF16); din("adaw0", [1024, 6144]); din("adaw1", [1024, 2048]); din("adabT0", [128, 48]); din("adabT1", [128, 48])
    din("brow_g1", [128, 1024]); din("brow_g2", [128, 1024]); din("gffnT", [128, 8]); din("gmixT1", [128, 8]); din("wo", [1024, 1024])
    din("wg", [NE, 1024, FF]); din("wu", [NE, 1024, FF]); din("wd", [NE, FF, 1024]); din("wqkv1", [1024, 3072]); din("gainq", [128, 512]); din("gaink", [128, 512])
    x2 = kb.dram("x2", [TOK, D], F32, "ExternalOutput")
    qT1 = kb.dram("qT1", [128, 8, TOK], BF16, "ExternalOutput")
    kT1 = kb.dram("kT1", [128, 8, TOK], BF16, "ExternalOutput")
    V1 = kb.dram("V1", [TOK, D], BF16, "ExternalOutput")
    with es:
        cx = setup_ctx(kb, I["idf"], I["idb"], I["cT"], I["consts"])
        xres = kb.sb("xres", [128, NT, D], F32)
        kb.dma('sp', xres[:], I["xin"].t.rearrange("(t p) d -> p t d", p=128), xres, reads=[I["xin"]], writes=[xres])
        adabT0 = kb.sb("adabT0", [128, 48], F32)
        kb.dma('sp', adabT0[:], I["adabT0"][:], adabT0, reads=[I["adabT0"]], writes=[adabT0])
        adabT1 = kb.sb("adabT1", [128, 48], F32)
        kb.dma('sp', adabT1[:], I["adabT1"][:], adabT1, reads=[I["adabT1"]], writes=[adabT1])
        kb.push_scope()
        brow = kb.sb("brow", [128, 1024], F32)
        kb.dma('sp', brow[:], I["brow_g1"][:], brow, reads=[I["brow_g1"]], writes=[brow])
        g1row = mod_row(kb, cx, I["adaw0"], 2048, brow, "g1row")
        oT_sb = kb.sb("oT_sb", [128, 8, TOK], BF16)
        kb.dma('sp', oT_sb[:], I["oTin"].t.rearrange("h e t -> e h t"), oT_sb, reads=[I["oTin"]], writes=[oT_sb])
        emit_wo(kb, cx, xres, oT_sb, I["wo"], g1row)
        kb.pop_scope()
        emit_moe_layer(kb, cx, xres, I["adaw0"], adabT0, I["brow_g2"], I["gffnT"], I["wr"], I["rb"], I["wg"], I["wu"], I["wd"], do_moe)
        kb.dma('sp', x2.t.rearrange("(t p) d -> p t d", p=128), xres[:], xres, reads=[xres], writes=[x2])
        kb.push_scope()
        S_T, SH_T = mod_S(kb, cx, I["adaw1"], adabT1, 0, 1024, 0, 8, I["gmixT1"], "l1a")
        hT_all = kb.sb("hT_all", [128, 8, TOK], BF16)
        kb.push_scope()
        emit_hT(kb, cx, xres, S_T, SH_T, hT_all)
        kb.pop_scope()
        emit_qkv1(kb, cx, hT_all, I["wqkv1"], I["gainq"], I["gaink"], qT1, kT1, V1)
        kb.pop_scope()
        kb.finish([x2, qT1, kT1, V1])
    return nc


def mod_S(kb, cx, adaw, adabT_sb, col_sh, col_sc, j_sh, j_sc, gT_dram, tag):
    shT = mod_T(kb, cx, adaw, col_sh, 8, adabT_sb, j_sh, "shT" + tag)
    scT = mod_T(kb, cx, adaw, col_sc, 8, adabT_sb, j_sc, "scT" + tag)
    gT = kb.sb("gT" + tag, [128, 8], F32)
    kb.dma('sp', gT[:], gT_dram[:], gT, reads=[gT_dram], writes=[gT])
    S_T = kb.sb("S_T" + tag, [128, 8], F32)
    kb.op('dve', lambda e: e.scalar_tensor_tensor(out=S_T[:], in0=scT[:], scalar=1.0, in1=gT[:], op0=ALU.add, op1=ALU.mult), reads=[scT, gT], writes=[S_T])
    return S_T, shT


def emit_moe_layer(kb, cx, xres, adaw, adabT_sb, brow_g2_dram, gffnT_dram, wr, rb, wg, wu, wd, do_moe=True):
    kb.push_scope()
    S_T, SH_T = mod_S(kb, cx, adaw, adabT_sb, 3072, 4096, 24, 32, gffnT_dram, "ffn")
    brow = kb.sb("brow2", [128, 1024], F32)
    kb.dma('sp', brow[:], brow_g2_dram[:], brow, reads=[brow_g2_dram], writes=[brow])
    g2row = mod_row(kb, cx, adaw, 5120, brow, "g2row")
    hT_all = kb.sb("hT_all", [128, 8, TOK], BF16)
    gates = None
    logits = kb.sb("logits", [128, NT, 16], F32)
    wr_sb = kb.sb("wr_sb", [128, 8, 16], F32)
    kb.dma('sp', wr_sb[:], wr.t.rearrange("(kc p) e -> p kc e", p=128), wr_sb, reads=[wr], writes=[wr_sb])
    rb_sb = kb.sb("rb_sb", [128, 16], F32)
    kb.dma('sp', rb_sb[:], rb[:], rb_sb, reads=[rb], writes=[rb_sb])
    gates_keep = kb.sb("gates_keep", [128, NT, 16], F32)
    kb.push_scope()
    emit_hT(kb, cx, xres, S_T, SH_T, hT_all, router=(wr_sb, logits))
    kb.pop_scope()
    kb.push_scope()
    gates = emit_routing(kb, cx, logits, rb_sb)
    kb.op('dve', lambda e: e.tensor_copy(out=gates_keep[:], in_=gates[:]), reads=[gates], writes=[gates_keep])
    kb.pop_scope()
    cx.last_gates = gates_keep
    if do_moe:
        emit_moe_dense(kb, cx, xres, hT_all, gates_keep, wg, wu, wd, g2row)
    kb.pop_scope()


def emit_qkv1(kb, cx, hT_all, wqkv, gainq, gaink, qT1, kT1, V1, after_kv=None):
    Wblk = [kb.sb(f"Wblk{i}", [128, 8, 512], BF16) for i in range(3)]
    gq = kb.sb("gq", [128, 512], F32)
    kb.dma('sp', gq[:], gainq[:], gq, reads=[gainq], writes=[gq])
    kb.op('dve', lambda e: e.tensor_scalar(out=gq[:], in0=gq[:], scalar1=0.125, scalar2=None, op0=ALU.mult), reads=[gq], writes=[gq])
    gk = kb.sb("gk", [128, 512], F32)
    kb.dma('sp', gk[:], gaink[:], gk, reads=[gaink], writes=[gk])
    sq = [kb.sb(f"qsq{i}", [128, 512], F32) for i in range(2)]
    ss8 = [kb.sb(f"qss8{i}", [128, 8], F32) for i in range(2)]
    tmp = [kb.sb(f"qtmp{i}", [128, 512], F32) for i in range(2)]
    nrm = [kb.sb(f"qnrm{i}", [128, 512], BF16) for i in range(2)]
    stg = [kb.sb(f"qstg{i}", [128, 4, 128], BF16) for i in range(2)]
    vst = [kb.sb(f"vst{i}", [128, 512], BF16) for i in range(2)]
    order = [2, 3, 4, 5, 0, 1]

    def issue_w(idx):
        cbw = order[idx]
        Ww = Wblk[idx % 3]
        kb.dma('pool', Ww[:], wqkv.t[:, cbw * 512:(cbw + 1) * 512].rearrange("(kc p) n -> p kc n", p=128), Ww, reads=[wqkv], writes=[Ww])
    issue_w(0)
    issue_w(1)
    for idx, cb in enumerate(order):
        Wb_ = Wblk[idx % 3]
        if idx + 2 < 6:
            issue_w(idx + 2)
        def q1(t, cb=cb, Wb_=Wb_):
            i2 = t % 2
            pq = cx.PB[i2]
            for kc in range(8):
                kb.op('pe', lambda e, kc=kc, pq=pq, t=t, Wb_=Wb_: e.matmul(pq[:, 0:512], lhsT=hT_all[:, kc, t * 128:(t + 1) * 128], rhs=Wb_[:, kc, :], start=(kc == 0), stop=(kc == 7)),
                      reads=[hT_all, Wb_], pwrites=[pq])

        def q2(t, cb=cb):
            i2 = t % 2
            pq = cx.PB[i2]
            if cb < 4:
                g = gq if cb < 2 else gk
                dst = qT1 if cb < 2 else kT1
                kb.op('act', lambda e, pq=pq, i2=i2: e.activation(out=sq[i2][:], in_=pq[:, 0:512], func=AF.Square), reads=[pq], writes=[sq[i2]])
                kb.op('dve', lambda e, i2=i2: e.tensor_reduce(out=ss8[i2][:], in_=sq[i2][:].rearrange("p (g d) -> p g d", d=64), axis=AX.X, op=ALU.add), reads=[sq[i2]], writes=[ss8[i2]])
                kb.op('act', lambda e, i2=i2: e.activation(out=ss8[i2][:], in_=ss8[i2][:], func=AF.Sqrt, scale=1.0 / 64, bias=cx.epsb[:, 0:1]), reads=[ss8[i2], cx.epsb], writes=[ss8[i2]])
                kb.op('dve', lambda e, i2=i2: e.reciprocal(out=ss8[i2][:], in_=ss8[i2][:]), reads=[ss8[i2]], writes=[ss8[i2]])
                kb.op('dve', lambda e, i2=i2, pq=pq: e.tensor_tensor(out=tmp[i2][:].rearrange("p (g d) -> p g d", d=64), in0=pq[:, 0:512].rearrange("p (g d) -> p g d", d=64),
                                                              in1=ss8[i2][:].unsqueeze(2).to_broadcast([128, 8, 64]), op=ALU.mult), reads=[pq, ss8[i2]], writes=[tmp[i2]])
                kb.op('pool', lambda e, i2=i2, g=g: e.tensor_tensor(out=nrm[i2][:], in0=tmp[i2][:], in1=g[:], op=ALU.mult), reads=[tmp[i2], g], writes=[nrm[i2]])
                pt = cx.PB[2 + i2]
                ptv = pt[:].bitcast(BF16)
                for c in range(4):
                    kb.op('pe', lambda e, c=c, ptv=ptv, i2=i2: e.transpose(out=ptv[:, c * 128:(c + 1) * 128], in_=nrm[i2][:, c * 128:(c + 1) * 128], identity=cx.identb[:]),
                          reads=[nrm[i2], cx.identb], pwrites=[pt])
                kb.op('act', lambda e, ptv=ptv, i2=i2: e.copy(out=stg[i2][:], in_=ptv[:, 0:512].rearrange("p (c t) -> p c t", t=128)), reads=[pt], writes=[stg[i2]])
                p0 = (cb % 2) * 4
                kb.dma('sp', dst.t[:, p0:p0 + 4, t * 128:(t + 1) * 128], stg[i2][:], stg[i2], reads=[stg[i2]], pwrites=[dst])
            else:
                kb.op('act', lambda e, pq=pq, i2=i2: e.copy(out=vst[i2][:], in_=pq[:, 0:512]), reads=[pq], writes=[vst[i2]])
                c0 = (cb - 4) * 512
                kb.dma('sp', V1.t[t * 128:(t + 1) * 128, c0:c0 + 512], vst[i2][:], vst[i2], reads=[vst[i2]], pwrites=[V1])
        q1(0)
        for t in range(NT):
            if t + 1 < NT:
                q1(t + 1)
            q2(t)
        if cb == 5 and after_kv is not None:
            after_kv()


def host_inputs_C(inp, j, x2, qT1, kT1, V1, kT_prev, V_prev):
    f32 = np.float32
    d = host_common(inp, j, 1)
    d["xin"] = np.ascontiguousarray(x2)
    d["qT1"] = np.ascontiguousarray(qT1); d["kT1"] = np.ascontiguousarray(kT1); d["V1"] = np.ascontiguousarray(V1)
    if kT_prev is None:
        d["kTh"] = np.zeros((128, 8, 512), NPBF); d["Vh"] = np.zeros((512, D), NPBF)
        d["hmask"] = np.full((128, 1), -30000.0, f32)
    else:
        d["kTh"] = np.ascontiguousarray(kT_prev[:, :, TOK - 512:]); d["Vh"] = np.ascontiguousarray(V_prev[TOK - 512:])
        d["hmask"] = np.zeros((128, 1), f32)
    rb = inp["b_rel_bias"][0]
    kk = np.arange(128)[:, None]; qq = np.arange(128)[None, :]
    tiles = np.zeros((128, 5, 16, 128), f32)
    mask = np.zeros((128, 5, 128), f32)
    for r in range(5):
        rel = (r - 4) * 128 + kk - qq
        idx = np.clip(rel, -256, 256) + 256
        dc = 2 * (r - 4) + kk // 64 - qq // 64
        mask[:, r, :] = np.where((dc >= -8) & (dc <= 0), 0.0, -30000.0)
        for half in range(2):
            for par in range(2):
                for p in range(4):
                    hd = half * 8 + 2 * p + par
                    tiles[:, r, half * 8 + par * 4 + p, :] = rb[hd][idx]
    d["relb"] = tiles; d["bmask"] = mask
    d["adaw1"] = np.ascontiguousarray(inp["ada_w"][1])
    d["adabT1"] = np.ascontiguousarray(inp["ada_b"][1].reshape(48, 128).T)
    d["brow_g1"] = rep(inp["ada_b"][1][2048:3072]); d["brow_g2"] = rep(inp["ada_b"][1][5120:6144])
    d["gffnT"] = np.ascontiguousarray(inp["norm_ffn_g"][1].reshape(8, 128).T)
    d["wo"] = np.ascontiguousarray(inp["b_w_o"][0])
    d["wg"] = np.ascontiguousarray(inp["moe_w_gate"][1]); d["wu"] = np.ascontiguousarray(inp["moe_w_up"][1]); d["wd"] = np.ascontiguousarray(inp["moe_w_down"][1])
    return d


def build_C(nc, do_moe=True):
    es = ExitStack()
    kb = KB(nc, es)
    I = {}
    def din(name, shape, dt=F32):
        I[name] = kb.dram(name, shape, dt, "ExternalInput")
    din("cT", [128, 8, 1]); din("idf", [128, 128]); din("idb", [128, 128], BF16); din("consts", [128, 20]); din("wr", [1024, 16]); din("rb", [128, 16])
    din("xin", [TOK, D]); din("qT1", [128, 8, TOK], BF16); din("kT1", [128, 8, TOK], BF16); din("V1", [TOK, D], BF16)
    din("kTh", [128, 8, 512], BF16); din("Vh", [512, D], BF16); din("hmask", [128, 1]); din("relb", [128, 5, 16, 128]); din("bmask", [128, 5, 128])
    din("adaw1", [1024, 6144]); din("adabT1", [128, 48]); din("brow_g1", [128, 1024]); din("brow_g2", [128, 1024]); din("gffnT", [128, 8]); din("wo", [1024, 1024])
    din("wg", [NE, 1024, FF]); din("wu", [NE, 1024, FF]); din("wd", [NE, FF, 1024])
    out = kb.dram("out", [TOK, D], F32, "ExternalOutput")
    with es:
        cx = setup_ctx(kb, I["idf"], I["idb"], I["cT"], I["consts"])
        xres = kb.sb("xres", [128, NT, D], F32)
        kb.dma('sp', xres[:], I["xin"].t.rearrange("(t p) d -> p t d", p=128), xres, reads=[I["xin"]], writes=[xres])
        adabT1 = kb.sb("adabT1", [128, 48], F32)
        kb.dma('sp', adabT1[:], I["adabT1"][:], adabT1, reads=[I["adabT1"]], writes=[adabT1])
        emit_attn1(kb, cx, xres, I)
        emit_moe_layer(kb, cx, xres, I["adaw1"], adabT1, I["brow_g2"], I["gffnT"], I["wr"], I["rb"], I["wg"], I["wu"], I["wd"], do_moe)
        kb.dma('sp', out.t.rearrange("(t p) d -> p t d", p=128), xres[:], xres, reads=[xres], writes=[out])
        kb.finish([out])
    return nc


def emit_attn1(kb, cx, xres, I):
    kb.push_scope()
    brow = kb.sb("brow", [128, 1024], F32)
    kb.dma('sp', brow[:], I["brow_g1"][:], brow, reads=[I["brow_g1"]], writes=[brow])
    g1row = mod_row(kb, cx, I["adaw1"], 2048, brow, "g1row")
    Wo = kb.sb("Wo", [128, 8, D], BF16)
    kb.dma('pool', Wo[:], I["wo"].t.rearrange("(c p) o -> p c o", p=128), Wo, reads=[I["wo"]], writes=[Wo])
    g1b = kb.sb("g1b", [128, D], BF16)
    kb.op('dve', lambda e: e.tensor_copy(out=g1b[:], in_=g1row[:]), reads=[g1row], writes=[g1b])
    kb.op('pool', lambda e: e.tensor_tensor(out=Wo[:], in0=Wo[:], in1=g1b[:].unsqueeze(1).to_broadcast([128, 8, D]), op=ALU.mult), reads=[Wo, g1b], writes=[Wo])
    hmask = kb.sb("hmask", [128, 1], F32)
    kb.dma('sp', hmask[:], I["hmask"][:], hmask, reads=[I["hmask"]], writes=[hmask])
    bmask = kb.sb("bmask", [128, 5, 128], BF16)
    kb.dma('pool', bmask[:], I["bmask"][:], bmask, reads=[I["bmask"]], writes=[bmask])
    qTh = kb.sb("qTh", [128, 4, TOK], BF16)
    kTa = kb.sb("kTa", [128, 4, TOK + 512], BF16)
    Va = kb.sb("Va", [128, 20, 8, 65], BF16)
    kb.op('pool', lambda e: e.memset(Va[:, :, :, 64:65], 1.0), pwrites=[Va])
    relb = kb.sb("relb", [128, 5, 8, 128], BF16)
    Pb = [kb.sb(f"P1_{i}", [128, 512], BF16) for i in range(4)]
    r4 = [kb.sb(f"r4_{i}", [128, 4], F32) for i in range(2)]
    onh = [kb.sb(f"onh{i}", [128, 512], BF16) for i in range(2)]
    oTt = [kb.sb(f"oTt{i}", [128, 4, 128], BF16) for i in range(2)]
    PB = cx.PB
    for half in range(2):
        kb.dma('sp', qTh[:], I["qT1"].t[:, half * 4:(half + 1) * 4, :], qTh, reads=[I["qT1"]], writes=[qTh])
        kb.dma('sp', kTa[:, :, 0:512], I["kTh"].t[:, half * 4:(half + 1) * 4, :], kTa, reads=[I["kTh"]], writes=[kTa])
        kb.dma('sp', kTa[:, :, 512:], I["kT1"].t[:, half * 4:(half + 1) * 4, :], kTa, reads=[I["kT1"]], pwrites=[kTa])
        kb.barrier_bufs = None
        for a in range(20):
            src = I["Vh"] if a < 4 else I["V1"]
            ra = a if a < 4 else a - 4
            kb.dma('sp', Va[:, a, :, 0:64], src.t[ra * 128:(ra + 1) * 128, half * 512:(half + 1) * 512].rearrange("p (h d) -> p h d", d=64), Va,
                   reads=[src], writes=([Va] if (a == 0) else []), pwrites=([] if a == 0 else [Va]))
        kb.dma('pool', relb[:], I["relb"].t[:, :, half * 8:(half + 1) * 8, :], relb, reads=[I["relb"]], writes=[relb])
        kb.op('dve', lambda e: e.tensor_tensor(out=relb[:], in0=relb[:], in1=bmask[:].unsqueeze(2).to_broadcast([128, 5, 8, 128]), op=ALU.add), reads=[relb, bmask], writes=[relb])
        accb = [PB[4], PB[5]]
        steps = [(t, r, par) for t in range(NT) for r in range(5) for par in range(2)]
        LA = 3

        def e_qk(s):
            t, r, par = steps[s]
            a_ = t + r
            Sp = PB[s % 4]
            for p in range(4):
                kb.op('pe', lambda e, Sp=Sp, p=p, par=par, a_=a_, t=t: e.matmul(Sp[:, p * 128:(p + 1) * 128], lhsT=kTa[par * 64:(par + 1) * 64, p, a_ * 128:(a_ + 1) * 128],
                                                                         rhs=qTh[par * 64:(par + 1) * 64, p, t * 128:(t + 1) * 128], start=True, stop=True),
                      reads=[kTa, qTh], pwrites=[Sp])

        def e_rest(s):
            t, r, par = steps[s]
            a_ = t + r
            Sp = PB[s % 4]
            P = Pb[s % 4]
            kb.op('dve', lambda e, Sp=Sp, r=r, par=par: e.tensor_tensor(out=Sp[:, 0:512], in0=Sp[:, 0:512], in1=relb[:, r, par * 4:(par + 1) * 4, :].rearrange("p h q -> p (h q)"), op=ALU.add),
                  reads=[Sp, relb], writes=[Sp])
            if a_ < 4:
                kb.op('act', lambda e, Sp=Sp, P=P: e.activation(out=P[:], in_=Sp[:, 0:512], func=AF.Exp, bias=hmask[:, 0:1]), reads=[Sp, hmask], writes=[P])
            else:
                kb.op('act', lambda e, Sp=Sp, P=P: e.activation(out=P[:], in_=Sp[:, 0:512], func=AF.Exp), reads=[Sp], writes=[P])
            for p in range(4):
                hs = 2 * p + par
                kb.op('pe', lambda e, P=P, p=p, par=par, hs=hs, a_=a_, r=r: e.matmul(accb[par][:, p * 65:(p + 1) * 65], lhsT=P[:, p * 128:(p + 1) * 128], rhs=Va[:, a_, hs, :],
                                                                              start=(r == 0 and p == 0), stop=(r == 4), skip_group_check=True),
                      reads=[P, Va], pwrites=[accb[par]])

        def e_fin(t):
            i2 = t % 2
            for par in range(2):
                av = accb[par][:, 0:260].rearrange("p (h d) -> p h d", d=65)
                kb.op('dve', lambda e, av=av, i2=i2: e.reciprocal(out=r4[i2][:], in_=av[:, :, 64]), reads=[accb[par]], writes=[r4[i2]])
                kb.op('dve', lambda e, av=av, i2=i2, par=par: e.tensor_tensor(out=onh[i2][:].rearrange("p (h two d) -> p h two d", two=2, d=64)[:, :, par, :], in0=av[:, :, 0:64],
                                                                        in1=r4[i2][:].unsqueeze(2).to_broadcast([128, 4, 64]), op=ALU.mult),
                      reads=[accb[par], r4[i2]], pwrites=[onh[i2]])

        def e_tail(t):
            i2 = t % 2
            pt = PB[6]
            ptv = pt[:].bitcast(BF16)
            for c in range(4):
                kb.op('pe', lambda e, c=c, i2=i2, ptv=ptv: e.transpose(out=ptv[:, c * 128:(c + 1) * 128], in_=onh[i2][:, c * 128:(c + 1) * 128], identity=cx.identb[:]),
                      reads=[onh[i2], cx.identb], pwrites=[pt])
            kb.op('act', lambda e, ptv=ptv, i2=i2: e.copy(out=oTt[i2][:], in_=ptv[:, 0:512].rearrange("p (c t) -> p c t", t=128)), reads=[pt], writes=[oTt[i2]])
            for hf in range(2):
                py = PB[7]
                for c in range(4):
                    kb.op('pe', lambda e, c=c, hf=hf, i2=i2, py=py, half=half: e.matmul(py[:, 0:512], lhsT=oTt[i2][:, c, :], rhs=Wo[:, half * 4 + c, hf * 512:(hf + 1) * 512],
                                                                                 start=(c == 0), stop=(c == 3)), reads=[oTt[i2], Wo], pwrites=[py])
                kb.op('dve', lambda e, hf=hf, py=py, t=t: e.tensor_tensor(out=xres[:, t, hf * 512:(hf + 1) * 512], in0=py[:, 0:512], in1=xres[:, t, hf * 512:(hf + 1) * 512], op=ALU.add),
                      reads=[py, xres], pwrites=[xres])

        ns = len(steps)
        for s0 in range(min(LA, ns)):
            e_qk(s0)
        for s_ in range(ns):
            if s_ + LA < ns:
                e_qk(s_ + LA)
            e_rest(s_)
            t, r, par = steps[s_]
            if r == 4 and par == 1:
                e_fin(t)
            if r == 1 and par == 1 and t > 0:
                e_tail(t - 1)
        e_tail(NT - 1)
    kb.pop_scope()


GROUPS = [[0, 1, 2, 3], [4, 5, 6, 7]]


def host_inputs_F(inp, j):
    f32 = np.float32
    b, i = j // 4, j % 4
    d = {}
    a0 = host_inputs_A(inp, 2 * i)
    a1 = host_inputs_A(inp, 2 * i + 1)
    d["x_b"] = np.ascontiguousarray(inp["x"][b])
    cb = inp["c"][b].reshape(8, 128).T
    d["cT2"] = np.ascontiguousarray(np.stack([cb, cb], axis=2))
    d["gmixT0"] = a0["gT"]
    d["w2"] = np.ascontiguousarray(np.stack([a0["w"], a1["w"]], 0))
    d["gain"] = a0["gain"]; d["lam"] = a0["lam"]; d["subg"] = a0["subg"]; d["maskT"] = a0["maskT"]
    d["biasT2"] = np.ascontiguousarray(np.stack([a0["biasT"], a1["biasT"]], 1))
    d["c15_2"] = np.ascontiguousarray(np.concatenate([a0["c15"], a1["c15"]], 1))
    d.update(host_common(inp, j, 0))
    r0 = j * TOK
    d["xin"] = np.ascontiguousarray(inp["x"].reshape(-1, D)[r0:r0 + TOK])
    d["adaw0"] = np.ascontiguousarray(inp["ada_w"][0]); d["adaw1"] = np.ascontiguousarray(inp["ada_w"][1])
    d["adabT0"] = np.ascontiguousarray(inp["ada_b"][0].reshape(48, 128).T)
    d["adabT1"] = np.ascontiguousarray(inp["ada_b"][1].reshape(48, 128).T)
    for l in range(2):
        d[f"brow_g1_{l}"] = rep(inp["ada_b"][l][2048:3072]); d[f"brow_g2_{l}"] = rep(inp["ada_b"][l][5120:6144])
        d[f"gffnT{l}"] = np.ascontiguousarray(inp["norm_ffn_g"][l].reshape(8, 128).T)
        d[f"wg{l}"] = np.ascontiguousarray(inp["moe_w_gate"][l]); d[f"wu{l}"] = np.ascontiguousarray(inp["moe_w_up"][l]); d[f"wd{l}"] = np.ascontiguousarray(inp["moe_w_down"][l])
    d["gmixT1"] = np.ascontiguousarray(inp["norm_mix_g"][1].reshape(8, 128).T)
    d["wo0"] = np.ascontiguousarray(inp["a_w_o"][0]); d["wo1"] = np.ascontiguousarray(inp["b_w_o"][0])
    d["wqkv1"] = np.ascontiguousarray(inp["b_w_qkv"][0])
    d["gainq"] = rep(np.tile(inp["b_q_gain"][0], 8)); d["gaink"] = rep(np.tile(inp["b_k_gain"][0], 8))
    cdum = host_inputs_C(inp, j, np.zeros((1, 1), f32), np.zeros((1, 1), NPBF), np.zeros((1, 1), NPBF), np.zeros((1, 1), NPBF), None, None)
    d["relb"] = cdum["relb"]; d["bmask"] = cdum["bmask"]
    d["hmask"] = np.full((128, 1), -30000.0 if i == 0 else 0.0, f32)
    p = np.arange(128)
    d["idx_o"] = np.ascontiguousarray(np.stack([(i * 1024 + (h // 2) * 256 + (h % 2) * 128 + p) for h in range(8)], 1)).astype(np.int32)
    prev = max(i - 1, 0)
    d["idx_k"] = (prev * 128 + p).astype(np.int32).reshape(128, 1)
    d["idx_v"] = np.ascontiguousarray(np.stack([prev * 512 + a * 128 + p for a in range(4)], 1)).astype(np.int32)
    return d


def build_F(nc, stage=99):
    es = ExitStack()
    kb = KB(nc, es)
    I = {}
    def din(name, shape, dt=F32):
        I[name] = kb.dram(name, shape, dt, "ExternalInput")
    def dint(name, shape, dt=BF16):
        I[name] = kb.dram(name, shape, dt, "Internal")
    din("x_b", [8192, D]); din("cT2", [128, 8, 2]); din("gmixT0", [128, 8]); din("w2", [2, 1024, 384]); din("gain", [128, 256]); din("lam", [128, 256])
    din("subg", [128, 128]); din("maskT", [128, 4, 512]); din("biasT2", [128, 2, 9, 512]); din("c15_2", [128, 2])
    din("cT", [128, 8, 1]); din("idf", [128, 128]); din("idb", [128, 128], BF16); din("consts", [128, 20]); din("wr", [1024, 16]); din("rb", [128, 16])
    din("xin", [TOK, D]); din("adaw0", [1024, 6144]); din("adaw1", [1024, 6144]); din("adabT0", [128, 48]); din("adabT1", [128, 48])
    for l in range(2):
        din(f"brow_g1_{l}", [128, 1024]); din(f"brow_g2_{l}", [128, 1024]); din(f"gffnT{l}", [128, 8])
        din(f"wg{l}", [NE, 1024, FF]); din(f"wu{l}", [NE, 1024, FF]); din(f"wd{l}", [NE, FF, 1024])
    din("gmixT1", [128, 8]); din("wo0", [1024, 1024]); din("wo1", [1024, 1024]); din("wqkv1", [1024, 3072]); din("gainq", [128, 512]); din("gaink", [128, 512])
    din("relb", [128, 5, 16, 128]); din("bmask", [128, 5, 128]); din("hmask", [128, 1])
    din("idx_o", [128, 8], I32); din("idx_k", [128, 1], I32); din("idx_v", [128, 4], I32)
    out = kb.dram("out", [TOK, D], F32, "ExternalOutput")
    for a in range(4):
        dint(f"ag_in{a}", [2, 128, TOK])
    dint("ag_out", [4 * 4 * 2 * 128, TOK]); dint("xTs", [64, 128, 1024])
    dint("qT1", [128, 8, TOK]); dint("kT1", [128, 8, TOK]); dint("V1", [TOK, D])
    dint("agk_in", [128, 4096]); dint("agk_out", [512, 4096]); dint("agv_in", [512, D]); dint("agv_out", [2048, D]); dint("kTh", [128, 8, 512]); dint("Vh", [512, D])
    with es:
        cx = setup_ctx(kb, I["idf"], I["idb"], I["cT"], I["consts"])
        dummy = kb.sb("ccdummy", [128, 1], F32)
        idx_o = kb.sb("idx_o", [128, 8], I32); idx_k = kb.sb("idx_k", [128, 1], I32); idx_v = kb.sb("idx_v", [128, 4], I32)
        kb.dma('sp', idx_o[:], I["idx_o"][:], idx_o, reads=[I["idx_o"]], writes=[idx_o])
        kb.dma('sp', idx_k[:], I["idx_k"][:], idx_k, reads=[I["idx_k"]], writes=[idx_k])
        kb.dma('sp', idx_v[:], I["idx_v"][:], idx_v, reads=[I["idx_v"]], writes=[idx_v])
        kb.push_scope()
        adabT0a = kb.sb("adabT0a", [128, 16], F32)
        ag_ins = [I[f"ag_in{a}"] for a in range(4)]

        deferred = []

        def after_chunk(a, force=False):
            if a == 3 and not force:
                deferred.append(a)
                return
            kb.collective_allgather(T(ag_ins[a].t.rearrange("h e t -> (h e) t"), ag_ins[a].b),
                                    T(I["ag_out"].t[a * 1024:(a + 1) * 1024, :], I["ag_out"].b), GROUPS, dummy, partial=True)
        emit_A2(kb, cx, I["x_b"], I["cT2"], I["adaw0"], kb_slice(I["adabT0"], 16), I["gmixT0"], I["w2"], I["gain"], I["lam"], I["subg"], I["biasT2"], I["c15_2"],
                I["maskT"], ag_ins, after_chunk, I["xTs"])
        kb.pop_scope()
        for a in deferred:
            after_chunk(a, force=True)
        if stage == 1:
            kb.finish([out]); return nc
        xres = kb.sb("xres", [128, NT, D], F32)
        kb.dma('sp', xres[:], I["xin"].t.rearrange("(t p) d -> p t d", p=128), xres, reads=[I["xin"]], writes=[xres])
        adabT0 = kb.sb("adabT0", [128, 48], F32)
        kb.dma('sp', adabT0[:], I["adabT0"][:], adabT0, reads=[I["adabT0"]], writes=[adabT0])
        adabT1 = kb.sb("adabT1", [128, 48], F32)
        kb.dma('sp', adabT1[:], I["adabT1"][:], adabT1, reads=[I["adabT1"]], writes=[adabT1])
        kb.push_scope()
        brow = kb.sb("brow", [128, 1024], F32)
        kb.dma('sp', brow[:], I["brow_g1_0"][:], brow, reads=[I["brow_g1_0"]], writes=[brow])
        g1row = mod_row(kb, cx, I["adaw0"], 2048, brow, "g1row")
        oT_sb = kb.sb("oT_sb", [128, 8, TOK], BF16)
        for h in range(8):
            kb.igather(oT_sb[:, h, :], I["ag_out"][:, :], idx_o[:, h:h + 1], oT_sb, reads=[I["ag_out"], idx_o],
                       writes=([oT_sb] if h == 0 else []), pwrites=([] if h == 0 else [oT_sb]))
        emit_wo(kb, cx, xres, oT_sb, I["wo0"], g1row)
        kb.pop_scope()
        if stage == 2:
            kb.dma('sp', out.t.rearrange("(t p) d -> p t d", p=128), xres[:], xres, reads=[xres], writes=[out])
            kb.finish([out]); return nc
        emit_moe_layer(kb, cx, xres, I["adaw0"], adabT0, I["brow_g2_0"], I["gffnT0"], I["wr"], I["rb"], I["wg0"], I["wu0"], I["wd0"], True)
        kb.push_scope()
        S_T, SH_T = mod_S(kb, cx, I["adaw1"], adabT1, 0, 1024, 0, 8, I["gmixT1"], "l1a")
        hT_all = kb.sb("hT_all", [128, 8, TOK], BF16)
        kb.push_scope()
        emit_hT(kb, cx, xres, S_T, SH_T, hT_all)
        kb.pop_scope()
        def after_kv():
            cpown = Buf("cpown")
            kb.all_bufs.append(cpown)
            kb.dma('sp', I["agk_in"].t.rearrange("p (h t) -> p h t", t=512), I["kT1"].t[:, :, TOK - 512:], cpown, reads=[I["kT1"]], writes=[I["agk_in"]])
            kb.dma('sp', I["agv_in"].t[:, :], I["V1"].t[TOK - 512:, :], cpown, reads=[I["V1"]], writes=[I["agv_in"]])
            kb.collective_allgather(I["agk_in"], I["agk_out"], GROUPS, dummy)
            kb.collective_allgather(I["agv_in"], I["agv_out"], GROUPS, dummy)
        emit_qkv1(kb, cx, hT_all, I["wqkv1"], I["gainq"], I["gaink"], I["qT1"], I["kT1"], I["V1"], after_kv)
        kb.pop_scope()
        if stage == 3:
            kb.dma('sp', out.t.rearrange("(t p) d -> p t d", p=128), xres[:], xres, reads=[xres], writes=[out])
            kb.finish([out]); return nc
        kb.push_scope()
        kst = kb.sb("kst", [128, 4096], BF16)
        vst = kb.sb("vhst", [128, 4, D], BF16)
        kb.igather(kst[:, :], I["agk_out"][:, :], idx_k[:, 0:1], kst, reads=[I["agk_out"], idx_k], writes=[kst])
        for a in range(4):
            kb.igather(vst[:, a, :], I["agv_out"][:, :], idx_v[:, a:a + 1], vst, reads=[I["agv_out"], idx_v],
                       writes=([vst] if a == 0 else []), pwrites=([] if a == 0 else [vst]))
        kb.dma('sp', I["kTh"].t.rearrange("p h t -> p (h t)"), kst[:], kst, reads=[kst], writes=[I["kTh"]])
        kb.dma('sp', I["Vh"].t.rearrange("(a p) c -> p a c", p=128), vst[:], vst, reads=[vst], writes=[I["Vh"]])
        kb.pop_scope()
        if stage == 4:
            kb.dma('sp', out.t.rearrange("(t p) d -> p t d", p=128), xres[:], xres, reads=[xres], writes=[out])
            kb.finish([out]); return nc
        IC = {"brow_g1": I["brow_g1_1"], "adaw1": I["adaw1"], "wo": I["wo1"], "hmask": I["hmask"], "bmask": I["bmask"], "qT1": I["qT1"], "kTh": I["kTh"],
              "kT1": I["kT1"], "Vh": I["Vh"], "V1": I["V1"], "relb": I["relb"]}
        emit_attn1(kb, cx, xres, IC)
        emit_moe_layer(kb, cx, xres, I["adaw1"], adabT1, I["brow_g2_1"], I["gffnT1"], I["wr"], I["rb"], I["wg1"], I["wu1"], I["wd1"], True)
        kb.dma('sp', out.t.rearrange("(t p) d -> p t d", p=128), xres[:], xres, reads=[xres], writes=[out])
        kb.finish([out])
    return nc


def kb_slice(Td, ncol):
    return T(Td.t[:, 0:ncol], Td.b)


_NC_CACHE = {}


def kernel(**inputs):
    inp = {k: np.asarray(v) for k, v in inputs.items()}
    cores = list(range(8))
    if "F" not in _NC_CACHE:
        nc = bass.Bass("TRN2", target_bir_lowering=False)
        build_F(nc)
        _NC_CACHE["F"] = nc
    res = run_bass_kernel_spmd(_NC_CACHE["F"], [host_inputs_F(inp, j) for j in cores], core_ids=cores)
    out = np.concatenate([np.asarray(res.results[j]["out"]) for j in cores], axis=0)
    return out.reshape(2, 8192, 1024).astype(np.float32)
```

```python
import numpy as np, math
from contextlib import ExitStack
import ml_dtypes
import concourse.bass as bass
import concourse.mybir as mybir
from concourse.bass_utils import run_bass_kernel_spmd

F32 = mybir.dt.float32
BF16 = mybir.dt.bfloat16
I32 = mybir.dt.int32
ALU = mybir.AluOpType
AF = mybir.ActivationFunctionType
AX = mybir.AxisListType
NPBF = ml_dtypes.bfloat16
EPS = 1e-6


class Buf:
    def __init__(self, name):
        self.name = name
        self.w = {}
        self.r = {}
        self.ds = {}


class T:
    def __init__(self, t, buf):
        self.t = t
        self.b = buf

    def __getitem__(self, k):
        return self.t[k]


class Eng:
    def __init__(self, name, h):
        self.name = name
        self.h = h
        self.sem = None
        self.cnt = 0
        self.waited = {}


class KB:
    def __init__(self, nc, es):
        self.nc = nc
        self.es = es
        self.root = es
        self.scopes = []
        self.E = {n: Eng(n, h) for n, h in [('pe', nc.tensor), ('act', nc.scalar), ('dve', nc.vector),
                                            ('pool', nc.gpsimd), ('sp', nc.sync)]}
        self.nsem = 0
        self.ntens = 0
        self.all_bufs = []
        self.free_sems = {'hw': [], 'sw': []}

    def newsem(self, nm):
        self.nsem += 1
        return self.root.enter_context(self.nc.semaphore(f"s_{nm}{self.nsem}"))

    def push_scope(self):
        st = ExitStack()
        self.scopes.append((self.es, len(self.all_bufs)))
        self.es = st
        return st

    def pop_scope(self):
        self.barrier()
        self.es.close()
        self.es, nb = self.scopes.pop()
        for b in self.all_bufs[nb:]:
            for kind, (dsem, dcnt) in b.ds.items():
                self.free_sems[kind].append((dsem, dcnt))
            b.ds = {}
        del self.all_bufs[nb:]

    def collective_allgather(self, in_T, out_T, groups, dummy, partial=False):
        e = self.E['pool']
        rb, wb = self._bufs([in_T]), self._bufs([out_T])
        self._wait(e, self._deps(rb, [], wb) if partial else self._deps(rb, wb, []))
        sem = self.newsem('cc')
        e.h.collective_compute("AllGather", ALU.bypass, replica_groups=groups, ins=[in_T[:]], outs=[out_T[:]]).then_inc(sem)
        e.h.wait_ge(sem, 1)
        if partial:
            self.op('pool', lambda g: g.memset(dummy[:], 0.0), reads=[in_T], writes=[dummy], pwrites=[out_T])
        else:
            self.op('pool', lambda g: g.memset(dummy[:], 0.0), reads=[in_T], writes=[out_T, dummy])

    def igather(self, out, in_, idx_ap, sem_owner, reads=(), writes=(), pwrites=()):
        e = self.E['pool']
        reads, writes, pwrites = self._bufs(reads), self._bufs(writes), self._bufs(pwrites)
        self._wait(e, self._deps(reads, writes, pwrites))
        b = sem_owner.b if isinstance(sem_owner, T) else sem_owner
        ent = self._dsem(b, 'sw')
        inst = e.h.indirect_dma_start(out=out, out_offset=None, in_=in_, in_offset=bass.IndirectOffsetOnAxis(ap=idx_ap, axis=0))
        ent[1] += 16
        inst.then_inc(ent[0], 16)
        self._commit((ent[0], ent[1], 'dma'), reads, writes, pwrites)
        return inst

    def _dsem(self, b, kind):
        if kind not in b.ds:
            if self.free_sems[kind]:
                sem, cnt = self.free_sems[kind].pop()
            else:
                sem, cnt = self.newsem('d' + kind), 0
            b.ds[kind] = [sem, cnt]
        return b.ds[kind]

    def barrier(self):
        toks = {}
        for en, ee in self.E.items():
            if ee.sem is not None and ee.cnt > 0:
                toks[id(ee.sem)] = (ee.sem, ee.cnt, 'bar')
        for b in self.all_bufs:
            for kind, (dsem, dcnt) in b.ds.items():
                if dcnt > 0:
                    toks[id(dsem)] = (dsem, dcnt, 'bar')
        for en, e in self.E.items():
            for k, (sem, val, src) in toks.items():
                if e.sem is not None and k == id(e.sem):
                    continue
                if e.waited.get(k, 0) >= val:
                    continue
                e.h.wait_ge(sem, val)
                e.waited[k] = val

    def sb(self, name, shape, dt):
        self.ntens += 1
        t = self.es.enter_context(self.nc.sbuf_tensor(f"{name}_{self.ntens}", list(shape), dt))
        b = Buf(name)
        self.all_bufs.append(b)
        return T(t, b)

    def ps(self, name, shape, dt):
        self.ntens += 1
        t = self.es.enter_context(self.nc.psum_tensor(f"{name}_{self.ntens}", list(shape), dt))
        b = Buf(name)
        self.all_bufs.append(b)
        return T(t, b)

    def dram(self, name, shape, dt, kind):
        t = self.nc.dram_tensor(name, list(shape), dt, kind=kind)
        b = Buf(name)
        self.all_bufs.append(b)
        return T(t.ap(), b)

    @staticmethod
    def _bufs(lst):
        return [x.b if isinstance(x, T) else x for x in lst]

    def _deps(self, reads, writes, pwrites):
        deps = {}

        def add(d):
            for k, v in d.items():
                if k not in deps or deps[k][1] < v[1]:
                    deps[k] = v
        for b in reads:
            add(b.w)
        for b in writes:
            add(b.w)
            add(b.r)
        for b in pwrites:
            add(b.r)
        return deps

    def _wait(self, e, deps):
        for k, (sem, val, src) in deps.items():
            if src == 'pe' and e.name == 'pe':
                continue
            if e.waited.get(k, 0) >= val:
                continue
            e.h.wait_ge(sem, val)
            e.waited[k] = val

    def _commit(self, tok, reads, writes, pwrites):
        k = id(tok[0])
        for b in writes:
            b.w = {k: tok}
            b.r = {}
        for b in pwrites:
            if k not in b.w or b.w[k][1] < tok[1]:
                b.w[k] = tok
        for b in reads:
            if k not in b.r or b.r[k][1] < tok[1]:
                b.r[k] = tok

    def op(self, en, fn, reads=(), writes=(), pwrites=()):
        e = self.E[en]
        reads, writes, pwrites = self._bufs(reads), self._bufs(writes), self._bufs(pwrites)
        self._wait(e, self._deps(reads, writes, pwrites))
        if e.sem is None or e.cnt >= 32000:
            e.sem = self.newsem(en)
            e.cnt = 0
        inst = fn(e.h)
        e.cnt += 1
        inst.then_inc(e.sem, 1)
        self._commit((e.sem, e.cnt, en), reads, writes, pwrites)
        return inst

    def dma(self, qn, out, in_, sem_owner, reads=(), writes=(), pwrites=(), **kw):
        e = self.E[qn]
        reads, writes, pwrites = self._bufs(reads), self._bufs(writes), self._bufs(pwrites)
        self._wait(e, self._deps(reads, writes, pwrites))
        b = sem_owner.b if isinstance(sem_owner, T) else sem_owner
        ent = self._dsem(b, 'sw' if qn == 'pool' else 'hw')
        inst = e.h.dma_start(out=out, in_=in_, **kw)
        ent[1] += 16
        inst.then_inc(ent[0], 16)
        self._commit((ent[0], ent[1], 'dma'), reads, writes, pwrites)
        return inst

    def finish(self, out_bufs):
        e = self.E['sp']
        deps = {}
        for b in self._bufs(out_bufs):
            for k, v in b.w.items():
                deps[k] = v
        self._wait(e, deps)
        for en, ee in self.E.items():
            if ee.sem is not None and ee.cnt > 0:
                k = id(ee.sem)
                if e.waited.get(k, 0) < ee.cnt:
                    e.h.wait_ge(ee.sem, ee.cnt)
                    e.waited[k] = ee.cnt
        for b in self.all_bufs:
            for kind, (dsem, dcnt) in b.ds.items():
                k = id(dsem)
                if dcnt > 0 and e.waited.get(k, 0) < dcnt:
                    e.h.wait_ge(dsem, dcnt)
                    e.waited[k] = dcnt


S = 8192; D = 1024; NB = 2
LAMBDA_INIT0 = 0.8 - 0.6 * math.exp(-0.3 * 0)
NEAR_DELTAS = [-640 + 128 * i for i in range(9)]


def t5_bucket_np(rel):
    nb = 16
    ret = np.where(rel > 0, nb, 0)
    n = np.abs(rel)
    max_exact = 8
    nf = np.maximum(n, 1).astype(np.float32)
    large = max_exact + (np.log(nf / np.float32(max_exact)) / np.float32(math.log(1024 / max_exact))
                         * np.float32(nb - max_exact)).astype(np.int32)
    large = np.minimum(large, nb - 1)
    return ret + np.where(n < max_exact, n, large)


def host_inputs_A(inp, h):
    f32 = np.float32
    d = {}
    d["x"] = np.ascontiguousarray(inp["x"].reshape(NB * S, D))
    c = inp["c"]
    d["cT"] = np.ascontiguousarray(c.reshape(NB, 8, 128).transpose(2, 1, 0))
    d["adaw"] = np.ascontiguousarray(inp["ada_w"][0][:, 0:2048])
    d["adabT"] = np.ascontiguousarray(inp["ada_b"][0][0:2048].reshape(16, 128).T)
    d["gT"] = np.ascontiguousarray(inp["norm_mix_g"][0].reshape(8, 128).T)
    w = inp["a_w_qkv"][0]
    d["w"] = np.ascontiguousarray(np.concatenate([w[:, h * 128:(h + 1) * 128], w[:, 1024 + h * 128:1024 + (h + 1) * 128],
                                                  w[:, 2048 + h * 128:2048 + (h + 1) * 128]], axis=1))
    qg = inp["a_q_gain"][0]; kg = inp["a_k_gain"][0]
    d["gain"] = np.ascontiguousarray(np.broadcast_to(np.concatenate([qg, qg, kg, kg])[None, :], (128, 256))).astype(f32)
    d["lam"] = np.ascontiguousarray(np.broadcast_to(inp["a_lambda"][0].reshape(1, 256), (128, 256))).astype(f32)
    d["subg"] = np.ascontiguousarray(np.broadcast_to(inp["a_subln_g"][0][None, :], (128, 128))).astype(f32)
    kk = np.arange(128)[:, None]; qq = np.arange(512)[None, :]
    t5 = inp["t5_bias"]
    bt = np.stack([t5[t5_bucket_np(dl + kk - qq), h] for dl in NEAR_DELTAS], axis=1)
    d["biasT"] = np.ascontiguousarray(bt).astype(f32)
    d["c15"] = np.full((128, 1), t5[15, h], f32)
    mk = np.stack([np.where(((dl + kk) // 64) <= (qq // 64), 0.0, -30000.0) for dl in (0, 128, 256, 384)], axis=1)
    d["maskT"] = np.ascontiguousarray(mk).astype(f32)
    d["idb"] = np.eye(128).astype(NPBF)
    return d


def build_A(nc, nb_run=NB, nqg_run=16):
    es = ExitStack()
    kb = KB(nc, es)
    x = kb.dram("x", [NB * S, D], F32, "ExternalInput")
    cT = kb.dram("cT", [128, 8, 2], F32, "ExternalInput")
    adaw = kb.dram("adaw", [1024, 2048], F32, "ExternalInput")
    adabT = kb.dram("adabT", [128, 16], F32, "ExternalInput")
    gT = kb.dram("gT", [128, 8], F32, "ExternalInput")
    w = kb.dram("w", [1024, 384], F32, "ExternalInput")
    gain = kb.dram("gain", [128, 256], F32, "ExternalInput")
    lam = kb.dram("lam", [128, 256], F32, "ExternalInput")
    subg = kb.dram("subg", [128, 128], F32, "ExternalInput")
    biasT = kb.dram("biasT", [128, 9, 512], F32, "ExternalInput")
    c15 = kb.dram("c15", [128, 1], F32, "ExternalInput")
    maskT = kb.dram("maskT", [128, 4, 512], F32, "ExternalInput")
    idb = kb.dram("idb", [128, 128], BF16, "ExternalInput")
    oT = kb.dram("oT", [128, NB * S], BF16, "ExternalOutput")
    with es:
        emit_A(kb, x, cT, adaw, adabT, gT, w, gain, lam, subg, biasT, c15, maskT, idb, oT, nb_run, nqg_run)
        kb.finish([oT])
    return nc


def emit_A(kb, x, cT, adaw, adabT, gT, w, gain, lam, subg, biasT, c15, maskT, idb, oT, nb_run=NB, nqg_run=16):
    ident = kb.sb("ident", [128, 128], BF16)
    kb.dma('sp', ident[:], idb[:], ident, reads=[idb], writes=[ident])
    epsb = kb.sb("epsb", [128, 1], F32)
    kb.op('dve', lambda e: e.memset(epsb[:], EPS), writes=[epsb])
    cT_sb = kb.sb("cT_sb", [128, 8, 2], F32)
    kb.dma('sp', cT_sb[:], cT[:], cT_sb, reads=[cT], writes=[cT_sb])
    siluT = kb.sb("siluT", [128, 8, 2], F32)
    kb.op('act', lambda e: e.activation(out=siluT[:], in_=cT_sb[:], func=AF.Silu), reads=[cT_sb], writes=[siluT])
    adabT_sb = kb.sb("adabT_sb", [128, 16], F32)
    kb.dma('sp', adabT_sb[:], adabT[:], adabT_sb, reads=[adabT], writes=[adabT_sb])
    gT_sb = kb.sb("gT_sb", [128, 8], F32)
    kb.dma('sp', gT_sb[:], gT[:], gT_sb, reads=[gT], writes=[gT_sb])
    w_sb = kb.sb("w_sb", [128, 8, 384], F32)
    kb.dma('sp', w_sb[:], w.t.rearrange("(kc p) n -> p kc n", p=128), w_sb, reads=[w], writes=[w_sb])
    gain_sb = kb.sb("gain_sb", [128, 256], F32)
    kb.dma('sp', gain_sb[:], gain[:], gain_sb, reads=[gain], writes=[gain_sb])
    kb.op('dve', lambda e: e.tensor_scalar(out=gain_sb[:, 0:128], in0=gain_sb[:, 0:128], scalar1=0.125, scalar2=None, op0=ALU.mult),
          reads=[gain_sb], writes=[gain_sb])
    lam_sb = kb.sb("lam_sb", [128, 256], F32)
    kb.dma('sp', lam_sb[:], lam[:], lam_sb, reads=[lam], writes=[lam_sb])
    subg_sb = kb.sb("subg_sb", [128, 128], F32)
    kb.dma('sp', subg_sb[:], subg[:], subg_sb, reads=[subg], writes=[subg_sb])
    kb.op('dve', lambda e: e.tensor_scalar(out=subg_sb[:], in0=subg_sb[:], scalar1=1.0 - LAMBDA_INIT0, scalar2=None, op0=ALU.mult),
          reads=[subg_sb], writes=[subg_sb])
    bias_sb = kb.sb("bias_sb", [128, 9, 512], F32)
    kb.dma('sp', bias_sb[:], biasT[:], bias_sb, reads=[biasT], writes=[bias_sb])
    mask_sb = kb.sb("mask_sb", [128, 4, 512], F32)
    kb.dma('sp', mask_sb[:], maskT[:], mask_sb, reads=[maskT], writes=[mask_sb])
    kb.op('dve', lambda e: e.tensor_tensor(out=bias_sb[:, 5:9, :], in0=bias_sb[:, 5:9, :], in1=mask_sb[:], op=ALU.add),
          reads=[bias_sb, mask_sb], writes=[bias_sb])
    c15_sb = kb.sb("c15_sb", [128, 1], F32)
    kb.dma('sp', c15_sb[:], c15[:], c15_sb, reads=[c15], writes=[c15_sb])

    PB = [kb.ps(f"pb{i}", [128, 512], F32) for i in range(8)]

    lprod = kb.sb("lprod", [128, 2, 64], F32)
    kb.op('dve', lambda e: e.tensor_tensor(out=lprod[:, 0, :], in0=lam_sb[:, 0:64], in1=lam_sb[:, 64:128], op=ALU.mult), reads=[lam_sb], writes=[lprod])
    kb.op('dve', lambda e: e.tensor_tensor(out=lprod[:, 1, :], in0=lam_sb[:, 128:192], in1=lam_sb[:, 192:256], op=ALU.mult), reads=[lam_sb, lprod], writes=[lprod])
    lsum = kb.sb("lsum", [128, 2], F32)
    kb.op('dve', lambda e: e.tensor_reduce(out=lsum[:], in_=lprod[:], axis=AX.X, op=ALU.add), reads=[lprod], writes=[lsum])
    lexp = kb.sb("lexp", [128, 2], F32)
    kb.op('act', lambda e: e.activation(out=lexp[:], in_=lsum[:], func=AF.Exp), reads=[lsum], writes=[lexp])
    neglam = kb.sb("neglam", [128, 1], F32)
    kb.op('dve', lambda e: e.tensor_tensor(out=neglam[:], in0=lexp[:, 1:2], in1=lexp[:, 0:1], op=ALU.subtract), reads=[lexp], writes=[neglam])
    kb.op('dve', lambda e: e.tensor_scalar(out=neglam[:], in0=neglam[:], scalar1=-LAMBDA_INIT0, scalar2=None, op0=ALU.add), reads=[neglam], writes=[neglam])

    adaw_sb = kb.sb("adaw_sb", [128, 8, 512], F32)
    modT = kb.sb("modT", [128, 16, 2], F32)
    pm = PB[0]
    for g in range(4):
        kb.dma('sp', adaw_sb[:], adaw.t[:, g * 512:(g + 1) * 512].rearrange("(kc p) n -> p kc n", p=128), adaw_sb,
               reads=[adaw], writes=[adaw_sb])
        for jj in range(4):
            j = g * 4 + jj
            for kc in range(8):
                kb.op('pe', lambda e, j=j, jj=jj, kc=kc: e.matmul(pm[:, j * 2:(j + 1) * 2], lhsT=adaw_sb[:, kc, jj * 128:(jj + 1) * 128],
                                                                rhs=siluT[:, kc, :], start=(kc == 0), stop=(kc == 7)),
                      reads=[adaw_sb, siluT], pwrites=[pm])
    kb.op('dve', lambda e: e.tensor_tensor(out=modT[:], in0=pm[:, 0:32].rearrange("p (j b) -> p j b", b=2),
                                           in1=adabT_sb[:].unsqueeze(2).to_broadcast([128, 16, 2]), op=ALU.add),
          reads=[pm, adabT_sb], writes=[modT])
    Ssc = kb.sb("Ssc", [128, 8, 2], F32)
    kb.op('dve', lambda e: e.scalar_tensor_tensor(out=Ssc[:], in0=modT[:, 8:16, :], scalar=1.0, in1=gT_sb[:].unsqueeze(2).to_broadcast([128, 8, 2]),
                                                  op0=ALU.add, op1=ALU.mult), reads=[modT, gT_sb], writes=[Ssc])
    Wb = [kb.sb(f"Wb{b}", [128, 8, 384], BF16) for b in range(NB)]
    for b in range(NB):
        for kc in range(8):
            kb.op('dve', lambda e, b=b, kc=kc: e.tensor_scalar(out=Wb[b][:, kc, :], in0=w_sb[:, kc, :], scalar1=Ssc[:, kc, b:b + 1], scalar2=None, op0=ALU.mult),
                  reads=[w_sb, Ssc], pwrites=[Wb[b]])
    SHrep = kb.sb("SHrep", [128, 8, 128], F32)
    biasbc = [kb.sb(f"biasbc{b}", [128, 384], F32) for b in range(NB)]
    for b in range(NB):
        kb.op('dve', lambda e, b=b: e.tensor_copy(out=SHrep[:], in_=modT[:, 0:8, b:b + 1].to_broadcast([128, 8, 128])), reads=[modT], writes=[SHrep])
        pbias = PB[1]
        for kc in range(8):
            kb.op('pe', lambda e, kc=kc: e.matmul(pbias[:, 0:384], lhsT=SHrep[:, kc, :], rhs=w_sb[:, kc, :], start=(kc == 0), stop=(kc == 7)),
                  reads=[SHrep, w_sb], pwrites=[pbias])
        kb.op('dve', lambda e, b=b: e.tensor_copy(out=biasbc[b][:], in_=pbias[:, 0:384]), reads=[pbias], writes=[biasbc[b]])

    qT = kb.sb("qT", [128, S], BF16)
    kT = kb.sb("kT", [128, S], BF16)
    Vb = kb.sb("Vb", [128, 64, 129], BF16)
    kb.op('pool', lambda e: e.memset(Vb[:, :, 128:129], 1.0), pwrites=[Vb])
    NX = 3
    xbuf = [kb.sb(f"xbuf{i}", [128, 1024], F32) for i in range(NX)]
    junk = kb.sb("junk", [128, 1024], BF16)
    ssb = [kb.sb(f"ss{i}", [128, 1], F32) for i in range(2)]
    rstd = [kb.sb(f"rstd{i}", [128, 1], F32) for i in range(2)]
    xs = [kb.sb(f"xs{i}", [128, 1024], BF16) for i in range(2)]
    xT = [kb.sb(f"xT{i}", [128, 1024], BF16) for i in range(2)]
    qkv = [kb.sb(f"qkv{i}", [128, 384], F32) for i in range(2)]
    sq = [kb.sb(f"sq{i}", [128, 256], F32) for i in range(2)]
    ss4 = [kb.sb(f"ss4{i}", [128, 4], F32) for i in range(2)]
    qkt = [kb.sb(f"qkt{i}", [128, 256], F32) for i in range(2)]
    qkn = [kb.sb(f"qkn{i}", [128, 256], BF16) for i in range(2)]
    Pb = [kb.sb(f"P{i}", [128, 2, 512], BF16) for i in range(3)]
    ostage = [kb.sb(f"ost{i}", [128, 512], BF16) for i in range(2)]
    rr = [kb.sb(f"rr{i}", [128, 3], F32) for i in range(2)]
    t1 = [kb.sb(f"t1{i}", [128, 128], F32) for i in range(2)]
    ob = [kb.sb(f"ob{i}", [128, 128], F32) for i in range(2)]
    oss = [kb.sb(f"oss{i}", [128, 1], F32) for i in range(2)]
    on = [kb.sb(f"on{i}", [128, 128], BF16) for i in range(2)]

    def acc(m, j):
        a = m * 4 + j
        return PB[4 + a // 3], (a % 3) * 129, a

    for b in range(nb_run):
        for t in range(64):
            i2 = t % 2
            xt = xbuf[t % NX]
            r0 = b * S + t * 128
            kb.dma('sp', xt[:], x.t[r0:r0 + 128, :], xt, reads=[x], writes=[xt])
            kb.op('act', lambda e, xt=xt, i2=i2: e.activation(out=junk[:], in_=xt[:], func=AF.Square, accum_out=ssb[i2][:]), reads=[xt], writes=[junk, ssb[i2]])
            kb.op('act', lambda e, i2=i2: e.activation(out=rstd[i2][:], in_=ssb[i2][:], func=AF.Sqrt, scale=1.0 / 1024, bias=epsb[:, 0:1]),
                  reads=[ssb[i2], epsb], writes=[rstd[i2]])
            kb.op('dve', lambda e, i2=i2: e.reciprocal(out=rstd[i2][:], in_=rstd[i2][:]), reads=[rstd[i2]], writes=[rstd[i2]])
            kb.op('dve', lambda e, xt=xt, i2=i2: e.tensor_scalar(out=xs[i2][:], in0=xt[:], scalar1=rstd[i2][:, 0:1], scalar2=None, op0=ALU.mult),
                  reads=[xt, rstd[i2]], writes=[xs[i2]])
            pT = PB[i2]
            pTv = pT[:].bitcast(BF16)
            for kc in range(8):
                kb.op('pe', lambda e, kc=kc, i2=i2, pTv=pTv: e.transpose(out=pTv[:, kc * 128:(kc + 1) * 128], in_=xs[i2][:, kc * 128:(kc + 1) * 128], identity=ident[:]),
                      reads=[xs[i2], ident], pwrites=[pT])
            kb.op('act', lambda e, i2=i2, pTv=pTv: e.copy(out=xT[i2][:], in_=pTv[:, 0:1024]), reads=[pT], writes=[xT[i2]])
            pq = PB[2 + i2]
            for kc in range(8):
                kb.op('pe', lambda e, kc=kc, i2=i2, pq=pq: e.matmul(pq[:, 0:384], lhsT=xT[i2][:, kc * 128:(kc + 1) * 128], rhs=Wb[b][:, kc, :], start=(kc == 0), stop=(kc == 7)),
                      reads=[xT[i2], Wb[b]], pwrites=[pq])
            kb.op('dve', lambda e, i2=i2, pq=pq: e.tensor_tensor(out=qkv[i2][:], in0=pq[:, 0:384], in1=biasbc[b][:], op=ALU.add), reads=[pq, biasbc[b]], writes=[qkv[i2]])
            kb.op('act', lambda e, i2=i2: e.activation(out=sq[i2][:], in_=qkv[i2][:, 0:256], func=AF.Square), reads=[qkv[i2]], writes=[sq[i2]])
            kb.op('dve', lambda e, i2=i2: e.tensor_reduce(out=ss4[i2][:], in_=sq[i2][:].rearrange("p (g d) -> p g d", d=64), axis=AX.X, op=ALU.add),
                  reads=[sq[i2]], writes=[ss4[i2]])
            kb.op('act', lambda e, i2=i2: e.activation(out=ss4[i2][:], in_=ss4[i2][:], func=AF.Sqrt, scale=1.0 / 64, bias=epsb[:, 0:1]),
                  reads=[ss4[i2], epsb], writes=[ss4[i2]])
            kb.op('dve', lambda e, i2=i2: e.reciprocal(out=ss4[i2][:], in_=ss4[i2][:]), reads=[ss4[i2]], writes=[ss4[i2]])
            kb.op('dve', lambda e, i2=i2: e.tensor_tensor(out=qkt[i2][:].rearrange("p (g d) -> p g d", d=64), in0=qkv[i2][:, 0:256].rearrange("p (g d) -> p g d", d=64),
                                                          in1=ss4[i2][:].unsqueeze(2).to_broadcast([128, 4, 64]), op=ALU.mult),
                  reads=[qkv[i2], ss4[i2]], writes=[qkt[i2]])
            kb.op('dve', lambda e, i2=i2: e.tensor_tensor(out=qkn[i2][:], in0=qkt[i2][:], in1=gain_sb[:], op=ALU.mult), reads=[qkt[i2], gain_sb], writes=[qkn[i2]])
            kb.op('act', lambda e, i2=i2, t=t: e.copy(out=Vb[:, t, 0:128], in_=qkv[i2][:, 256:384]), reads=[qkv[i2]], pwrites=[Vb])
            pqk = PB[4 + i2]
            pqkv = pqk[:].bitcast(BF16)
            kb.op('pe', lambda e, i2=i2, pqkv=pqkv: e.transpose(out=pqkv[:, 0:128], in_=qkn[i2][:, 0:128], identity=ident[:]), reads=[qkn[i2], ident], pwrites=[pqk])
            kb.op('pe', lambda e, i2=i2, pqkv=pqkv: e.transpose(out=pqkv[:, 128:256], in_=qkn[i2][:, 128:256], identity=ident[:]), reads=[qkn[i2], ident], pwrites=[pqk])
            kb.op('dve', lambda e, t=t, pqkv=pqkv: e.tensor_copy(out=qT[:, t * 128:(t + 1) * 128], in_=pqkv[:, 0:128]), reads=[pqk], pwrites=[qT])
            kb.op('dve', lambda e, t=t, pqkv=pqkv: e.tensor_copy(out=kT[:, t * 128:(t + 1) * 128], in_=pqkv[:, 128:256]), reads=[pqk], pwrites=[kT])

        for qg in range(nqg_run):
            nkt = 4 * qg + 4
            qbase = qg * 512

            def emit_qk(kt):
                sl = kt % 2
                d = kt * 128 - qbase
                q0 = max(d, 0)
                for m in range(2):
                    Sp = PB[sl * 2 + m]
                    kb.op('pe', lambda e, m=m, Sp=Sp, q0=q0, kt=kt: e.matmul(Sp[:, q0:512], lhsT=kT[m * 64:(m + 1) * 64, kt * 128:(kt + 1) * 128],
                                                                      rhs=qT[m * 64:(m + 1) * 64, qbase + q0:qbase + 512], start=True, stop=True),
                          reads=[kT, qT], writes=[Sp])

            emit_qk(0)
            for kt in range(nkt):
                if kt + 1 < nkt:
                    emit_qk(kt + 1)
                sl = kt % 2
                d = kt * 128 - qbase
                q0 = max(d, 0)
                P = Pb[kt % 3]
                for m in range(2):
                    Sp = PB[sl * 2 + m]
                    if d >= -640:
                        bi = (d + 640) // 128
                        kb.op('dve', lambda e, Sp=Sp, bi=bi, q0=q0: e.tensor_tensor(out=Sp[:, q0:512], in0=Sp[:, q0:512], in1=bias_sb[:, bi, q0:512], op=ALU.add),
                              reads=[Sp, bias_sb], writes=[Sp])
                        kb.op('act', lambda e, Sp=Sp, P=P, m=m, q0=q0: e.activation(out=P[:, m, q0:512], in_=Sp[:, q0:512], func=AF.Exp),
                              reads=[Sp], pwrites=[P])
                    else:
                        kb.op('act', lambda e, Sp=Sp, P=P, m=m, q0=q0: e.activation(out=P[:, m, q0:512], in_=Sp[:, q0:512], func=AF.Exp, bias=c15_sb[:, 0:1]),
                              reads=[Sp, c15_sb], pwrites=[P])
                for m in range(2):
                    for j in range(q0 // 128, 4):
                        bank, off, a = acc(m, j)
                        kb.op('pe', lambda e, bank=bank, off=off, a=a, P=P, m=m, j=j, kt=kt: e.matmul(
                            bank[:, off:off + 129], lhsT=P[:, m, j * 128:(j + 1) * 128], rhs=Vb[:, kt, :],
                            start=(kt == 0 and a % 3 == 0), stop=(kt == 4 * qg + j), skip_group_check=True),
                            reads=[P, Vb], pwrites=[bank])
            ost = ostage[qg % 2]
            for j in range(4):
                i2 = j % 2
                b1, o1, _ = acc(0, j)
                b2, o2, _ = acc(1, j)
                kb.op('dve', lambda e, i2=i2, b1=b1, o1=o1: e.reciprocal(out=rr[i2][:, 0:1], in_=b1[:, o1 + 128:o1 + 129]), reads=[b1], writes=[rr[i2]])
                kb.op('dve', lambda e, i2=i2, b2=b2, o2=o2: e.reciprocal(out=rr[i2][:, 1:2], in_=b2[:, o2 + 128:o2 + 129]), reads=[b2, rr[i2]], writes=[rr[i2]])
                kb.op('dve', lambda e, i2=i2: e.tensor_tensor(out=rr[i2][:, 2:3], in0=rr[i2][:, 1:2], in1=neglam[:], op=ALU.mult), reads=[rr[i2], neglam], writes=[rr[i2]])
                kb.op('dve', lambda e, i2=i2, b1=b1, o1=o1: e.tensor_scalar(out=t1[i2][:], in0=b1[:, o1:o1 + 128], scalar1=rr[i2][:, 0:1], scalar2=None, op0=ALU.mult),
                      reads=[b1, rr[i2]], writes=[t1[i2]])
                kb.op('dve', lambda e, i2=i2, b2=b2, o2=o2: e.scalar_tensor_tensor(out=ob[i2][:], in0=b2[:, o2:o2 + 128], scalar=rr[i2][:, 2:3], in1=t1[i2][:],
                                                                           op0=ALU.mult, op1=ALU.add), reads=[b2, rr[i2], t1[i2]], writes=[ob[i2]])
                kb.op('act', lambda e, i2=i2: e.activation(out=junk[:, 0:128], in_=ob[i2][:], func=AF.Square, accum_out=oss[i2][:]), reads=[ob[i2]], writes=[junk, oss[i2]])
                kb.op('act', lambda e, i2=i2: e.activation(out=oss[i2][:], in_=oss[i2][:], func=AF.Sqrt, scale=1.0 / 128, bias=epsb[:, 0:1]), reads=[oss[i2], epsb], writes=[oss[i2]])
                kb.op('dve', lambda e, i2=i2: e.reciprocal(out=oss[i2][:], in_=oss[i2][:]), reads=[oss[i2]], writes=[oss[i2]])
                kb.op('dve', lambda e, i2=i2: e.scalar_tensor_tensor(out=on[i2][:], in0=ob[i2][:], scalar=oss[i2][:, 0:1], in1=subg_sb[:], op0=ALU.mult, op1=ALU.mult),
                      reads=[ob[i2], oss[i2], subg_sb], writes=[on[i2]])
                pt = PB[7]
                ptv = pt[:].bitcast(BF16)
                kb.op('pe', lambda e, i2=i2, ptv=ptv, j=j: e.transpose(out=ptv[:, j * 128:(j + 1) * 128], in_=on[i2][:], identity=ident[:]), reads=[on[i2], ident], pwrites=[pt])
            kb.op('dve', lambda e, ost=ost: e.tensor_copy(out=ost[:], in_=PB[7][:].bitcast(BF16)[:, 0:512]), reads=[PB[7]], writes=[ost])
            c0 = b * S + qbase
            kb.dma('sp', oT.t[:, c0:c0 + 512], ost[:], ost, reads=[ost], pwrites=[oT])


def emit_A2(kb, cx, x, cT, adaw, adabT, gT, w2, gain, lam, subg, biasT2, c15_2, maskT, ag_ins, after_chunk=None, xTs=None, nb_run=2, nqg_run=16):
    ident = cx.identb
    epsb = cx.epsb
    cT_sb = kb.sb("cT_sb", [128, 8, 2], F32)
    kb.dma('sp', cT_sb[:], cT[:], cT_sb, reads=[cT], writes=[cT_sb])
    siluT = kb.sb("siluT", [128, 8, 2], F32)
    kb.op('act', lambda e: e.activation(out=siluT[:], in_=cT_sb[:], func=AF.Silu), reads=[cT_sb], writes=[siluT])
    adabT_sb = kb.sb("adabT_sb", [128, 16], F32)
    kb.dma('sp', adabT_sb[:], adabT[:], adabT_sb, reads=[adabT], writes=[adabT_sb])
    gT_sb = kb.sb("gT_sb", [128, 8], F32)
    kb.dma('sp', gT_sb[:], gT[:], gT_sb, reads=[gT], writes=[gT_sb])
    w_sb = kb.sb("w_sb", [128, 8, 384], F32)
    gain_sb = kb.sb("gain_sb", [128, 256], F32)
    kb.dma('sp', gain_sb[:], gain[:], gain_sb, reads=[gain], writes=[gain_sb])
    kb.op('dve', lambda e: e.tensor_scalar(out=gain_sb[:, 0:128], in0=gain_sb[:, 0:128], scalar1=0.125, scalar2=None, op0=ALU.mult),
          reads=[gain_sb], writes=[gain_sb])
    lam_sb = kb.sb("lam_sb", [128, 256], F32)
    kb.dma('sp', lam_sb[:], lam[:], lam_sb, reads=[lam], writes=[lam_sb])
    subg_sb = kb.sb("subg_sb", [128, 128], F32)
    kb.dma('sp', subg_sb[:], subg[:], subg_sb, reads=[subg], writes=[subg_sb])
    kb.op('dve', lambda e: e.tensor_scalar(out=subg_sb[:], in0=subg_sb[:], scalar1=1.0 - LAMBDA_INIT0, scalar2=None, op0=ALU.mult),
          reads=[subg_sb], writes=[subg_sb])
    bias_sb = kb.sb("bias_sb", [128, 9, 512], F32)
    mask_sb = kb.sb("mask_sb", [128, 4, 512], F32)
    kb.dma('sp', mask_sb[:], maskT[:], mask_sb, reads=[maskT], writes=[mask_sb])
    c15_sb = kb.sb("c15_sb", [128, 2], F32)
    kb.dma('sp', c15_sb[:], c15_2[:], c15_sb, reads=[c15_2], writes=[c15_sb])
    PB = cx.PB

    lprod = kb.sb("lprod", [128, 2, 64], F32)
    kb.op('dve', lambda e: e.tensor_tensor(out=lprod[:, 0, :], in0=lam_sb[:, 0:64], in1=lam_sb[:, 64:128], op=ALU.mult), reads=[lam_sb], writes=[lprod])
    kb.op('dve', lambda e: e.tensor_tensor(out=lprod[:, 1, :], in0=lam_sb[:, 128:192], in1=lam_sb[:, 192:256], op=ALU.mult), reads=[lam_sb, lprod], writes=[lprod])
    lsum = kb.sb("lsum", [128, 2], F32)
    kb.op('dve', lambda e: e.tensor_reduce(out=lsum[:], in_=lprod[:], axis=AX.X, op=ALU.add), reads=[lprod], writes=[lsum])
    lexp = kb.sb("lexp", [128, 2], F32)
    kb.op('act', lambda e: e.activation(out=lexp[:], in_=lsum[:], func=AF.Exp), reads=[lsum], writes=[lexp])
    neglam = kb.sb("neglam", [128, 1], F32)
    kb.op('dve', lambda e: e.tensor_tensor(out=neglam[:], in0=lexp[:, 1:2], in1=lexp[:, 0:1], op=ALU.subtract), reads=[lexp], writes=[neglam])
    kb.op('dve', lambda e: e.tensor_scalar(out=neglam[:], in0=neglam[:], scalar1=-LAMBDA_INIT0, scalar2=None, op0=ALU.add), reads=[neglam], writes=[neglam])

    adaw_sb = cx.adaw_sb
    modT = kb.sb("modT", [128, 16, 2], F32)
    pm = PB[0]
    for g in range(4):
        kb.dma('sp', adaw_sb[:], adaw.t[:, g * 512:(g + 1) * 512].rearrange("(kc p) n -> p kc n", p=128), adaw_sb,
               reads=[adaw], writes=[adaw_sb])
        for jj in range(4):
            j = g * 4 + jj
            for kc in range(8):
                kb.op('pe', lambda e, j=j, jj=jj, kc=kc: e.matmul(pm[:, j * 2:(j + 1) * 2], lhsT=adaw_sb[:, kc, jj * 128:(jj + 1) * 128],
                                                                rhs=siluT[:, kc, :], start=(kc == 0), stop=(kc == 7)),
                      reads=[adaw_sb, siluT], pwrites=[pm])
    kb.op('dve', lambda e: e.tensor_tensor(out=modT[:], in0=pm[:, 0:32].rearrange("p (j b) -> p j b", b=2),
                                           in1=adabT_sb[:].unsqueeze(2).to_broadcast([128, 16, 2]), op=ALU.add),
          reads=[pm, adabT_sb], writes=[modT])
    Ssc = kb.sb("Ssc", [128, 8, 2], F32)
    kb.op('dve', lambda e: e.scalar_tensor_tensor(out=Ssc[:], in0=modT[:, 8:16, :], scalar=1.0, in1=gT_sb[:].unsqueeze(2).to_broadcast([128, 8, 2]),
                                                  op0=ALU.add, op1=ALU.mult), reads=[modT, gT_sb], writes=[Ssc])
    Wb0 = kb.sb("Wb0", [128, 8, 384], BF16)
    SHrep = kb.sb("SHrep", [128, 8, 128], F32)
    biasbc0 = kb.sb("biasbc0", [128, 384], F32)

    qT = kb.sb("qT", [128, S], BF16)
    kT = kb.sb("kT", [128, S], BF16)
    Vb = kb.sb("Vb", [128, 64, 129], BF16)
    kb.op('pool', lambda e: e.memset(Vb[:, :, 128:129], 1.0), pwrites=[Vb])
    NX = 6
    xbuf = [kb.sb(f"xbuf{i}", [128, 1024], F32) for i in range(NX)]
    junk = cx.junk
    ssb = [kb.sb(f"ss{i}", [128, 1], F32) for i in range(2)]
    rstd = [kb.sb(f"rstd{i}", [128, 1], F32) for i in range(2)]
    xs = [kb.sb(f"xs{i}", [128, 1024], BF16) for i in range(2)]
    xT = [kb.sb(f"xT{i}", [128, 1024], BF16) for i in range(2)]
    qkv = [kb.sb(f"qkv{i}", [128, 384], F32) for i in range(4)]
    sq = [kb.sb(f"sq{i}", [128, 256], F32) for i in range(4)]
    ss4 = [kb.sb(f"ss4{i}", [128, 4], F32) for i in range(4)]
    qkt = [kb.sb(f"qkt{i}", [128, 256], F32) for i in range(4)]
    qkn = [kb.sb(f"qkn{i}", [128, 256], BF16) for i in range(4)]
    Pb = [kb.sb(f"P{i}", [128, 2, 512], BF16) for i in range(3)]
    ostage = [kb.sb(f"ost{i}", [128, 512], BF16) for i in range(2)]
    rr = [kb.sb(f"rr{i}", [128, 3], F32) for i in range(2)]
    t1 = [kb.sb(f"t1{i}", [128, 128], F32) for i in range(2)]
    ob = [kb.sb(f"ob{i}", [128, 128], F32) for i in range(2)]
    oss = [kb.sb(f"oss{i}", [128, 1], F32) for i in range(2)]
    on = [kb.sb(f"on{i}", [128, 128], BF16) for i in range(2)]

    PQB = [2, 3, 6, 7]

    def acc(m, j):
        a = m * 4 + j
        return PB[4 + a // 3], (a % 3) * 129, a

    for b in range(nb_run):
        kb.dma('sp', w_sb[:], w2.t[b].rearrange("(kc p) n -> p kc n", p=128), w_sb, reads=[w2], writes=[w_sb])
        for kc in range(8):
            kb.op('dve', lambda e, b=b, kc=kc: e.tensor_scalar(out=Wb0[:, kc, :], in0=w_sb[:, kc, :], scalar1=Ssc[:, kc, b:b + 1], scalar2=None, op0=ALU.mult),
                  reads=[w_sb, Ssc], writes=([Wb0] if kc == 0 else []), pwrites=([] if kc == 0 else [Wb0]))
        kb.op('dve', lambda e, b=b: e.tensor_copy(out=SHrep[:], in_=modT[:, 0:8, b:b + 1].to_broadcast([128, 8, 128])), reads=[modT], writes=[SHrep])
        pbias = PB[1]
        for kc in range(8):
            kb.op('pe', lambda e, kc=kc: e.matmul(pbias[:, 0:384], lhsT=SHrep[:, kc, :], rhs=w_sb[:, kc, :], start=(kc == 0), stop=(kc == 7)),
                  reads=[SHrep, w_sb], pwrites=[pbias])
        kb.op('dve', lambda e: e.tensor_copy(out=biasbc0[:], in_=pbias[:, 0:384]), reads=[pbias], writes=[biasbc0])
        kb.dma('sp', bias_sb[:], biasT2.t[:, b], bias_sb, reads=[biasT2], writes=[bias_sb])
        kb.op('dve', lambda e: e.tensor_tensor(out=bias_sb[:, 5:9, :], in0=bias_sb[:, 5:9, :], in1=mask_sb[:], op=ALU.add),
              reads=[bias_sb, mask_sb], writes=[bias_sb])
        kb.op('dve', lambda e, b=b: e.tensor_scalar(out=bias_sb[:], in0=bias_sb[:], scalar1=c15_sb[:, b:b + 1], scalar2=None, op0=ALU.subtract),
              reads=[bias_sb, c15_sb], writes=[bias_sb])
        def p1(t):
            i2 = t % 2
            if xTs is not None and b > 0:
                kb.dma('sp', xT[i2][:], xTs.t[t], xT[i2], reads=[xTs], writes=[xT[i2]])
            else:
                xt = xbuf[t % NX]
                r0 = t * 128
                kb.dma('sp', xt[:], x.t[r0:r0 + 128, :], xt, reads=[x], writes=[xt])
                kb.op('act', lambda e, xt=xt, i2=i2: e.activation(out=junk[:], in_=xt[:], func=AF.Square, accum_out=ssb[i2][:]), reads=[xt], writes=[junk, ssb[i2]])
                kb.op('act', lambda e, i2=i2: e.activation(out=rstd[i2][:], in_=ssb[i2][:], func=AF.Sqrt, scale=1.0 / 1024, bias=epsb[:, 0:1]),
                      reads=[ssb[i2], epsb], writes=[rstd[i2]])
                kb.op('dve', lambda e, i2=i2: e.reciprocal(out=rstd[i2][:], in_=rstd[i2][:]), reads=[rstd[i2]], writes=[rstd[i2]])
                kb.op('dve', lambda e, xt=xt, i2=i2: e.tensor_scalar(out=xs[i2][:], in0=xt[:], scalar1=rstd[i2][:, 0:1], scalar2=None, op0=ALU.mult),
                      reads=[xt, rstd[i2]], writes=[xs[i2]])
                pT = PB[i2]
                pTv = pT[:].bitcast(BF16)
                for kc in range(8):
                    kb.op('pe', lambda e, kc=kc, i2=i2, pTv=pTv: e.transpose(out=pTv[:, kc * 128:(kc + 1) * 128], in_=xs[i2][:, kc * 128:(kc + 1) * 128], identity=ident[:]),
                          reads=[xs[i2], ident], pwrites=[pT])
                kb.op('act', lambda e, i2=i2, pTv=pTv: e.copy(out=xT[i2][:], in_=pTv[:, 0:1024]), reads=[pT], writes=[xT[i2]])
                if xTs is not None:
                    kb.dma('pool', xTs.t[t], xT[i2][:], xT[i2], reads=[xT[i2]], pwrites=[xTs])
            pq = PB[PQB[t % 4]]
            for kc in range(8):
                kb.op('pe', lambda e, kc=kc, i2=i2, pq=pq: e.matmul(pq[:, 0:384], lhsT=xT[i2][:, kc * 128:(kc + 1) * 128], rhs=Wb0[:, kc, :], start=(kc == 0), stop=(kc == 7)),
                      reads=[xT[i2], Wb0], pwrites=[pq])

        def p2_stages():
            st = []

            def S(fn):
                st.append(fn)
            S(lambda t: kb.op('dve', lambda e, i=t % 4, pq=PB[PQB[t % 4]]: e.tensor_tensor(out=qkv[i][:], in0=pq[:, 0:384], in1=biasbc0[:], op=ALU.add),
                              reads=[PB[PQB[t % 4]], biasbc0], writes=[qkv[t % 4]]))
            S(lambda t: kb.op('act', lambda e, i=t % 4: e.activation(out=sq[i][:], in_=qkv[i][:, 0:256], func=AF.Square), reads=[qkv[t % 4]], writes=[sq[t % 4]]))
            S(lambda t: kb.op('dve', lambda e, i=t % 4: e.tensor_reduce(out=ss4[i][:], in_=sq[i][:].rearrange("p (g d) -> p g d", d=64), axis=AX.X, op=ALU.add),
                              reads=[sq[t % 4]], writes=[ss4[t % 4]]))
            S(lambda t: kb.op('act', lambda e, i=t % 4: e.activation(out=ss4[i][:], in_=ss4[i][:], func=AF.Sqrt, scale=1.0 / 64, bias=epsb[:, 0:1]),
                              reads=[ss4[t % 4], epsb], writes=[ss4[t % 4]]))
            S(lambda t: kb.op('dve', lambda e, i=t % 4: e.reciprocal(out=ss4[i][:], in_=ss4[i][:]), reads=[ss4[t % 4]], writes=[ss4[t % 4]]))
            S(lambda t: kb.op('dve', lambda e, i=t % 4: e.tensor_tensor(out=qkt[i][:].rearrange("p (g d) -> p g d", d=64), in0=qkv[i][:, 0:256].rearrange("p (g d) -> p g d", d=64),
                                                                  in1=ss4[i][:].unsqueeze(2).to_broadcast([128, 4, 64]), op=ALU.mult),
                              reads=[qkv[t % 4], ss4[t % 4]], writes=[qkt[t % 4]]))
            S(lambda t: kb.op('dve', lambda e, i=t % 4: e.tensor_tensor(out=qkn[i][:], in0=qkt[i][:], in1=gain_sb[:], op=ALU.mult), reads=[qkt[t % 4], gain_sb], writes=[qkn[t % 4]]))
            S(lambda t: kb.op('act', lambda e, i=t % 4, t=t: e.copy(out=Vb[:, t, 0:128], in_=qkv[i][:, 256:384]), reads=[qkv[t % 4]], pwrites=[Vb]))

            def tr(t):
                i = t % 4
                pqk = PB[4 + i // 2]
                pqkv = pqk[:].bitcast(BF16)
                c0 = (i % 2) * 256
                kb.op('pe', lambda e: e.transpose(out=pqkv[:, c0:c0 + 128], in_=qkn[i][:, 0:128], identity=ident[:]), reads=[qkn[i], ident], pwrites=[pqk])
                kb.op('pe', lambda e: e.transpose(out=pqkv[:, c0 + 128:c0 + 256], in_=qkn[i][:, 128:256], identity=ident[:]), reads=[qkn[i], ident], pwrites=[pqk])
            S(tr)

            def cp(t):
                i = t % 4
                pqk = PB[4 + i // 2]
                pqkv = pqk[:].bitcast(BF16)
                c0 = (i % 2) * 256
                kb.op('dve', lambda e: e.tensor_copy(out=qT[:, t * 128:(t + 1) * 128], in_=pqkv[:, c0:c0 + 128]), reads=[pqk], pwrites=[qT])
                kb.op('dve', lambda e: e.tensor_copy(out=kT[:, t * 128:(t + 1) * 128], in_=pqkv[:, c0 + 128:c0 + 256]), reads=[pqk], pwrites=[kT])
            S(cp)
            return st

        stages = p2_stages()
        G = 4
        for t in range(G):
            p1(t)
        for g in range(64 // G):
            tiles = list(range(g * G, (g + 1) * G))
            for t in tiles:
                stages[0](t)
            if g + 1 < 64 // G:
                for t in range((g + 1) * G, (g + 2) * G):
                    p1(t)
            for stg_ in stages[1:]:
                for t in tiles:
                    stg_(t)

        for qg in range(nqg_run):
            nkt = 4 * qg + 4
            qbase = qg * 512

            def emit_qk(kt):
                sl = kt % 2
                d = kt * 128 - qbase
                q0 = max(d, 0)
                for m in range(2):
                    Sp = PB[sl * 2 + m]
                    kb.op('pe', lambda e, m=m, Sp=Sp, q0=q0, kt=kt: e.matmul(Sp[:, q0:512], lhsT=kT[m * 64:(m + 1) * 64, kt * 128:(kt + 1) * 128],
                                                                      rhs=qT[m * 64:(m + 1) * 64, qbase + q0:qbase + 512], start=True, stop=True),
                          reads=[kT, qT], writes=[Sp])

            emit_qk(0)
            for kt in range(nkt):
                if kt + 1 < nkt:
                    emit_qk(kt + 1)
                sl = kt % 2
                d = kt * 128 - qbase
                q0 = max(d, 0)
                P = Pb[kt % 3]
                for m in range(2):
                    Sp = PB[sl * 2 + m]
                    if d >= -640:
                        bi = (d + 640) // 128
                        kb.op('dve', lambda e, Sp=Sp, bi=bi, q0=q0: e.tensor_tensor(out=Sp[:, q0:512], in0=Sp[:, q0:512], in1=bias_sb[:, bi, q0:512], op=ALU.add),
                              reads=[Sp, bias_sb], writes=[Sp])
                        kb.op('act', lambda e, Sp=Sp, P=P, m=m, q0=q0: e.activation(out=P[:, m, q0:512], in_=Sp[:, q0:512], func=AF.Exp),
                              reads=[Sp], pwrites=[P])
                    else:
                        kb.op('act', lambda e, Sp=Sp, P=P, m=m, q0=q0: e.activation(out=P[:, m, q0:512], in_=Sp[:, q0:512], func=AF.Exp),
                              reads=[Sp], pwrites=[P])
                for m in range(2):
                    for j in range(q0 // 128, 4):
                        bank, off, a = acc(m, j)
                        kb.op('pe', lambda e, bank=bank, off=off, a=a, P=P, m=m, j=j, kt=kt: e.matmul(
                            bank[:, off:off + 129], lhsT=P[:, m, j * 128:(j + 1) * 128], rhs=Vb[:, kt, :],
                            start=(kt == 0 and a % 3 == 0), stop=(kt == 4 * qg + j), skip_group_check=True),
                            reads=[P, Vb], pwrites=[bank])
            ost = ostage[qg % 2]
            for j in range(4):
                i2 = j % 2
                b1, o1, _ = acc(0, j)
                b2, o2, _ = acc(1, j)
                kb.op('dve', lambda e, i2=i2, b1=b1, o1=o1: e.reciprocal(out=rr[i2][:, 0:1], in_=b1[:, o1 + 128:o1 + 129]), reads=[b1], writes=[rr[i2]])
                kb.op('dve', lambda e, i2=i2, b2=b2, o2=o2: e.reciprocal(out=rr[i2][:, 1:2], in_=b2[:, o2 + 128:o2 + 129]), reads=[b2, rr[i2]], writes=[rr[i2]])
                kb.op('dve', lambda e, i2=i2: e.tensor_tensor(out=rr[i2][:, 2:3], in0=rr[i2][:, 1:2], in1=neglam[:], op=ALU.mult), reads=[rr[i2], neglam], writes=[rr[i2]])
                kb.op('dve', lambda e, i2=i2, b1=b1, o1=o1: e.tensor_scalar(out=t1[i2][:], in0=b1[:, o1:o1 + 128], scalar1=rr[i2][:, 0:1], scalar2=None, op0=ALU.mult),
                      reads=[b1, rr[i2]], writes=[t1[i2]])
                kb.op('dve', lambda e, i2=i2, b2=b2, o2=o2: e.scalar_tensor_tensor(out=ob[i2][:], in0=b2[:, o2:o2 + 128], scalar=rr[i2][:, 2:3], in1=t1[i2][:],
                                                                           op0=ALU.mult, op1=ALU.add), reads=[b2, rr[i2], t1[i2]], writes=[ob[i2]])
                kb.op('act', lambda e, i2=i2: e.activation(out=junk[:, 0:128], in_=ob[i2][:], func=AF.Square, accum_out=oss[i2][:]), reads=[ob[i2]], writes=[junk, oss[i2]])
                kb.op('act', lambda e, i2=i2: e.activation(out=oss[i2][:], in_=oss[i2][:], func=AF.Sqrt, scale=1.0 / 128, bias=epsb[:, 0:1]), reads=[oss[i2], epsb], writes=[oss[i2]])
                kb.op('dve', lambda e, i2=i2: e.reciprocal(out=oss[i2][:], in_=oss[i2][:]), reads=[oss[i2]], writes=[oss[i2]])
                kb.op('dve', lambda e, i2=i2: e.scalar_tensor_tensor(out=on[i2][:], in0=ob[i2][:], scalar=oss[i2][:, 0:1], in1=subg_sb[:], op0=ALU.mult, op1=ALU.mult),
                      reads=[ob[i2], oss[i2], subg_sb], writes=[on[i2]])
                pt = PB[7]
                ptv = pt[:].bitcast(BF16)
                kb.op('pe', lambda e, i2=i2, ptv=ptv, j=j: e.transpose(out=ptv[:, j * 128:(j + 1) * 128], in_=on[i2][:], identity=ident[:]), reads=[on[i2], ident], pwrites=[pt])
            kb.op('dve', lambda e, ost=ost: e.tensor_copy(out=ost[:], in_=PB[7][:].bitcast(BF16)[:, 0:512]), reads=[PB[7]], writes=[ost])
            kb.dma('sp', ag_ins[qg // 4].t[b, :, (qg % 4) * 512:(qg % 4) * 512 + 512], ost[:], ost, reads=[ost], pwrites=[ag_ins[qg // 4]])
            if after_chunk is not None and b == nb_run - 1 and qg % 4 == 3:
                after_chunk(qg // 4)


NT = 16
TOK = 2048
D = 1024
NE = 16
FF = 512


class Ctx:
    pass


def setup_ctx(kb, idf, idb, cT, consts):
    cx = Ctx()
    cx.identf = kb.sb("identf", [128, 128], F32)
    kb.dma('sp', cx.identf[:], idf[:], cx.identf, reads=[idf], writes=[cx.identf])
    cx.identb = kb.sb("identb", [128, 128], BF16)
    kb.dma('sp', cx.identb[:], idb[:], cx.identb, reads=[idb], writes=[cx.identb])
    cx.epsb = kb.sb("epsb", [128, 1], F32)
    kb.op('dve', lambda e: e.memset(cx.epsb[:], EPS), writes=[cx.epsb])
    cx.consts = kb.sb("consts", [128, 20], F32)
    kb.dma('sp', cx.consts[:], consts[:], cx.consts, reads=[consts], writes=[cx.consts])
    cT_sb = kb.sb("cT_sb", [128, 8, 1], F32)
    kb.dma('sp', cT_sb[:], cT[:], cT_sb, reads=[cT], writes=[cT_sb])
    cx.siluT = kb.sb("siluT", [128, 8, 1], F32)
    kb.op('act', lambda e: e.activation(out=cx.siluT[:], in_=cT_sb[:], func=AF.Silu), reads=[cT_sb], writes=[cx.siluT])
    cx.silubc = kb.sb("silubc", [128, 8, 128], F32)
    kb.op('dve', lambda e: e.tensor_copy(out=cx.silubc[:], in_=cx.siluT[:].to_broadcast([128, 8, 128])), reads=[cx.siluT], writes=[cx.silubc])
    cx.PB = [kb.ps(f"pb{i}", [128, 512], F32) for i in range(8)]
    cx.adaw_sb = kb.sb("adaw_sb", [128, 8, 512], F32)
    cx.junk = kb.sb("junk", [128, 1024], BF16)
    return cx


def mod_T(kb, cx, adaw, col0, nch, adabT_sb, j0, name):
    out = kb.sb(name, [128, nch], F32)
    pm = cx.PB[0]
    done = 0
    while done < nch:
        n = min(4, nch - done)
        c0 = col0 + done * 128
        kb.dma('sp', cx.adaw_sb[:, :, 0:n * 128], adaw.t[:, c0:c0 + n * 128].rearrange("(kc p) n -> p kc n", p=128), cx.adaw_sb,
               reads=[adaw], writes=[cx.adaw_sb])
        for jj in range(n):
            j = done + jj
            for kc in range(8):
                kb.op('pe', lambda e, j=j, jj=jj, kc=kc: e.matmul(pm[:, j:j + 1], lhsT=cx.adaw_sb[:, kc, jj * 128:(jj + 1) * 128],
                                                                rhs=cx.siluT[:, kc, :], start=(kc == 0), stop=(kc == 7)),
                      reads=[cx.adaw_sb, cx.siluT], pwrites=[pm])
        done += n
    kb.op('dve', lambda e: e.tensor_tensor(out=out[:], in0=pm[:, 0:nch], in1=adabT_sb[:, j0:j0 + nch], op=ALU.add), reads=[pm, adabT_sb], writes=[out])
    return out


def mod_row(kb, cx, adaw, col0, brow_sb, name):
    out = kb.sb(name, [128, 1024], F32)
    for hf in range(2):
        c0 = col0 + hf * 512
        kb.dma('sp', cx.adaw_sb[:], adaw.t[:, c0:c0 + 512].rearrange("(kc p) n -> p kc n", p=128), cx.adaw_sb, reads=[adaw], writes=[cx.adaw_sb])
        pm = cx.PB[1]
        for kc in range(8):
            kb.op('pe', lambda e, kc=kc: e.matmul(pm[:, 0:512], lhsT=cx.silubc[:, kc, :], rhs=cx.adaw_sb[:, kc, :], start=(kc == 0), stop=(kc == 7)),
                  reads=[cx.silubc, cx.adaw_sb], pwrites=[pm])
        kb.op('dve', lambda e, hf=hf: e.tensor_tensor(out=out[:, hf * 512:(hf + 1) * 512], in0=pm[:, 0:512], in1=brow_sb[:, hf * 512:(hf + 1) * 512], op=ALU.add),
              reads=[pm, brow_sb], pwrites=[out])
    return out


def emit_hT(kb, cx, xres, S_T, SH_T, hT_all, router=None):
    ss = [kb.sb(f"hss{i}", [128, 1], F32) for i in range(2)]
    xh = [kb.sb(f"xh{i}", [128, 1024], F32) for i in range(2)]
    hTf = [kb.sb(f"hTf{i}", [128, 8, 128], F32) for i in range(2)]
    def h1(t):
        i2 = t % 2
        kb.op('act', lambda e, t=t, i2=i2: e.activation(out=cx.junk[:], in_=xres[:, t, :], func=AF.Square, accum_out=ss[i2][:]), reads=[xres], writes=[cx.junk, ss[i2]])
        kb.op('act', lambda e, i2=i2: e.activation(out=ss[i2][:], in_=ss[i2][:], func=AF.Sqrt, scale=1.0 / 1024, bias=cx.epsb[:, 0:1]), reads=[ss[i2], cx.epsb], writes=[ss[i2]])
        kb.op('dve', lambda e, i2=i2: e.reciprocal(out=ss[i2][:], in_=ss[i2][:]), reads=[ss[i2]], writes=[ss[i2]])
        kb.op('dve', lambda e, t=t, i2=i2: e.tensor_scalar(out=xh[i2][:], in0=xres[:, t, :], scalar1=ss[i2][:, 0:1], scalar2=None, op0=ALU.mult),
              reads=[xres, ss[i2]], writes=[xh[i2]])
        pa, pb = cx.PB[2 + 2 * i2], cx.PB[3 + 2 * i2]
        for kc in range(8):
            pp = pa if kc < 4 else pb
            kb.op('pe', lambda e, kc=kc, pp=pp, i2=i2: e.transpose(out=pp[:, (kc % 4) * 128:(kc % 4 + 1) * 128], in_=xh[i2][:, kc * 128:(kc + 1) * 128], identity=cx.identf[:]),
                  reads=[xh[i2], cx.identf], pwrites=[pp])

    def h2(t):
        i2 = t % 2
        pa, pb = cx.PB[2 + 2 * i2], cx.PB[3 + 2 * i2]
        for kc in range(8):
            pp = pa if kc < 4 else pb
            kb.op('act', lambda e, kc=kc, pp=pp, i2=i2: e.activation(out=hTf[i2][:, kc, :], in_=pp[:, (kc % 4) * 128:(kc % 4 + 1) * 128], func=AF.Identity,
                                                                  scale=S_T[:, kc:kc + 1], bias=SH_T[:, kc:kc + 1]),
                  reads=[pp, S_T, SH_T], pwrites=[hTf[i2]])
        kb.op('dve', lambda e, t=t, i2=i2: e.tensor_copy(out=hT_all[:, :, t * 128:(t + 1) * 128], in_=hTf[i2][:]), reads=[hTf[i2]], pwrites=[hT_all])
        if router is not None:
            wr_sb, logits = router
            pl = cx.PB[6 + i2]
            for kc in range(8):
                kb.op('pe', lambda e, kc=kc, pl=pl, i2=i2: e.matmul(pl[:, 0:16], lhsT=hTf[i2][:, kc, :], rhs=wr_sb[:, kc, :], start=(kc == 0), stop=(kc == 7)),
                      reads=[hTf[i2], wr_sb], pwrites=[pl])
            kb.op('dve', lambda e, t=t, pl=pl: e.tensor_copy(out=logits[:, t, :], in_=pl[:, 0:16]), reads=[pl], pwrites=[logits])
    h1(0)
    for t in range(NT):
        if t + 1 < NT:
            h1(t + 1)
        h2(t)


def emit_routing(kb, cx, logits, rb_sb):
    def tl(name, shape):
        return kb.sb(name, shape, F32)
    n = [0]

    def dv(fn, reads, writes):
        kb.op('dve', fn, reads=reads, writes=writes)
    scores = tl("scores", [128, NT, 16])
    kb.op('act', lambda e: e.activation(out=scores[:], in_=logits[:], func=AF.Sigmoid), reads=[logits], writes=[scores])
    sel = tl("sel", [128, NT, 16])
    dv(lambda e: e.tensor_tensor(out=sel[:], in0=scores[:], in1=rb_sb[:].unsqueeze(1).to_broadcast([128, NT, 16]), op=ALU.add), [scores, rb_sb], [sel])
    s4 = sel[:].rearrange("p t (g k) -> p (t g) k", k=4)
    gs = tl("gs", [128, NT * 4])
    tmp = tl("tmpg", [128, NT * 4])
    pairs = [(0, 1), (0, 2), (0, 3), (1, 2), (1, 3), (2, 3)]
    for idx, (a, b) in enumerate(pairs):
        dst = gs if idx == 0 else tmp
        dv(lambda e, a=a, b=b, dst=dst: e.tensor_tensor(out=dst[:], in0=s4[:, :, a], in1=s4[:, :, b], op=ALU.add), [sel], [dst])
        if idx > 0:
            dv(lambda e: e.tensor_tensor(out=gs[:], in0=gs[:], in1=tmp[:], op=ALU.max), [gs, tmp], [gs])
    gs3 = gs[:].rearrange("p (t g) -> p t g", g=4)
    gmax = tl("gmax", [128, NT])
    dv(lambda e: e.tensor_reduce(out=gmax[:], in_=gs3, axis=AX.X, op=ALU.max), [gs], [gmax])
    eqg = tl("eqg", [128, NT, 4])
    dv(lambda e: e.tensor_tensor(out=eqg[:], in0=gs3, in1=gmax[:].unsqueeze(2).to_broadcast([128, NT, 4]), op=ALU.is_equal), [gs, gmax], [eqg])
    WG = cx.consts[:, 0:4].unsqueeze(1).to_broadcast([128, NT, 4])
    WE = cx.consts[:, 4:20].unsqueeze(1).to_broadcast([128, NT, 16])
    dv(lambda e: e.tensor_tensor(out=eqg[:], in0=eqg[:], in1=WG, op=ALU.mult), [eqg, cx.consts], [eqg])
    wmax = tl("wmax", [128, NT])
    dv(lambda e: e.tensor_reduce(out=wmax[:], in_=eqg[:], axis=AX.X, op=ALU.max), [eqg], [wmax])
    ohg = tl("ohg", [128, NT * 4])
    dv(lambda e: e.tensor_tensor(out=ohg[:].rearrange("p (t g) -> p t g", g=4), in0=eqg[:], in1=wmax[:].unsqueeze(2).to_broadcast([128, NT, 4]), op=ALU.is_equal),
       [eqg, wmax], [ohg])
    gm = tl("gm", [128, NT, 16])
    dv(lambda e: e.tensor_copy(out=gm[:].rearrange("p t (g k) -> p (t g) k", k=4), in_=ohg[:].unsqueeze(2).to_broadcast([128, NT * 4, 4])), [ohg], [gm])
    dv(lambda e: e.tensor_scalar(out=gm[:], in0=gm[:], scalar1=-1.0, scalar2=1e30, op0=ALU.add, op1=ALU.mult), [gm], [gm])
    selm = tl("selm", [128, NT, 16])
    dv(lambda e: e.tensor_tensor(out=selm[:], in0=sel[:], in1=gm[:], op=ALU.add), [sel, gm], [selm])
    ohs = []
    for r in range(2):
        m = tl(f"m{r}", [128, NT])
        dv(lambda e, m=m: e.tensor_reduce(out=m[:], in_=selm[:], axis=AX.X, op=ALU.max), [selm], [m])
        eq = tl(f"eq{r}", [128, NT, 16])
        dv(lambda e, m=m, eq=eq: e.tensor_tensor(out=eq[:], in0=selm[:], in1=m[:].unsqueeze(2).to_broadcast([128, NT, 16]), op=ALU.is_equal), [selm, m], [eq])
        dv(lambda e, eq=eq: e.tensor_tensor(out=eq[:], in0=eq[:], in1=WE, op=ALU.mult), [eq, cx.consts], [eq])
        wm = tl(f"wm{r}", [128, NT])
        dv(lambda e, wm=wm, eq=eq: e.tensor_reduce(out=wm[:], in_=eq[:], axis=AX.X, op=ALU.max), [eq], [wm])
        oh = tl(f"oh{r}", [128, NT, 16])
        dv(lambda e, oh=oh, eq=eq, wm=wm: e.tensor_tensor(out=oh[:], in0=eq[:], in1=wm[:].unsqueeze(2).to_broadcast([128, NT, 16]), op=ALU.is_equal), [eq, wm], [oh])
        ohs.append(oh)
        if r == 0:
            dv(lambda e, oh=oh: e.scalar_tensor_tensor(out=selm[:], in0=oh[:], scalar=-1e30, in1=selm[:], op0=ALU.mult, op1=ALU.add), [oh, selm], [selm])
    sv = []
    for r in range(2):
        pr = tl(f"pr{r}", [128, NT, 16])
        dv(lambda e, pr=pr, r=r: e.tensor_tensor(out=pr[:], in0=scores[:], in1=ohs[r][:], op=ALU.mult), [scores, ohs[r]], [pr])
        s = tl(f"sv{r}", [128, NT])
        dv(lambda e, pr=pr, s=s: e.tensor_reduce(out=s[:], in_=pr[:], axis=AX.X, op=ALU.add), [pr], [s])
        sv.append(s)
    den = tl("den", [128, NT])
    dv(lambda e: e.tensor_tensor(out=den[:], in0=sv[0][:], in1=sv[1][:], op=ALU.add), [sv[0], sv[1]], [den])
    dv(lambda e: e.reciprocal(out=den[:], in_=den[:]), [den], [den])
    for r in range(2):
        dv(lambda e, r=r: e.tensor_tensor(out=sv[r][:], in0=sv[r][:], in1=den[:], op=ALU.mult), [sv[r], den], [sv[r]])
    gates = tl("gates", [128, NT, 16])
    dv(lambda e: e.tensor_tensor(out=gates[:], in0=ohs[0][:], in1=sv[0][:].unsqueeze(2).to_broadcast([128, NT, 16]), op=ALU.mult), [ohs[0], sv[0]], [gates])
    dv(lambda e: e.tensor_tensor(out=ohs[1][:], in0=ohs[1][:], in1=sv[1][:].unsqueeze(2).to_broadcast([128, NT, 16]), op=ALU.mult), [ohs[1], sv[1]], [ohs[1]])
    dv(lambda e: e.tensor_tensor(out=gates[:], in0=gates[:], in1=ohs[1][:], op=ALU.add), [gates, ohs[1]], [gates])
    return gates


def emit_moe_dense(kb, cx, xres, hT_all, gates, wg, wu, wd, g2row):
    Wg = [kb.sb(f"Wg{i}", [128, 8, FF], BF16) for i in range(2)]
    Wu = [kb.sb(f"Wu{i}", [128, 8, FF], BF16) for i in range(2)]
    Wd = [kb.sb(f"Wd{i}", [128, 4, D], BF16) for i in range(2)]
    g2b = kb.sb("g2b", [128, D], BF16)
    kb.op('dve', lambda e: e.tensor_copy(out=g2b[:], in_=g2row[:]), reads=[g2row], writes=[g2b])
    sg = [kb.sb(f"sg{i}", [128, 512], F32) for i in range(2)]
    heT = [kb.sb(f"heT{i}", [128, 4, 512], BF16) for i in range(2)]
    PB = cx.PB
    for e_ in range(NE):
        s = e_ % 2
        kb.dma('pool', Wg[s][:], wg.t[e_].rearrange("(kc p) f -> p kc f", p=128), Wg[s], reads=[wg], writes=[Wg[s]])
        kb.dma('pool', Wu[s][:], wu.t[e_].rearrange("(kc p) f -> p kc f", p=128), Wu[s], reads=[wu], writes=[Wu[s]])
        kb.dma('pool', Wd[s][:], wd.t[e_].rearrange("(fc p) o -> p fc o", p=128), Wd[s], reads=[wd], writes=[Wd[s]])
        kb.op('pool', lambda e, s=s: e.tensor_tensor(out=Wd[s][:], in0=Wd[s][:], in1=g2b[:].unsqueeze(1).to_broadcast([128, 4, D]), op=ALU.mult),
              reads=[Wd[s], g2b], writes=[Wd[s]])
        for tg in range(4):
            hs = heT[tg % 2]
            for fc in range(4):
                pg, pu = PB[(fc % 2) * 2], PB[(fc % 2) * 2 + 1]
                for kc in range(8):
                    kb.op('pe', lambda e, kc=kc, fc=fc, pg=pg, s=s, tg=tg: e.matmul(pg[:, 0:512], lhsT=Wg[s][:, kc, fc * 128:(fc + 1) * 128], rhs=hT_all[:, kc, tg * 512:(tg + 1) * 512],
                                                                               start=(kc == 0), stop=(kc == 7)), reads=[Wg[s], hT_all], pwrites=[pg])
                for kc in range(8):
                    kb.op('pe', lambda e, kc=kc, fc=fc, pu=pu, s=s, tg=tg: e.matmul(pu[:, 0:512], lhsT=Wu[s][:, kc, fc * 128:(fc + 1) * 128], rhs=hT_all[:, kc, tg * 512:(tg + 1) * 512],
                                                                               start=(kc == 0), stop=(kc == 7)), reads=[Wu[s], hT_all], pwrites=[pu])
                sgs = sg[fc % 2]
                kb.op('act', lambda e, pg=pg, sgs=sgs: e.activation(out=sgs[:], in_=pg[:, 0:512], func=AF.Silu), reads=[pg], writes=[sgs])
                kb.op('dve', lambda e, pu=pu, sgs=sgs, hs=hs, fc=fc: e.tensor_tensor(out=hs[:, fc, :], in0=pu[:, 0:512], in1=sgs[:], op=ALU.mult), reads=[pu, sgs], pwrites=[hs])
            for tt in range(4):
                t = tg * 4 + tt
                py = [PB[4 + (tt % 2) * 2], PB[5 + (tt % 2) * 2]]
                for hf in range(2):
                    for fc in range(4):
                        kb.op('pe', lambda e, fc=fc, hf=hf, py=py, hs=hs, tt=tt, s=s: e.matmul(py[hf][:, 0:512], lhsT=hs[:, fc, tt * 128:(tt + 1) * 128], rhs=Wd[s][:, fc, hf * 512:(hf + 1) * 512],
                                                                                         start=(fc == 0), stop=(fc == 3)), reads=[hs, Wd[s]], pwrites=[py[hf]])
                for hf in range(2):
                    kb.op('dve', lambda e, hf=hf, py=py, t=t, e_=e_: e.scalar_tensor_tensor(out=xres[:, t, hf * 512:(hf + 1) * 512], in0=py[hf][:, 0:512], scalar=gates[:, t, e_:e_ + 1],
                                                                                      in1=xres[:, t, hf * 512:(hf + 1) * 512], op0=ALU.mult, op1=ALU.add),
                          reads=[py[hf], gates, xres], pwrites=[xres])


def emit_wo(kb, cx, xres, oT_sb, wo, g1row, nchunk=8):
    Wo = kb.sb("Wo", [128, 8, D], BF16)
    kb.dma('pool', Wo[:], wo.t.rearrange("(c p) o -> p c o", p=128), Wo, reads=[wo], writes=[Wo])
    g1b = kb.sb("g1b", [128, D], BF16)
    kb.op('dve', lambda e: e.tensor_copy(out=g1b[:], in_=g1row[:]), reads=[g1row], writes=[g1b])
    kb.op('pool', lambda e: e.tensor_tensor(out=Wo[:], in0=Wo[:], in1=g1b[:].unsqueeze(1).to_broadcast([128, 8, D]), op=ALU.mult), reads=[Wo, g1b], writes=[Wo])
    for t in range(NT):
        py = [cx.PB[(t % 2) * 2], cx.PB[(t % 2) * 2 + 1]]
        for hf in range(2):
            for c in range(8):
                kb.op('pe', lambda e, c=c, hf=hf, py=py, t=t: e.matmul(py[hf][:, 0:512], lhsT=oT_sb[:, c, t * 128:(t + 1) * 128], rhs=Wo[:, c, hf * 512:(hf + 1) * 512],
                                                                 start=(c == 0), stop=(c == 7)), reads=[oT_sb, Wo], pwrites=[py[hf]])
        for hf in range(2):
            kb.op('dve', lambda e, hf=hf, py=py, t=t: e.tensor_tensor(out=xres[:, t, hf * 512:(hf + 1) * 512], in0=py[hf][:, 0:512], in1=xres[:, t, hf * 512:(hf + 1) * 512], op=ALU.add),
                  reads=[py[hf], xres], pwrites=[xres])


def host_common(inp, j, layer):
    f32 = np.float32
    b = j // 4
    d = {}
    d["cT"] = np.ascontiguousarray(inp["c"][b].reshape(8, 128).T.reshape(128, 8, 1))
    d["idf"] = np.eye(128, dtype=f32)
    d["idb"] = np.eye(128).astype(NPBF)
    d["consts"] = np.ascontiguousarray(np.broadcast_to(np.concatenate([np.arange(4, 0, -1), np.arange(16, 0, -1)]).astype(f32)[None, :], (128, 20)))
    d["wr"] = np.ascontiguousarray(inp["router_w"])
    d["rb"] = np.ascontiguousarray(np.broadcast_to(inp["router_bias"][None, :], (128, 16))).astype(f32)
    return d


def rep(v, n=128):
    return np.ascontiguousarray(np.broadcast_to(np.asarray(v, np.float32).reshape(1, -1), (n, np.asarray(v).size)))


def host_inputs_B(inp, j, oT_all):
    f32 = np.float32
    d = host_common(inp, j, 0)
    r0 = j * TOK
    d["xin"] = np.ascontiguousarray(inp["x"].reshape(-1, D)[r0:r0 + TOK])
    d["oTin"] = np.ascontiguousarray(oT_all[:, :, r0:r0 + TOK])
    d["adaw0"] = np.ascontiguousarray(inp["ada_w"][0])
    d["adaw1"] = np.ascontiguousarray(inp["ada_w"][1][:, 0:2048])
    d["adabT0"] = np.ascontiguousarray(inp["ada_b"][0].reshape(48, 128).T)
    d["adabT1"] = np.ascontiguousarray(inp["ada_b"][1].reshape(48, 128).T)
    d["brow_g1"] = rep(inp["ada_b"][0][2048:3072])
    d["brow_g2"] = rep(inp["ada_b"][0][5120:6144])
    d["gffnT"] = np.ascontiguousarray(inp["norm_ffn_g"][0].reshape(8, 128).T)
    d["gmixT1"] = np.ascontiguousarray(inp["norm_mix_g"][1].reshape(8, 128).T)
    d["wo"] = np.ascontiguousarray(inp["a_w_o"][0])
    d["wg"] = np.ascontiguousarray(inp["moe_w_gate"][0]); d["wu"] = np.ascontiguousarray(inp["moe_w_up"][0]); d["wd"] = np.ascontiguousarray(inp["moe_w_down"][0])
    d["wqkv1"] = np.ascontiguousarray(inp["b_w_qkv"][0])
    d["gainq"] = rep(np.tile(inp["b_q_gain"][0], 8)); d["gaink"] = rep(np.tile(inp["b_k_gain"][0], 8))
    return d


def build_B(nc, do_moe=True):
    es = ExitStack()
    kb = KB(nc, es)
    I = {}
    def din(name, shape, dt=F32):
        I[name] = kb.dram(name, shape, dt, "ExternalInput")
    din("cT", [128, 8, 1]); din("idf", [128, 128]); din("idb", [128, 128], BF16); din("consts", [128, 20]); din("wr", [1024, 16]); din("rb", [128, 16])
    din("xin", [TOK, D]); din("oTin", [8, 128, TOK], BF16); din("adaw0", [1024, 6144]); din("adaw1", [1024, 2048]); din("adabT0", [128, 48]); din("adabT1", [128, 48])
    din("brow_g1", [128, 1024]); din("brow_g2", [128, 1024]); din("gffnT", [128, 8]); din("gmixT1", [128, 8]); din("wo", [1024, 1024])
    din("wg", [NE, 1024, FF]); din("wu", [NE, 1024, FF]); din("wd", [NE, FF, 1024]); din("wqkv1", [1024, 3072]); din("gainq", [128, 512]); din("gaink", [128, 512])
    x2 = kb.dram("x2", [TOK, D], F32, "ExternalOutput")
    qT1 = kb.dram("qT1", [128, 8, TOK], BF16, "ExternalOutput")
    kT1 = kb.dram("kT1", [128, 8, TOK], BF16, "ExternalOutput")
    V1 = kb.dram("V1", [TOK, D], BF16, "ExternalOutput")
    with es:
        cx = setup_ctx(kb, I["idf"], I["idb"], I["cT"], I["consts"])
        xres = kb.sb("xres", [128, NT, D], F32)
        kb.dma('sp', xres[:], I["xin"].t.rearrange("(t p) d -> p t d", p=128), xres, reads=[I["xin"]], writes=[xres])
        adabT0 = kb.sb("adabT0", [128, 48], F32)
        kb.dma('sp', adabT0[:], I["adabT0"][:], adabT0, reads=[I["adabT0"]], writes=[adabT0])
        adabT1 = kb.sb("adabT1", [128, 48], F32)
        kb.dma('sp', adabT1[:], I["adabT1"][:], adabT1, reads=[I["adabT1"]], writes=[adabT1])
        kb.push_scope()
        brow = kb.sb("brow", [128, 1024], F32)
        kb.dma('sp', brow[:], I["brow_g1"][:], brow, reads=[I["brow_g1"]], writes=[brow])
        g1row = mod_row(kb, cx, I["adaw0"], 2048, brow, "g1row")
        oT_sb = kb.sb("oT_sb", [128, 8, TOK], BF16)
        kb.dma('sp', oT_sb[:], I["oTin"].t.rearrange("h e t -> e h t"), oT_sb, reads=[I["oTin"]], writes=[oT_sb])
        emit_wo(kb, cx, xres, oT_sb, I["wo"], g1row)
        kb.pop_scope()
        emit_moe_layer(kb, cx, xres, I["adaw0"], adabT0, I["brow_g2"], I["gffnT"], I["wr"], I["rb"], I["wg"], I["wu"], I["wd"], do_moe)
        kb.dma('sp', x2.t.rearrange("(t p) d -> p t d", p=128), xres[:], xres, reads=[xres], writes=[x2])
        kb.push_scope()
        S_T, SH_T = mod_S(kb, cx, I["adaw1"], adabT1, 0, 1024, 0, 8, I["gmixT1"], "l1a")
        hT_all = kb.sb("hT_all", [128, 8, TOK], BF16)
        kb.push_scope()
        emit_hT(kb, cx, xres, S_T, SH_T, hT_all)
        kb.pop_scope()
        emit_qkv1(kb, cx, hT_all, I["wqkv1"], I["gainq"], I["gaink"], qT1, kT1, V1)
        kb.pop_scope()
        kb.finish([x2, qT1, kT1, V1])
    return nc


def mod_S(kb, cx, adaw, adabT_sb, col_sh, col_sc, j_sh, j_sc, gT_dram, tag):
    shT = mod_T(kb, cx, adaw, col_sh, 8, adabT_sb, j_sh, "shT" + tag)
    scT = mod_T(kb, cx, adaw, col_sc, 8, adabT_sb, j_sc, "scT" + tag)
    gT = kb.sb("gT" + tag, [128, 8], F32)
    kb.dma('sp', gT[:], gT_dram[:], gT, reads=[gT_dram], writes=[gT])
    S_T = kb.sb("S_T" + tag, [128, 8], F32)
    kb.op('dve', lambda e: e.scalar_tensor_tensor(out=S_T[:], in0=scT[:], scalar=1.0, in1=gT[:], op0=ALU.add, op1=ALU.mult), reads=[scT, gT], writes=[S_T])
    return S_T, shT


def emit_moe_layer(kb, cx, xres, adaw, adabT_sb, brow_g2_dram, gffnT_dram, wr, rb, wg, wu, wd, do_moe=True):
    kb.push_scope()
    S_T, SH_T = mod_S(kb, cx, adaw, adabT_sb, 3072, 4096, 24, 32, gffnT_dram, "ffn")
    brow = kb.sb("brow2", [128, 1024], F32)
    kb.dma('sp', brow[:], brow_g2_dram[:], brow, reads=[brow_g2_dram], writes=[brow])
    g2row = mod_row(kb, cx, adaw, 5120, brow, "g2row")
    hT_all = kb.sb("hT_all", [128, 8, TOK], BF16)
    gates = None
    logits = kb.sb("logits", [128, NT, 16], F32)
    wr_sb = kb.sb("wr_sb", [128, 8, 16], F32)
    kb.dma('sp', wr_sb[:], wr.t.rearrange("(kc p) e -> p kc e", p=128), wr_sb, reads=[wr], writes=[wr_sb])
    rb_sb = kb.sb("rb_sb", [128, 16], F32)
    kb.dma('sp', rb_sb[:], rb[:], rb_sb, reads=[rb], writes=[rb_sb])
    gates_keep = kb.sb("gates_keep", [128, NT, 16], F32)
    kb.push_scope()
    emit_hT(kb, cx, xres, S_T, SH_T, hT_all, router=(wr_sb, logits))
    kb.pop_scope()
    kb.push_scope()
    gates = emit_routing(kb, cx, logits, rb_sb)
    kb.op('dve', lambda e: e.tensor_copy(out=gates_keep[:], in_=gates[:]), reads=[gates], writes=[gates_keep])
    kb.pop_scope()
    cx.last_gates = gates_keep
    if do_moe:
        emit_moe_dense(kb, cx, xres, hT_all, gates_keep, wg, wu, wd, g2row)
    kb.pop_scope()


def emit_qkv1(kb, cx, hT_all, wqkv, gainq, gaink, qT1, kT1, V1, after_kv=None):
    Wblk = [kb.sb(f"Wblk{i}", [128, 8, 512], BF16) for i in range(3)]
    gq = kb.sb("gq", [128, 512], F32)
    kb.dma('sp', gq[:], gainq[:], gq, reads=[gainq], writes=[gq])
    kb.op('dve', lambda e: e.tensor_scalar(out=gq[:], in0=gq[:], scalar1=0.125, scalar2=None, op0=ALU.mult), reads=[gq], writes=[gq])
    gk = kb.sb("gk", [128, 512], F32)
    kb.dma('sp', gk[:], gaink[:], gk, reads=[gaink], writes=[gk])
    sq = [kb.sb(f"qsq{i}", [128, 512], F32) for i in range(2)]
    ss8 = [kb.sb(f"qss8{i}", [128, 8], F32) for i in range(2)]
    tmp = [kb.sb(f"qtmp{i}", [128, 512], F32) for i in range(2)]
    nrm = [kb.sb(f"qnrm{i}", [128, 512], BF16) for i in range(2)]
    stg = [kb.sb(f"qstg{i}", [128, 4, 128], BF16) for i in range(2)]
    vst = [kb.sb(f"vst{i}", [128, 512], BF16) for i in range(2)]
    order = [2, 3, 4, 5, 0, 1]

    def issue_w(idx):
        cbw = order[idx]
        Ww = Wblk[idx % 3]
        kb.dma('pool', Ww[:], wqkv.t[:, cbw * 512:(cbw + 1) * 512].rearrange("(kc p) n -> p kc n", p=128), Ww, reads=[wqkv], writes=[Ww])
    issue_w(0)
    issue_w(1)
    for idx, cb in enumerate(order):
        Wb_ = Wblk[idx % 3]
        if idx + 2 < 6:
            issue_w(idx + 2)
        def q1(t, cb=cb, Wb_=Wb_):
            i2 = t % 2
            pq = cx.PB[i2]
            for kc in range(8):
                kb.op('pe', lambda e, kc=kc, pq=pq, t=t, Wb_=Wb_: e.matmul(pq[:, 0:512], lhsT=hT_all[:, kc, t * 128:(t + 1) * 128], rhs=Wb_[:, kc, :], start=(kc == 0), stop=(kc == 7)),
                      reads=[hT_all, Wb_], pwrites=[pq])

        def q2(t, cb=cb):
            i2 = t % 2
            pq = cx.PB[i2]
            if cb < 4:
                g = gq if cb < 2 else gk
                dst = qT1 if cb < 2 else kT1
                kb.op('act', lambda e, pq=pq, i2=i2: e.activation(out=sq[i2][:], in_=pq[:, 0:512], func=AF.Square), reads=[pq], writes=[sq[i2]])
                kb.op('dve', lambda e, i2=i2: e.tensor_reduce(out=ss8[i2][:], in_=sq[i2][:].rearrange("p (g d) -> p g d", d=64), axis=AX.X, op=ALU.add), reads=[sq[i2]], writes=[ss8[i2]])
                kb.op('act', lambda e, i2=i2: e.activation(out=ss8[i2][:], in_=ss8[i2][:], func=AF.Sqrt, scale=1.0 / 64, bias=cx.epsb[:, 0:1]), reads=[ss8[i2], cx.epsb], writes=[ss8[i2]])
                kb.op('dve', lambda e, i2=i2: e.reciprocal(out=ss8[i2][:], in_=ss8[i2][:]), reads=[ss8[i2]], writes=[ss8[i2]])
                kb.op('dve', lambda e, i2=i2, pq=pq: e.tensor_tensor(out=tmp[i2][:].rearrange("p (g d) -> p g d", d=64), in0=pq[:, 0:512].rearrange("p (g d) -> p g d", d=64),
                                                              in1=ss8[i2][:].unsqueeze(2).to_broadcast([128, 8, 64]), op=ALU.mult), reads=[pq, ss8[i2]], writes=[tmp[i2]])
                kb.op('pool', lambda e, i2=i2, g=g: e.tensor_tensor(out=nrm[i2][:], in0=tmp[i2][:], in1=g[:], op=ALU.mult), reads=[tmp[i2], g], writes=[nrm[i2]])
                pt = cx.PB[2 + i2]
                ptv = pt[:].bitcast(BF16)
                for c in range(4):
                    kb.op('pe', lambda e, c=c, ptv=ptv, i2=i2: e.transpose(out=ptv[:, c * 128:(c + 1) * 128], in_=nrm[i2][:, c * 128:(c + 1) * 128], identity=cx.identb[:]),
                          reads=[nrm[i2], cx.identb], pwrites=[pt])
                kb.op('act', lambda e, ptv=ptv, i2=i2: e.copy(out=stg[i2][:], in_=ptv[:, 0:512].rearrange("p (c t) -> p c t", t=128)), reads=[pt], writes=[stg[i2]])
                p0 = (cb % 2) * 4
                kb.dma('sp', dst.t[:, p0:p0 + 4, t * 128:(t + 1) * 128], stg[i2][:], stg[i2], reads=[stg[i2]], pwrites=[dst])
            else:
                kb.op('act', lambda e, pq=pq, i2=i2: e.copy(out=vst[i2][:], in_=pq[:, 0:512]), reads=[pq], writes=[vst[i2]])
                c0 = (cb - 4) * 512
                kb.dma('sp', V1.t[t * 128:(t + 1) * 128, c0:c0 + 512], vst[i2][:], vst[i2], reads=[vst[i2]], pwrites=[V1])
        q1(0)
        for t in range(NT):
            if t + 1 < NT:
                q1(t + 1)
            q2(t)
        if cb == 5 and after_kv is not None:
            after_kv()


def host_inputs_C(inp, j, x2, qT1, kT1, V1, kT_prev, V_prev):
    f32 = np.float32
    d = host_common(inp, j, 1)
    d["xin"] = np.ascontiguousarray(x2)
    d["qT1"] = np.ascontiguousarray(qT1); d["kT1"] = np.ascontiguousarray(kT1); d["V1"] = np.ascontiguousarray(V1)
    if kT_prev is None:
        d["kTh"] = np.zeros((128, 8, 512), NPBF); d["Vh"] = np.zeros((512, D), NPBF)
        d["hmask"] = np.full((128, 1), -30000.0, f32)
    else:
        d["kTh"] = np.ascontiguousarray(kT_prev[:, :, TOK - 512:]); d["Vh"] = np.ascontiguousarray(V_prev[TOK - 512:])
        d["hmask"] = np.zeros((128, 1), f32)
    rb = inp["b_rel_bias"][0]
    kk = np.arange(128)[:, None]; qq = np.arange(128)[None, :]
    tiles = np.zeros((128, 5, 16, 128), f32)
    mask = np.zeros((128, 5, 128), f32)
    for r in range(5):
        rel = (r - 4) * 128 + kk - qq
        idx = np.clip(rel, -256, 256) + 256
        dc = 2 * (r - 4) + kk // 64 - qq // 64
        mask[:, r, :] = np.where((dc >= -8) & (dc <= 0), 0.0, -30000.0)
        for half in range(2):
            for par in range(2):
                for p in range(4):
                    hd = half * 8 + 2 * p + par
                    tiles[:, r, half * 8 + par * 4 + p, :] = rb[hd][idx]
    d["relb"] = tiles; d["bmask"] = mask
    d["adaw1"] = np.ascontiguousarray(inp["ada_w"][1])
    d["adabT1"] = np.ascontiguousarray(inp["ada_b"][1].reshape(48, 128).T)
    d["brow_g1"] = rep(inp["ada_b"][1][2048:3072]); d["brow_g2"] = rep(inp["ada_b"][1][5120:6144])
    d["gffnT"] = np.ascontiguousarray(inp["norm_ffn_g"][1].reshape(8, 128).T)
    d["wo"] = np.ascontiguousarray(inp["b_w_o"][0])
    d["wg"] = np.ascontiguousarray(inp["moe_w_gate"][1]); d["wu"] = np.ascontiguousarray(inp["moe_w_up"][1]); d["wd"] = np.ascontiguousarray(inp["moe_w_down"][1])
    return d


def build_C(nc, do_moe=True):
    es = ExitStack()
    kb = KB(nc, es)
    I = {}
    def din(name, shape, dt=F32):
        I[name] = kb.dram(name, shape, dt, "ExternalInput")
    din("cT", [128, 8, 1]); din("idf", [128, 128]); din("idb", [128, 128], BF16); din("consts", [128, 20]); din("wr", [1024, 16]); din("rb", [128, 16])
    din("xin", [TOK, D]); din("qT1", [128, 8, TOK], BF16); din("kT1", [128, 8, TOK], BF16); din("V1", [TOK, D], BF16)
    din("kTh", [128, 8, 512], BF16); din("Vh", [512, D], BF16); din("hmask", [128, 1]); din("relb", [128, 5, 16, 128]); din("bmask", [128, 5, 128])
    din("adaw1", [1024, 6144]); din("adabT1", [128, 48]); din("brow_g1", [128, 1024]); din("brow_g2", [128, 1024]); din("gffnT", [128, 8]); din("wo", [1024, 1024])
    din("wg", [NE, 1024, FF]); din("wu", [NE, 1024, FF]); din("wd", [NE, FF, 1024])
    out = kb.dram("out", [TOK, D], F32, "ExternalOutput")
    with es:
        cx = setup_ctx(kb, I["idf"], I["idb"], I["cT"], I["consts"])
        xres = kb.sb("xres", [128, NT, D], F32)
        kb.dma('sp', xres[:], I["xin"].t.rearrange("(t p) d -> p t d", p=128), xres, reads=[I["xin"]], writes=[xres])
        adabT1 = kb.sb("adabT1", [128, 48], F32)
        kb.dma('sp', adabT1[:], I["adabT1"][:], adabT1, reads=[I["adabT1"]], writes=[adabT1])
        emit_attn1(kb, cx, xres, I)
        emit_moe_layer(kb, cx, xres, I["adaw1"], adabT1, I["brow_g2"], I["gffnT"], I["wr"], I["rb"], I["wg"], I["wu"], I["wd"], do_moe)
        kb.dma('sp', out.t.rearrange("(t p) d -> p t d", p=128), xres[:], xres, reads=[xres], writes=[out])
        kb.finish([out])
    return nc


def emit_attn1(kb, cx, xres, I):
    kb.push_scope()
    brow = kb.sb("brow", [128, 1024], F32)
    kb.dma('sp', brow[:], I["brow_g1"][:], brow, reads=[I["brow_g1"]], writes=[brow])
    g1row = mod_row(kb, cx, I["adaw1"], 2048, brow, "g1row")
    Wo = kb.sb("Wo", [128, 8, D], BF16)
    kb.dma('pool', Wo[:], I["wo"].t.rearrange("(c p) o -> p c o", p=128), Wo, reads=[I["wo"]], writes=[Wo])
    g1b = kb.sb("g1b", [128, D], BF16)
    kb.op('dve', lambda e: e.tensor_copy(out=g1b[:], in_=g1row[:]), reads=[g1row], writes=[g1b])
    kb.op('pool', lambda e: e.tensor_tensor(out=Wo[:], in0=Wo[:], in1=g1b[:].unsqueeze(1).to_broadcast([128, 8, D]), op=ALU.mult), reads=[Wo, g1b], writes=[Wo])
    hmask = kb.sb("hmask", [128, 1], F32)
    kb.dma('sp', hmask[:], I["hmask"][:], hmask, reads=[I["hmask"]], writes=[hmask])
    bmask = kb.sb("bmask", [128, 5, 128], BF16)
    kb.dma('pool', bmask[:], I["bmask"][:], bmask, reads=[I["bmask"]], writes=[bmask])
    qTh = kb.sb("qTh", [128, 4, TOK], BF16)
    kTa = kb.sb("kTa", [128, 4, TOK + 512], BF16)
    Va = kb.sb("Va", [128, 20, 8, 65], BF16)
    kb.op('pool', lambda e: e.memset(Va[:, :, :, 64:65], 1.0), pwrites=[Va])
    relb = kb.sb("relb", [128, 5, 8, 128], BF16)
    Pb = [kb.sb(f"P1_{i}", [128, 512], BF16) for i in range(4)]
    r4 = [kb.sb(f"r4_{i}", [128, 4], F32) for i in range(2)]
    onh = [kb.sb(f"onh{i}", [128, 512], BF16) for i in range(2)]
    oTt = [kb.sb(f"oTt{i}", [128, 4, 128], BF16) for i in range(2)]
    PB = cx.PB
    for half in range(2):
        kb.dma('sp', qTh[:], I["qT1"].t[:, half * 4:(half + 1) * 4, :], qTh, reads=[I["qT1"]], writes=[qTh])
        kb.dma('sp', kTa[:, :, 0:512], I["kTh"].t[:, half * 4:(half + 1) * 4, :], kTa, reads=[I["kTh"]], writes=[kTa])
        kb.dma('sp', kTa[:, :, 512:], I["kT1"].t[:, half * 4:(half + 1) * 4, :], kTa, reads=[I["kT1"]], pwrites=[kTa])
        kb.barrier_bufs = None
        for a in range(20):
            src = I["Vh"] if a < 4 else I["V1"]
            ra = a if a < 4 else a - 4
            kb.dma('sp', Va[:, a, :, 0:64], src.t[ra * 128:(ra + 1) * 128, half * 512:(half + 1) * 512].rearrange("p (h d) -> p h d", d=64), Va,
                   reads=[src], writes=([Va] if (a == 0) else []), pwrites=([] if a == 0 else [Va]))
        kb.dma('pool', relb[:], I["relb"].t[:, :, half * 8:(half + 1) * 8, :], relb, reads=[I["relb"]], writes=[relb])
        kb.op('dve', lambda e: e.tensor_tensor(out=relb[:], in0=relb[:], in1=bmask[:].unsqueeze(2).to_broadcast([128, 5, 8, 128]), op=ALU.add), reads=[relb, bmask], writes=[relb])
        accb = [PB[4], PB[5]]
        steps = [(t, r, par) for t in range(NT) for r in range(5) for par in range(2)]
        LA = 3

        def e_qk(s):
            t, r, par = steps[s]
            a_ = t + r
            Sp = PB[s % 4]
            for p in range(4):
                kb.op('pe', lambda e, Sp=Sp, p=p, par=par, a_=a_, t=t: e.matmul(Sp[:, p * 128:(p + 1) * 128], lhsT=kTa[par * 64:(par + 1) * 64, p, a_ * 128:(a_ + 1) * 128],
                                                                         rhs=qTh[par * 64:(par + 1) * 64, p, t * 128:(t + 1) * 128], start=True, stop=True),
                      reads=[kTa, qTh], pwrites=[Sp])

        def e_rest(s):
            t, r, par = steps[s]
            a_ = t + r
            Sp = PB[s % 4]
            P = Pb[s % 4]
            kb.op('dve', lambda e, Sp=Sp, r=r, par=par: e.tensor_tensor(out=Sp[:, 0:512], in0=Sp[:, 0:512], in1=relb[:, r, par * 4:(par + 1) * 4, :].rearrange("p h q -> p (h q)"), op=ALU.add),
                  reads=[Sp, relb], writes=[Sp])
            if a_ < 4:
                kb.op('act', lambda e, Sp=Sp, P=P: e.activation(out=P[:], in_=Sp[:, 0:512], func=AF.Exp, bias=hmask[:, 0:1]), reads=[Sp, hmask], writes=[P])
            else:
                kb.op('act', lambda e, Sp=Sp, P=P: e.activation(out=P[:], in_=Sp[:, 0:512], func=AF.Exp), reads=[Sp], writes=[P])
            for p in range(4):
                hs = 2 * p + par
                kb.op('pe', lambda e, P=P, p=p, par=par, hs=hs, a_=a_, r=r: e.matmul(accb[par][:, p * 65:(p + 1) * 65], lhsT=P[:, p * 128:(p + 1) * 128], rhs=Va[:, a_, hs, :],
                                                                              start=(r == 0 and p == 0), stop=(r == 4), skip_group_check=True),
                      reads=[P, Va], pwrites=[accb[par]])

        def e_fin(t):
            i2 = t % 2
            for par in range(2):
                av = accb[par][:, 0:260].rearrange("p (h d) -> p h d", d=65)
                kb.op('dve', lambda e, av=av, i2=i2: e.reciprocal(out=r4[i2][:], in_=av[:, :, 64]), reads=[accb[par]], writes=[r4[i2]])
                kb.op('dve', lambda e, av=av, i2=i2, par=par: e.tensor_tensor(out=onh[i2][:].rearrange("p (h two d) -> p h two d", two=2, d=64)[:, :, par, :], in0=av[:, :, 0:64],
                                                                        in1=r4[i2][:].unsqueeze(2).to_broadcast([128, 4, 64]), op=ALU.mult),
                      reads=[accb[par], r4[i2]], pwrites=[onh[i2]])

        def e_tail(t):
            i2 = t % 2
            pt = PB[6]
            ptv = pt[:].bitcast(BF16)
            for c in range(4):
                kb.op('pe', lambda e, c=c, i2=i2, ptv=ptv: e.transpose(out=ptv[:, c * 128:(c + 1) * 128], in_=onh[i2][:, c * 128:(c + 1) * 128], identity=cx.identb[:]),
                      reads=[onh[i2], cx.identb], pwrites=[pt])
            kb.op('act', lambda e, ptv=ptv, i2=i2: e.copy(out=oTt[i2][:], in_=ptv[:, 0:512].rearrange("p (c t) -> p c t", t=128)), reads=[pt], writes=[oTt[i2]])
            for hf in range(2):
                py = PB[7]
                for c in range(4):
                    kb.op('pe', lambda e, c=c, hf=hf, i2=i2, py=py, half=half: e.matmul(py[:, 0:512], lhsT=oTt[i2][:, c, :], rhs=Wo[:, half * 4 + c, hf * 512:(hf + 1) * 512],
                                                                                 start=(c == 0), stop=(c == 3)), reads=[oTt[i2], Wo], pwrites=[py])
                kb.op('dve', lambda e, hf=hf, py=py, t=t: e.tensor_tensor(out=xres[:, t, hf * 512:(hf + 1) * 512], in0=py[:, 0:512], in1=xres[:, t, hf * 512:(hf + 1) * 512], op=ALU.add),
                      reads=[py, xres], pwrites=[xres])

        ns = len(steps)
        for s0 in range(min(LA, ns)):
            e_qk(s0)
        for s_ in range(ns):
            if s_ + LA < ns:
                e_qk(s_ + LA)
            e_rest(s_)
            t, r, par = steps[s_]
            if r == 4 and par == 1:
                e_fin(t)
            if r == 1 and par == 1 and t > 0:
                e_tail(t - 1)
        e_tail(NT - 1)
    kb.pop_scope()


GROUPS = [[0, 1, 2, 3], [4, 5, 6, 7]]


def host_inputs_F(inp, j):
    f32 = np.float32
    b, i = j // 4, j % 4
    d = {}
    a0 = host_inputs_A(inp, 2 * i)
    a1 = host_inputs_A(inp, 2 * i + 1)
    d["x_b"] = np.ascontiguousarray(inp["x"][b])
    cb = inp["c"][b].reshape(8, 128).T
    d["cT2"] = np.ascontiguousarray(np.stack([cb, cb], axis=2))
    d["gmixT0"] = a0["gT"]
    d["w2"] = np.ascontiguousarray(np.stack([a0["w"], a1["w"]], 0))
    d["gain"] = a0["gain"]; d["lam"] = a0["lam"]; d["subg"] = a0["subg"]; d["maskT"] = a0["maskT"]
    d["biasT2"] = np.ascontiguousarray(np.stack([a0["biasT"], a1["biasT"]], 1))
    d["c15_2"] = np.ascontiguousarray(np.concatenate([a0["c15"], a1["c15"]], 1))
    d.update(host_common(inp, j, 0))
    r0 = j * TOK
    d["xin"] = np.ascontiguousarray(inp["x"].reshape(-1, D)[r0:r0 + TOK])
    d["adaw0"] = np.ascontiguousarray(inp["ada_w"][0]); d["adaw1"] = np.ascontiguousarray(inp["ada_w"][1])
    d["adabT0"] = np.ascontiguousarray(inp["ada_b"][0].reshape(48, 128).T)
    d["adabT1"] = np.ascontiguousarray(inp["ada_b"][1].reshape(48, 128).T)
    for l in range(2):
        d[f"brow_g1_{l}"] = rep(inp["ada_b"][l][2048:3072]); d[f"brow_g2_{l}"] = rep(inp["ada_b"][l][5120:6144])
        d[f"gffnT{l}"] = np.ascontiguousarray(inp["norm_ffn_g"][l].reshape(8, 128).T)
        d[f"wg{l}"] = np.ascontiguousarray(inp["moe_w_gate"][l]); d[f"wu{l}"] = np.ascontiguousarray(inp["moe_w_up"][l]); d[f"wd{l}"] = np.ascontiguousarray(inp["moe_w_down"][l])
    d["gmixT1"] = np.ascontiguousarray(inp["norm_mix_g"][1].reshape(8, 128).T)
    d["wo0"] = np.ascontiguousarray(inp["a_w_o"][0]); d["wo1"] = np.ascontiguousarray(inp["b_w_o"][0])
    d["wqkv1"] = np.ascontiguousarray(inp["b_w_qkv"][0])
    d["gainq"] = rep(np.tile(inp["b_q_gain"][0], 8)); d["gaink"] = rep(np.tile(inp["b_k_gain"][0], 8))
    cdum = host_inputs_C(inp, j, np.zeros((1, 1), f32), np.zeros((1, 1), NPBF), np.zeros((1, 1), NPBF), np.zeros((1, 1), NPBF), None, None)
    d["relb"] = cdum["relb"]; d["bmask"] = cdum["bmask"]
    d["hmask"] = np.full((128, 1), -30000.0 if i == 0 else 0.0, f32)
    p = np.arange(128)
    d["idx_o"] = np.ascontiguousarray(np.stack([(i * 1024 + (h // 2) * 256 + (h % 2) * 128 + p) for h in range(8)], 1)).astype(np.int32)
    prev = max(i - 1, 0)
    d["idx_k"] = (prev * 128 + p).astype(np.int32).reshape(128, 1)
    d["idx_v"] = np.ascontiguousarray(np.stack([prev * 512 + a * 128 + p for a in range(4)], 1)).astype(np.int32)
    return d


def build_F(nc, stage=99):
    es = ExitStack()
    kb = KB(nc, es)
    I = {}
    def din(name, shape, dt=F32):
        I[name] = kb.dram(name, shape, dt, "ExternalInput")
    def dint(name, shape, dt=BF16):
        I[name] = kb.dram(name, shape, dt, "Internal")
    din("x_b", [8192, D]); din("cT2", [128, 8, 2]); din("gmixT0", [128, 8]); din("w2", [2, 1024, 384]); din("gain", [128, 256]); din("lam", [128, 256])
    din("subg", [128, 128]); din("maskT", [128, 4, 512]); din("biasT2", [128, 2, 9, 512]); din("c15_2", [128, 2])
    din("cT", [128, 8, 1]); din("idf", [128, 128]); din("idb", [128, 128], BF16); din("consts", [128, 20]); din("wr", [1024, 16]); din("rb", [128, 16])
    din("xin", [TOK, D]); din("adaw0", [1024, 6144]); din("adaw1", [1024, 6144]); din("adabT0", [128, 48]); din("adabT1", [128, 48])
    for l in range(2):
        din(f"brow_g1_{l}", [128, 1024]); din(f"brow_g2_{l}", [128, 1024]); din(f"gffnT{l}", [128, 8])
        din(f"wg{l}", [NE, 1024, FF]); din(f"wu{l}", [NE, 1024, FF]); din(f"wd{l}", [NE, FF, 1024])
    din("gmixT1", [128, 8]); din("wo0", [1024, 1024]); din("wo1", [1024, 1024]); din("wqkv1", [1024, 3072]); din("gainq", [128, 512]); din("gaink", [128, 512])
    din("relb", [128, 5, 16, 128]); din("bmask", [128, 5, 128]); din("hmask", [128, 1])
    din("idx_o", [128, 8], I32); din("idx_k", [128, 1], I32); din("idx_v", [128, 4], I32)
    out = kb.dram("out", [TOK, D], F32, "ExternalOutput")
    for a in range(4):
        dint(f"ag_in{a}", [2, 128, TOK])
    dint("ag_out", [4 * 4 * 2 * 128, TOK]); dint("xTs", [64, 128, 1024])
    dint("qT1", [128, 8, TOK]); dint("kT1", [128, 8, TOK]); dint("V1", [TOK, D])
    dint("agk_in", [128, 4096]); dint("agk_out", [512, 4096]); dint("agv_in", [512, D]); dint("agv_out", [2048, D]); dint("kTh", [128, 8, 512]); dint("Vh", [512, D])
    with es:
        cx = setup_ctx(kb, I["idf"], I["idb"], I["cT"], I["consts"])
        dummy = kb.sb("ccdummy", [128, 1], F32)
        idx_o = kb.sb("idx_o", [128, 8], I32); idx_k = kb.sb("idx_k", [128, 1], I32); idx_v = kb.sb("idx_v", [128, 4], I32)
        kb.dma('sp', idx_o[:], I["idx_o"][:], idx_o, reads=[I["idx_o"]], writes=[idx_o])
        kb.dma('sp', idx_k[:], I["idx_k"][:], idx_k, reads=[I["idx_k"]], writes=[idx_k])
        kb.dma('sp', idx_v[:], I["idx_v"][:], idx_v, reads=[I["idx_v"]], writes=[idx_v])
        kb.push_scope()
        adabT0a = kb.sb("adabT0a", [128, 16], F32)
        ag_ins = [I[f"ag_in{a}"] for a in range(4)]

        deferred = []

        def after_chunk(a, force=False):
            if a == 3 and not force:
                deferred.append(a)
                return
            kb.collective_allgather(T(ag_ins[a].t.rearrange("h e t -> (h e) t"), ag_ins[a].b),
                                    T(I["ag_out"].t[a * 1024:(a + 1) * 1024, :], I["ag_out"].b), GROUPS, dummy, partial=True)
        emit_A2(kb, cx, I["x_b"], I["cT2"], I["adaw0"], kb_slice(I["adabT0"], 16), I["gmixT0"], I["w2"], I["gain"], I["lam"], I["subg"], I["biasT2"], I["c15_2"],
                I["maskT"], ag_ins, after_chunk, I["xTs"])
        kb.pop_scope()
        for a in deferred:
            after_chunk(a, force=True)
        if stage == 1:
            kb.finish([out]); return nc
        xres = kb.sb("xres", [128, NT, D], F32)
        kb.dma('sp', xres[:], I["xin"].t.rearrange("(t p) d -> p t d", p=128), xres, reads=[I["xin"]], writes=[xres])
        adabT0 = kb.sb("adabT0", [128, 48], F32)
        kb.dma('sp', adabT0[:], I["adabT0"][:], adabT0, reads=[I["adabT0"]], writes=[adabT0])
        adabT1 = kb.sb("adabT1", [128, 48], F32)
        kb.dma('sp', adabT1[:], I["adabT1"][:], adabT1, reads=[I["adabT1"]], writes=[adabT1])
        kb.push_scope()
        brow = kb.sb("brow", [128, 1024], F32)
        kb.dma('sp', brow[:], I["brow_g1_0"][:], brow, reads=[I["brow_g1_0"]], writes=[brow])
        g1row = mod_row(kb, cx, I["adaw0"], 2048, brow, "g1row")
        oT_sb = kb.sb("oT_sb", [128, 8, TOK], BF16)
        for h in range(8):
            kb.igather(oT_sb[:, h, :], I["ag_out"][:, :], idx_o[:, h:h + 1], oT_sb, reads=[I["ag_out"], idx_o],
                       writes=([oT_sb] if h == 0 else []), pwrites=([] if h == 0 else [oT_sb]))
        emit_wo(kb, cx, xres, oT_sb, I["wo0"], g1row)
        kb.pop_scope()
        if stage == 2:
            kb.dma('sp', out.t.rearrange("(t p) d -> p t d", p=128), xres[:], xres, reads=[xres], writes=[out])
            kb.finish([out]); return nc
        emit_moe_layer(kb, cx, xres, I["adaw0"], adabT0, I["brow_g2_0"], I["gffnT0"], I["wr"], I["rb"], I["wg0"], I["wu0"], I["wd0"], True)
        kb.push_scope()
        S_T, SH_T = mod_S(kb, cx, I["adaw1"], adabT1, 0, 1024, 0, 8, I["gmixT1"], "l1a")
        hT_all = kb.sb("hT_all", [128, 8, TOK], BF16)
        kb.push_scope()
        emit_hT(kb, cx, xres, S_T, SH_T, hT_all)
        kb.pop_scope()
        def after_kv():
            cpown = Buf("cpown")
            kb.all_bufs.append(cpown)
            kb.dma('sp', I["agk_in"].t.rearrange("p (h t) -> p h t", t=512), I["kT1"].t[:, :, TOK - 512:], cpown, reads=[I["kT1"]], writes=[I["agk_in"]])
            kb.dma('sp', I["agv_in"].t[:, :], I["V1"].t[TOK - 512:, :], cpown, reads=[I["V1"]], writes=[I["agv_in"]])
            kb.collective_allgather(I["agk_in"], I["agk_out"], GROUPS, dummy)
            kb.collective_allgather(I["agv_in"], I["agv_out"], GROUPS, dummy)
        emit_qkv1(kb, cx, hT_all, I["wqkv1"], I["gainq"], I["gaink"], I["qT1"], I["kT1"], I["V1"], after_kv)
        kb.pop_scope()
        if stage == 3:
            kb.dma('sp', out.t.rearrange("(t p) d -> p t d", p=128), xres[:], xres, reads=[xres], writes=[out])
            kb.finish([out]); return nc
        kb.push_scope()
        kst = kb.sb("kst", [128, 4096], BF16)
        vst = kb.sb("vhst", [128, 4, D], BF16)
        kb.igather(kst[:, :], I["agk_out"][:, :], idx_k[:, 0:1], kst, reads=[I["agk_out"], idx_k], writes=[kst])
        for a in range(4):
            kb.igather(vst[:, a, :], I["agv_out"][:, :], idx_v[:, a:a + 1], vst, reads=[I["agv_out"], idx_v],
                       writes=([vst] if a == 0 else []), pwrites=([] if a == 0 else [vst]))
        kb.dma('sp', I["kTh"].t.rearrange("p h t -> p (h t)"), kst[:], kst, reads=[kst], writes=[I["kTh"]])
        kb.dma('sp', I["Vh"].t.rearrange("(a p) c -> p a c", p=128), vst[:], vst, reads=[vst], writes=[I["Vh"]])
        kb.pop_scope()
        if stage == 4:
            kb.dma('sp', out.t.rearrange("(t p) d -> p t d", p=128), xres[:], xres, reads=[xres], writes=[out])
            kb.finish([out]); return nc
        IC = {"brow_g1": I["brow_g1_1"], "adaw1": I["adaw1"], "wo": I["wo1"], "hmask": I["hmask"], "bmask": I["bmask"], "qT1": I["qT1"], "kTh": I["kTh"],
              "kT1": I["kT1"], "Vh": I["Vh"], "V1": I["V1"], "relb": I["relb"]}
        emit_attn1(kb, cx, xres, IC)
        emit_moe_layer(kb, cx, xres, I["adaw1"], adabT1, I["brow_g2_1"], I["gffnT1"], I["wr"], I["rb"], I["wg1"], I["wu1"], I["wd1"], True)
        kb.dma('sp', out.t.rearrange("(t p) d -> p t d", p=128), xres[:], xres, reads=[xres], writes=[out])
        kb.finish([out])
    return nc


def kb_slice(Td, ncol):
    return T(Td.t[:, 0:ncol], Td.b)


_NC_CACHE = {}


def kernel(**inputs):
    inp = {k: np.asarray(v) for k, v in inputs.items()}
    cores = list(range(8))
    if "F" not in _NC_CACHE:
        nc = bass.Bass("TRN2", target_bir_lowering=False)
        build_F(nc)
        _NC_CACHE["F"] = nc
    res = run_bass_kernel_spmd(_NC_CACHE["F"], [host_inputs_F(inp, j) for j in cores], core_ids=cores)
    out = np.concatenate([np.asarray(res.results[j]["out"]) for j in cores], axis=0)
    return out.reshape(2, 8192, 1024).astype(np.float32)
```
